# Optimizing a Trainium2 kernel written in Bass

```python
import math
import jax
import jax.numpy as jnp
from jax import lax
import numpy as np

D_MODEL = 1024
BATCH = 8
SEQ = 2048
DEPTH = 2

GRID_W = 64
CTX_LEN = 256
HEAD_DIM = 64
N_MIXERS = 4
GROUP_WIDTH = D_MODEL // N_MIXERS
MIX_WIDTH = N_MIXERS * GROUP_WIDTH
NA_HEADS = GROUP_WIDTH // HEAD_DIM
NA_ROWS = 8
NA_COLS = 16
DIFF_HEADS = GROUP_WIDTH // HEAD_DIM
DIFF_QK_DIM = HEAD_DIM // 2
POOL_WINDOWS = (2, 4, 8, 16)
POOL_GROUP = GROUP_WIDTH // len(POOL_WINDOWS)
FFT_GROUPS = 4
FFT_GROUP = GROUP_WIDTH // FFT_GROUPS
OFF_NA_Q = 0 * GROUP_WIDTH
OFF_NA_K = 1 * GROUP_WIDTH
OFF_NA_V = 2 * GROUP_WIDTH
OFF_DF_Q = 3 * GROUP_WIDTH
OFF_DF_K = 4 * GROUP_WIDTH
OFF_DF_V = 5 * GROUP_WIDTH
OFF_POOL = 6 * GROUP_WIDTH
OFF_FFT = 7 * GROUP_WIDTH
IN_WIDTH = 8 * GROUP_WIDTH
N_EXPERTS = 32
TOP_K = 4
D_FF = D_MODEL
SWIGLU_ALPHA = 1.702
SWIGLU_LIMIT = 7.0
EXPERT_BLOCK = 256
Q_BLOCK = 128
ROPE_THETA = 10000.0
NORM_EPS = 1e-6
MASK_VALUE = -1e30

kernel_name = 'hybrid_prefix_dit_block'


def rms_norm(x, g):
    xf = x.astype(jnp.float32)
    y = xf * lax.rsqrt(jnp.mean(xf * xf, axis=-1, keepdims=True) + NORM_EPS)
    return (y * g.astype(jnp.float32)).astype(x.dtype)


def modulate(x, g, shift, scale):
    return rms_norm(x, g) * (1 + scale) + shift


def split_heads(t, n_heads):
    b, l, _ = t.shape
    return t.reshape(b, l, n_heads, -1).transpose(0, 2, 1, 3)


def merge_heads(t):
    b, h, l, d = t.shape
    return t.transpose(0, 2, 1, 3).reshape(b, l, h * d)


def diff_split(t):
    b, l, _ = t.shape
    return t.reshape(b, l, DIFF_HEADS, 2, DIFF_QK_DIM).transpose(0, 2, 3, 1, 4)


def axial_rope(t):
    l = t.shape[-2]
    pos = jnp.arange(l)
    half = DIFF_QK_DIM // 2
    inv = ROPE_THETA ** (-jnp.arange(0, half, 2, dtype=jnp.float32) / half)
    tf = t.astype(jnp.float32)
    outs = []
    for axis_pos, seg in ((pos // GRID_W, tf[..., :half]), (pos % GRID_W, tf[..., half:])):
        ang = axis_pos.astype(jnp.float32)[:, None] * inv
        cos, sin = jnp.cos(ang), jnp.sin(ang)
        s1, s2 = seg[..., :half // 2], seg[..., half // 2:]
        outs += [s1 * cos - s2 * sin, s2 * cos + s1 * sin]
    return jnp.concatenate(outs, axis=-1).astype(t.dtype)


def neighbourhood_attention(q, k, v, kc, vc, rpb):
    b, h, l, d = q.shape
    rows = l // GRID_W
    kr = min(NA_ROWS, rows)
    qg = q.reshape(b, h, rows, GRID_W, d)
    kg = k.reshape(b, h, rows, GRID_W, d)
    vg = v.reshape(b, h, rows, GRID_W, d)
    r = jnp.arange(rows)
    row_start = jnp.clip(r - NA_ROWS // 2, 0, rows - kr)
    row_idx = row_start[:, None] + jnp.arange(kr)
    k_rows = kg[:, :, row_idx]
    v_rows = vg[:, :, row_idx]
    col = jnp.arange(GRID_W)
    col_start = jnp.clip(col - NA_COLS // 2, 0, GRID_W - NA_COLS)
    in_win = (col[None, :] >= col_start[:, None]) & (col[None, :] < col_start[:, None] + NA_COLS)
    rel_r = row_idx - r[:, None] + NA_ROWS - 1
    rel_c = jnp.clip(col[None, :] - col[:, None], 1 - NA_COLS, NA_COLS - 1) + NA_COLS - 1
    bias = rpb.astype(jnp.float32)[:, rel_r[:, None, :, None], rel_c[None, :, None, :]]
    scale = d ** -0.5
    s_loc = jnp.einsum('bhrqd,bhrkcd->bhrqkc', qg, k_rows).astype(jnp.float32) * scale + bias
    s_loc = jnp.where(in_win[:, None, :], s_loc, MASK_VALUE)
    s_ctx = jnp.einsum('bhrqd,bhjd->bhrqj', qg, kc).astype(jnp.float32) * scale
    n_loc = kr * GRID_W
    s_all = jnp.concatenate([s_loc.reshape(b, h, rows, GRID_W, n_loc), s_ctx], axis=-1)
    p = jax.nn.softmax(s_all, axis=-1).astype(v.dtype)
    p_loc = p[..., :n_loc].reshape(b, h, rows, GRID_W, kr, GRID_W)
    out = (jnp.einsum('bhrqkc,bhrkcd->bhrqd', p_loc, v_rows)
           + jnp.einsum('bhrqj,bhjd->bhrqd', p[..., n_loc:], vc))
    return out.reshape(b, h, l, d)


def context_attention(q, k, v):
    s = jnp.einsum('bhqd,bhkd->bhqk', q, k).astype(jnp.float32) * q.shape[-1] ** -0.5
    return jnp.einsum('bhqk,bhkd->bhqd', jax.nn.softmax(s, axis=-1).astype(v.dtype), v)


def differential_attention(q, k, v, lam):
    s = jnp.einsum('bhiqd,bhikd->bhiqk', q, k).astype(jnp.float32) * q.shape[-1] ** -0.5
    p = jax.nn.softmax(s, axis=-1)
    a = p[:, :, 0] - lam * p[:, :, 1]
    return jnp.einsum('bhqk,bhkd->bhqd', a.astype(v.dtype), v)


def multiscale_pool(t, w, scale):
    b, l, _ = t.shape
    tf = t.astype(jnp.float32).reshape(b, l, len(POOL_WINDOWS), POOL_GROUP)
    cs = jnp.concatenate([jnp.zeros((b, 1, len(POOL_WINDOWS), POOL_GROUP), jnp.float32),
                          jnp.cumsum(tf, axis=1)], axis=1)
    pos = jnp.arange(l)
    outs = []
    for g, win in enumerate(POOL_WINDOWS):
        lo = jnp.clip(pos - win // 2, 0, l)
        hi = jnp.clip(pos - win // 2 + win, 0, l)
        csg = cs[:, :, g]
        mean = (csg[:, hi] - csg[:, lo]) / (hi - lo).astype(jnp.float32)[:, None]
        outs.append(mean - tf[:, :, g])
    m = jnp.stack(outs, axis=2)
    y = jnp.einsum('blgc,gce->blge', m, w.astype(jnp.float32)).reshape(b, l, GROUP_WIDTH)
    return (y * scale.astype(jnp.float32)).astype(t.dtype)


def fourier_mix(t, w):
    b, l, _ = t.shape
    tg = t.astype(jnp.float32).reshape(b, l, FFT_GROUPS, FFT_GROUP)
    f = jnp.fft.fft2(tg, axes=(1, 3), norm='ortho').real
    y = jnp.einsum('blgc,gce->blge', f, w.astype(jnp.float32)).reshape(b, l, GROUP_WIDTH)
    return y.astype(t.dtype)


def clamped_swiglu(u):
    glu = jnp.minimum(u[..., ::2], SWIGLU_LIMIT)
    lin = jnp.clip(u[..., 1::2], -SWIGLU_LIMIT, SWIGLU_LIMIT)
    return glu * jax.nn.sigmoid(SWIGLU_ALPHA * glu) * (lin + 1)


def expert_ffn(h, router_w, router_b, w1, b1, w2, b2):
    n, dm = h.shape
    logits = (h @ router_w).astype(jnp.float32) + router_b.astype(jnp.float32)
    top_val, top_idx = lax.top_k(logits, TOP_K)
    gate = jax.nn.softmax(top_val, axis=-1)
    n_assign = n * TOP_K
    flat_e = top_idx.reshape(n_assign)
    order = jnp.argsort(flat_e)
    sorted_e = flat_e[order]
    counts = jnp.bincount(flat_e, length=N_EXPERTS)
    padded = (counts + EXPERT_BLOCK - 1) // EXPERT_BLOCK * EXPERT_BLOCK
    group_start = jnp.cumsum(counts) - counts
    padded_end = jnp.cumsum(padded)
    padded_start = padded_end - padded
    dest = padded_start[sorted_e] + jnp.arange(n_assign) - group_start[sorted_e]
    n_blocks = -(-n_assign // EXPERT_BLOCK) + N_EXPERTS
    slots = n_blocks * EXPERT_BLOCK
    slot_tok = jnp.zeros((slots,), jnp.int32).at[dest].set((order // TOP_K).astype(jnp.int32))
    slot_w = jnp.zeros((slots,), jnp.float32).at[dest].set(gate.reshape(n_assign)[order])
    block_e = jnp.minimum(jnp.searchsorted(padded_end, jnp.arange(n_blocks) * EXPERT_BLOCK, side='right'),
                          N_EXPERTS - 1)

    def run_block(args):
        e, tok = args
        u = h[tok] @ w1[e] + b1[e]
        return clamped_swiglu(u) @ w2[e] + b2[e]

    y = lax.map(run_block, (block_e, slot_tok.reshape(n_blocks, EXPERT_BLOCK)))
    y = y.reshape(slots, dm) * slot_w[:, None].astype(h.dtype)
    return jnp.zeros_like(h).at[slot_tok].add(y)


def hybrid_layer(x, cx, c_act, cctx_act, p, layer_idx, ctx_out):
    b, l, dm = x.shape
    lc = cx.shape[1]
    gw = GROUP_WIDTH
    mod_x = (c_act @ p['w_ada'] + p['b_ada'])[:, None, :]
    mod_c = cctx_act @ p['w_ada'] + p['b_ada']
    sh1, sc1, g1, sh2, sc2, g2 = jnp.split(mod_x, 6, axis=-1)
    ch1, cs1, cg1, ch2, cs2, cg2 = jnp.split(mod_c, 6, axis=-1)
    w_in = p['w_in']

    hx = modulate(x, p['g1'], sh1, sc1)
    hc = modulate(cx, p['g1'], ch1, cs1)
    px = hx @ w_in
    if ctx_out:
        pc = hc @ w_in
        ccol = lambda off: pc[..., off:off + gw]
    else:
        ccol = lambda off: hc @ w_in[:, off:off + gw]
    xcol = lambda off: px[..., off:off + gw]

    qa = rms_norm(split_heads(xcol(OFF_NA_Q), NA_HEADS), p['na_qg'])
    ka = rms_norm(split_heads(xcol(OFF_NA_K), NA_HEADS), p['na_kg'])
    va = split_heads(xcol(OFF_NA_V), NA_HEADS)
    kca = rms_norm(split_heads(ccol(OFF_NA_K), NA_HEADS), p['na_kg'])
    vca = split_heads(ccol(OFF_NA_V), NA_HEADS)
    ya = merge_heads(neighbourhood_attention(qa, ka, va, kca, vca, p['na_rpb']))

    lam_init = 0.8 - 0.6 * math.exp(-0.3 * layer_idx)
    lam = (jnp.exp(jnp.sum(p['lq1'].astype(jnp.float32) * p['lk1'].astype(jnp.float32)))
           - jnp.exp(jnp.sum(p['lq2'].astype(jnp.float32) * p['lk2'].astype(jnp.float32))) + lam_init)
    qd = axial_rope(rms_norm(diff_split(xcol(OFF_DF_Q)), p['df_qg']))
    kd = axial_rope(rms_norm(diff_split(xcol(OFF_DF_K)), p['df_kg']))
    vd = split_heads(xcol(OFF_DF_V), DIFF_HEADS)
    kcd = rms_norm(diff_split(ccol(OFF_DF_K)), p['df_kg'])
    vcd = split_heads(ccol(OFF_DF_V), DIFF_HEADS)
    keys = jnp.concatenate([kcd, kd], axis=3)
    vals = jnp.concatenate([vcd, vd], axis=2)
    nb = l // Q_BLOCK
    q_blocks = jnp.moveaxis(qd.reshape(b, DIFF_HEADS, 2, nb, Q_BLOCK, DIFF_QK_DIM), 3, 0)
    yd = lax.map(lambda qb: differential_attention(qb, keys, vals, lam), q_blocks)
    yd = jnp.moveaxis(yd, 0, 2).reshape(b, DIFF_HEADS, l, HEAD_DIM)
    yd = merge_heads(rms_norm(yd, p['df_subln']) * (1 - lam_init))

    yb = multiscale_pool(xcol(OFF_POOL), p['pool_w'], p['pool_scale'])
    yf = fourier_mix(xcol(OFF_FFT), p['fft_w'])
    x = x + g1 * (jnp.concatenate([ya, yd, yb, yf], axis=-1) @ p['w_out'])

    if ctx_out:
        qca = rms_norm(split_heads(ccol(OFF_NA_Q), NA_HEADS), p['na_qg'])
        yca = merge_heads(context_attention(qca, kca, vca))
        qcd = rms_norm(diff_split(ccol(OFF_DF_Q)), p['df_qg'])
        ycd = merge_heads(rms_norm(differential_attention(qcd, kcd, vcd, lam), p['df_subln']) * (1 - lam_init))
        ycb = multiscale_pool(ccol(OFF_POOL), p['pool_w'], p['pool_scale'])
        ycf = fourier_mix(ccol(OFF_FFT), p['fft_w'])
        cx = cx + cg1 * (jnp.concatenate([yca, ycd, ycb, ycf], axis=-1) @ p['w_out'])

    hx2 = modulate(x, p['g2'], sh2, sc2).reshape(b * l, dm)
    moe_args = (p['router_w'], p['router_b'], p['w1'], p['b1'], p['w2'], p['b2'])
    if ctx_out:
        hc2 = modulate(cx, p['g2'], ch2, cs2).reshape(b * lc, dm)
        y = expert_ffn(jnp.concatenate([hx2, hc2], axis=0), *moe_args)
        x = x + g2 * y[:b * l].reshape(b, l, dm)
        cx = cx + cg2 * y[b * l:].reshape(b, lc, dm)
    else:
        x = x + g2 * expert_ffn(hx2, *moe_args).reshape(b, l, dm)
        cx = None
    return x, cx


def setup_inputs(seed: int = 0) -> dict:
    key = jax.random.key(seed)
    ks = jax.random.split(key, 32)
    f32 = jnp.float32
    nrm = lambda k, shape, s: jax.random.normal(k, shape, f32) * s
    L, D, E, F = DEPTH, D_MODEL, N_EXPERTS, D_FF
    return {
        'x': nrm(ks[0], (BATCH, SEQ, D), 1.0),
        'c': nrm(ks[1], (BATCH, D), 1.0),
        'ctx': nrm(ks[2], (BATCH, CTX_LEN, D), 1.0),
        'c_ctx': nrm(ks[3], (D,), 1.0),
        'w_ada': nrm(ks[4], (L, D, 6 * D), 0.5 * D ** -0.5),
        'b_ada': nrm(ks[5], (L, 6 * D), 0.02),
        'g_norm1': 1.0 + nrm(ks[6], (L, D), 0.05),
        'g_norm2': 1.0 + nrm(ks[7], (L, D), 0.05),
        'w_in': nrm(ks[8], (L, D, IN_WIDTH), D ** -0.5),
        'w_out': nrm(ks[9], (L, MIX_WIDTH, D), MIX_WIDTH ** -0.5),
        'na_q_gain': 1.0 + nrm(ks[10], (L, HEAD_DIM), 0.05),
        'na_k_gain': 1.0 + nrm(ks[11], (L, HEAD_DIM), 0.05),
        'na_rpb': nrm(ks[12], (L, NA_HEADS, 2 * NA_ROWS - 1, 2 * NA_COLS - 1), 0.5),
        'diff_q_gain': 1.0 + nrm(ks[13], (L, DIFF_QK_DIM), 0.05),
        'diff_k_gain': 1.0 + nrm(ks[14], (L, DIFF_QK_DIM), 0.05),
        'diff_lambda_q1': nrm(ks[15], (L, DIFF_QK_DIM), 0.1),
        'diff_lambda_k1': nrm(ks[16], (L, DIFF_QK_DIM), 0.1),
        'diff_lambda_q2': nrm(ks[17], (L, DIFF_QK_DIM), 0.1),
        'diff_lambda_k2': nrm(ks[18], (L, DIFF_QK_DIM), 0.1),
        'diff_subln': 1.0 + nrm(ks[19], (L, HEAD_DIM), 0.05),
        'pool_w': nrm(ks[20], (L, len(POOL_WINDOWS), POOL_GROUP, POOL_GROUP), POOL_GROUP ** -0.5),
        'pool_scale': 1.0 + nrm(ks[21], (L, GROUP_WIDTH), 0.1),
        'fft_w': nrm(ks[22], (L, FFT_GROUPS, FFT_GROUP, FFT_GROUP), FFT_GROUP ** -0.5),
        'router_w': nrm(ks[23], (L, D, E), D ** -0.5),
        'router_b': nrm(ks[24], (L, E), 0.01),
        'moe_w1': nrm(ks[25], (L, E, D, 2 * F), D ** -0.5),
        'moe_b1': nrm(ks[26], (L, E, 2 * F), 0.01),
        'moe_w2': nrm(ks[27], (L, E, F, D), F ** -0.5),
        'moe_b2': nrm(ks[28], (L, E, D), 0.01),
    }


def reference(x, c, ctx, c_ctx, w_ada, b_ada, g_norm1, g_norm2, w_in, w_out, na_q_gain, na_k_gain,
              na_rpb, diff_q_gain, diff_k_gain, diff_lambda_q1, diff_lambda_k1, diff_lambda_q2,
              diff_lambda_k2, diff_subln, pool_w, pool_scale, fft_w, router_w, router_b,
              moe_w1, moe_b1, moe_w2, moe_b2):
    c_act = jax.nn.silu(c)
    cctx_act = jax.nn.silu(c_ctx)
    cx = ctx
    for i in range(DEPTH):
        p = {
            'w_ada': w_ada[i], 'b_ada': b_ada[i], 'g1': g_norm1[i], 'g2': g_norm2[i],
            'w_in': w_in[i], 'w_out': w_out[i],
            'na_qg': na_q_gain[i], 'na_kg': na_k_gain[i], 'na_rpb': na_rpb[i],
            'df_qg': diff_q_gain[i], 'df_kg': diff_k_gain[i],
            'lq1': diff_lambda_q1[i], 'lk1': diff_lambda_k1[i],
            'lq2': diff_lambda_q2[i], 'lk2': diff_lambda_k2[i], 'df_subln': diff_subln[i],
            'pool_w': pool_w[i], 'pool_scale': pool_scale[i], 'fft_w': fft_w[i],
            'router_w': router_w[i], 'router_b': router_b[i],
            'w1': moe_w1[i], 'b1': moe_b1[i], 'w2': moe_w2[i], 'b2': moe_b2[i],
        }
        x, cx = hybrid_layer(x, cx, c_act, cctx_act, p, i, i < DEPTH - 1)
    return x
```

```python
import math
import numpy as np
import ml_dtypes
import concourse.bass as bass
import concourse.mybir as mybir
from concourse.bass_utils import run_bass_kernel_spmd

F32 = mybir.dt.float32
BF16 = mybir.dt.bfloat16
ALU = mybir.AluOpType
AF = mybir.ActivationFunctionType
ENGS = ("pe", "act", "dve", "pool", "sp")

D = 1024
L = 2048
LC = 256
NE = 32
ALPHA = 1.702
EPS = 1e-6
MASKV = -30000.0


class Op:
    __slots__ = ("eng", "fn", "reads", "writes", "dma", "waits", "signal", "sig_idx", "dma_val", "deps")

    def __init__(self, eng, fn, reads, writes, dma):
        self.eng = eng
        self.fn = fn
        self.reads = reads
        self.writes = writes
        self.dma = dma
        self.waits = []
        self.signal = False
        self.sig_idx = 0
        self.dma_val = 0
        self.deps = None


class Prog:
    def __init__(self):
        self.ops = []
        self.last_w = {}
        self.readers = {}
        self.dma_counts = {}
        self.group_streams = set()
        self.pending_barrier = {}

    def op(self, eng, fn, reads=(), writes=(), dma=None):
        o = Op(eng, fn, tuple(reads), tuple(writes), dma)
        idx = len(self.ops)
        deps = set()
        for k in o.reads:
            w = self.last_w.get(k)
            if w is not None:
                deps.add(w)
        for k in o.writes:
            w = self.last_w.get(k)
            if w is not None:
                deps.add(w)
            rs = self.readers.get(k)
            if rs:
                deps.update(rs)
        for k in o.reads:
            self.readers.setdefault(k, []).append(idx)
        for k in o.writes:
            self.last_w[k] = idx
            self.readers[k] = []
        if eng in self.pending_barrier:
            deps.update(self.pending_barrier.pop(eng))
        if dma is not None:
            self.dma_counts[dma] = self.dma_counts.get(dma, 0) + 16
            o.dma_val = self.dma_counts[dma]
        o.deps = deps
        self.ops.append(o)
        return idx

    def barrier(self):
        last = {}
        for i, o in enumerate(self.ops):
            last[o.eng] = i
        lastd = {}
        for i, o in enumerate(self.ops):
            if o.dma is not None:
                lastd[o.dma] = i
        s = set(last.values()) | set(lastd.values())
        for e in ENGS:
            self.pending_barrier[e] = set(s) | self.pending_barrier.get(e, set())

    def finalize(self):
        need = {}
        for ci, c in enumerate(self.ops):
            for pi in c.deps:
                p = self.ops[pi]
                if p.dma is None:
                    if p.eng == c.eng and p.eng in ("pe", "sp"):
                        continue
                    p.signal = True
                need.setdefault(ci, []).append(pi)
        cnt = {e: 0 for e in ENGS}
        for o in self.ops:
            if o.signal:
                cnt[o.eng] += 1
                o.sig_idx = cnt[o.eng]
        waited = {e: {} for e in ENGS}
        for ci, c in enumerate(self.ops):
            ws = {}
            for pi in need.get(ci, ()):
                p = self.ops[pi]
                if p.dma is not None:
                    key = ("dma", p.dma)
                    val = self.dma_counts[p.dma] if p.dma in self.group_streams else p.dma_val
                else:
                    key, val = ("eng", p.eng), p.sig_idx
                if ws.get(key, 0) < val:
                    ws[key] = val
            wd = waited[c.eng]
            for key, val in ws.items():
                if wd.get(key, 0) >= val:
                    continue
                wd[key] = val
                c.waits.append((key, val))

    def emit(self, nc, final_waits=()):
        import contextlib
        self.finalize()
        with contextlib.ExitStack() as es:
            sems = {}
            for e in ENGS:
                sems[("eng", e)] = es.enter_context(nc.semaphore("s_" + e))
            for d in self.dma_counts:
                sems[("dma", d)] = es.enter_context(nc.semaphore("d_" + d))
            block = es.enter_context(nc.Block())
            ops = self.ops
            counts = self.dma_counts

            def body(engname):
                def run(eng):
                    for o in ops:
                        if o.eng != engname:
                            continue
                        for key, val in o.waits:
                            eng.wait_ge(sems[key], val)
                        ins = o.fn(eng)
                        if o.dma is not None:
                            ins.then_inc(sems[("dma", o.dma)], 16)
                        elif o.signal:
                            ins.then_inc(sems[("eng", engname)], 1)
                    if engname == "sp":
                        for d in final_waits:
                            eng.wait_ge(sems[("dma", d)], counts[d])
                return run

            block.tensor(body("pe"))
            block.scalar(body("act"))
            block.vector(body("dve"))
            block.gpsimd(body("pool"))
            block.sync(body("sp"))


def _bf(a):
    return np.ascontiguousarray(a.astype(ml_dtypes.bfloat16))


CB_OFF = {}
CF_OFF = {}
PF_OFF = {}


def _layout(offs, items):
    o = 0
    for name, n in items:
        offs[name] = (o, n)
        o += n
    return o


NCB = _layout(CB_OFF, [("ident", 128), ("bd64", 128), ("bd32", 128), ("perm", 128)])
NCF = _layout(CF_OFF, [("identf", 128), ("onesf", 128), ("eps", 1), ("mask12", 2), ("seven", 1)])
NPF = _layout(PF_OFF, [("cvec", 16), ("bada", 96), ("g1", 16), ("g2", 16), ("naq", 2), ("nak", 2), ("dfq", 2),
                       ("dfk", 2), ("lam", 256), ("subln", 128), ("pscale", 4), ("rb", 64)])

_CONST_CACHE = {}


def host_constants():
    if _CONST_CACHE:
        return _CONST_CACHE
    p = np.arange(128)
    cb = np.zeros((128, NCB), np.float32)
    cb[:, 0:128] = np.eye(128)
    cb[:, 128:256] = (p[:, None] // 64 == p[None, :] // 64) / 64.0
    cb[:, 256:384] = (p[:, None] // 32 == p[None, :] // 32) / 32.0
    partner = np.where((p % 16) < 8, p + 8, p - 8)
    perm = np.zeros((128, 128), np.float32)
    perm[partner, p] = 1.0
    cb[:, 384:512] = perm
    lt = np.arange(2)[None, :, None]
    lin = p[:, None, None]
    k = np.arange(256)[None, None, :]
    ang = 2 * np.pi * ((lt * 128 + lin) * k % 256) / 256.0
    t256 = np.zeros((128, 1024), np.float32)
    t256[:, 0:512] = (np.cos(ang) / 16.0).reshape(128, 512)
    t256[:, 512:1024] = (-np.sin(ang) / 16.0).reshape(128, 512)
    cf = np.zeros((128, NCF), np.float32)
    cf[:, 0:128] = np.eye(128)
    cf[:, 128:256] = 1.0
    cf[:, 256] = EPS
    cf[:, 257] = (p % 64 < 32)
    cf[:, 258] = (p % 64 >= 32)
    m = np.arange(64)[:, None]
    c = np.arange(64)[None, :]
    a64 = 2 * np.pi * (m * c % 64) / 64.0
    cs = np.zeros((64, 2, 2, 128), np.float32)
    cs[:, 0, 0, 0:64] = np.cos(a64) / 8.0
    cs[:, 0, 1, 64:128] = np.cos(a64) / 8.0
    cs[:, 1, 0, 0:64] = np.sin(a64) / 8.0
    cs[:, 1, 1, 64:128] = np.sin(a64) / 8.0
    cf[:, 259] = 7.0
    d = p % 32
    seg = d // 16
    i = d % 16
    j = i % 8
    inv = 10000.0 ** (-(2.0 * j) / 16.0)
    t = np.arange(L)
    pos = np.where(seg[:, None] == 0, (t // 64)[None, :], (t % 64)[None, :]).astype(np.float64)
    angr = pos * inv[:, None]
    rope = np.zeros((128, 2, L), np.float32)
    rope[:, 0, :] = np.cos(angr)
    rope[:, 1, :] = np.where((i < 8)[:, None], -np.sin(angr), np.sin(angr))
    pband = np.zeros((128, 4, 5, 128), np.float32)
    Lp = 512
    posp = np.arange(Lp)
    for g, win in enumerate((2, 4, 8, 16)):
        lo = np.clip(posp - win // 2, 0, Lp)
        hi = np.clip(posp - win // 2 + win, 0, Lp)
        M = np.zeros((Lp, Lp), np.float64)
        for o in range(Lp):
            M[o, lo[o]:hi[o]] = 1.0 / (hi[o] - lo[o])
        M -= np.eye(Lp)
        pband[:, g, 0, :] = M[128:256, 0:128].T
        pband[:, g, 1, :] = M[128:256, 128:256].T
        pband[:, g, 2, :] = M[128:256, 256:384].T
        pband[:, g, 3, :] = M[0:128, 0:128].T
        pband[:, g, 4, :] = M[384:512, 384:512].T
    kb = np.arange(16)[:, None, None, None]
    lin4 = np.arange(128)[None, :, None, None]
    lt4 = np.arange(16)[None, None, :, None]
    kk = np.arange(128)[None, None, None, :]
    prod = ((lt4 * 128 + lin4) * (kb * 128 + kk)) % L
    angL = 2 * np.pi * prod / float(L)
    s = 1.0 / math.sqrt(L)
    cl = (np.cos(angL) * s).reshape(16, 128, 2048)
    sl = (-np.sin(angL) * s).reshape(16, 128, 2048)
    _CONST_CACHE.update(dict(cb=_bf(cb), cf=cf, t256=_bf(t256), cs64p=np.ascontiguousarray(cs.reshape(64, 512)), rope=_bf(rope.reshape(128, 2 * L)),
                             pband=_bf(pband.reshape(128, 4 * 5 * 128)), cl=_bf(cl), sl=_bf(sl)))
    return _CONST_CACHE


def host_layouts(inp, nlayers=2):
    f = lambda a: np.asarray(a, np.float32)
    out = {}
    p = np.arange(128)
    rpb = f(inp["na_rpb"])
    ck = np.arange(64)[:, None]
    cq = np.arange(64)[None, :]
    col_start = np.clip(np.arange(64) - 8, 0, 48)
    inwin = (ck >= col_start[None, :]) & (ck < col_start[None, :] + 16)
    relc = np.clip(ck - cq, -15, 15) + 15
    nab = np.full((nlayers, 2, 64, 4, 14, 64), MASKV, np.float32)
    for l in range(nlayers):
        for h in range(4):
            for m0 in range(14):
                for jj in range(2):
                    blk = rpb[l, h, m0 + jj][relc]
                    nab[l, jj, :, h, m0, :] = np.where(inwin, blk, MASKV)
    out["nab"] = nab.reshape(nlayers, 128, 4 * 14 * 64)
    pw = f(inp["pool_w"])
    poolw = np.zeros((nlayers, 128, 4, 128), np.float32)
    fw = f(inp["fft_w"])
    fftw = np.zeros((nlayers, 64, 4, 128), np.float32)
    for l in range(nlayers):
        for g in range(4):
            o = (g % 2) * 64
            poolw[l, o:o + 64, g, o:o + 64] = pw[l, g]
            fftw[l, :, g, o:o + 64] = fw[l, g]
    out["poolw"] = poolw.reshape(nlayers, 128, 512)
    out["fftw"] = fftw.reshape(nlayers, 64, 512)
    rw = f(inp["router_w"])
    out["rw"] = np.ascontiguousarray(rw.reshape(nlayers, 8, 128, 32).transpose(0, 2, 1, 3)).reshape(nlayers, 128, 256)
    b1 = f(inp["moe_b1"])
    b1t = b1.reshape(nlayers, NE, 8, 128, 2).transpose(0, 3, 1, 2, 4)
    out["b1t"] = np.ascontiguousarray(b1t).reshape(nlayers, 128, NE * 16)
    out["b2"] = np.ascontiguousarray(f(inp["moe_b2"]))
    return out


def host_pf(inp, b, nlayers=2):
    f = lambda a: np.asarray(a, np.float32)
    p = np.arange(128)
    pf = np.zeros((128, NPF), np.float32)

    def put(name, arr):
        o, n = PF_OFF[name]
        pf[:, o:o + n] = arr.reshape(128, n)

    cv = np.zeros((128, 8, 2), np.float32)
    cv[:, :, 0] = f(inp["c"])[b].reshape(8, 128).T
    cv[:, :, 1] = f(inp["c_ctx"]).reshape(8, 128).T
    put("cvec", cv)
    put("bada", f(inp["b_ada"]).reshape(nlayers, 48, 128).transpose(2, 0, 1))
    put("g1", f(inp["g_norm1"]).reshape(nlayers, 8, 128).transpose(2, 0, 1))
    put("g2", f(inp["g_norm2"]).reshape(nlayers, 8, 128).transpose(2, 0, 1))
    put("naq", f(inp["na_q_gain"])[:, p % 64].T)
    put("nak", f(inp["na_k_gain"])[:, p % 64].T)
    put("dfq", f(inp["diff_q_gain"])[:, p % 32].T)
    put("dfk", f(inp["diff_k_gain"])[:, p % 32].T)
    lam = np.stack([f(inp["diff_lambda_q1"]), f(inp["diff_lambda_k1"]), f(inp["diff_lambda_q2"]),
                    f(inp["diff_lambda_k2"])], axis=1)
    put("lam", np.broadcast_to(lam[None], (128, nlayers, 4, 32)).copy())
    put("subln", np.broadcast_to(f(inp["diff_subln"])[None], (128, nlayers, 64)).copy())
    put("pscale", f(inp["pool_scale"]).reshape(nlayers, 2, 128).transpose(2, 0, 1))
    put("rb", np.broadcast_to(f(inp["router_b"])[None], (128, nlayers, 32)).copy())
    return pf


def build_nc(nlayers=2, do_moe=True, n_exp=NE, stop=None, mixers=(0, 1, 2, 3), ne_decl=NE):
    nc = bass.Bass("TRN2", target_bir_lowering=False)
    P = Prog()
    P.group_streams = {"const", "xin"}

    def din(name, shape, dt=F32):
        return nc.dram_tensor(name, list(shape), dt, kind="ExternalInput").ap()

    x_d = din("x", [L, D])
    cx_d = din("cx", [LC, D])
    pf_d = din("pf", [128, NPF])
    cb_d = din("cb", [128, NCB], BF16)
    cf_d = din("cf", [128, NCF])
    wada_d = din("wada", [nlayers, D, 6 * D])
    win_d = din("win", [nlayers, D, 2048])
    wout_d = din("wout", [nlayers, D, D])
    nab_d = din("nab", [nlayers, 128, 4 * 14 * 64])
    rope_d = din("rope", [128, 2 * L], BF16)
    pband_d = din("pband", [128, 2560], BF16)
    poolw_d = din("poolw", [nlayers, 128, 512])
    fftw_d = din("fftw", [nlayers, 64, 512])
    cl_d = din("cl", [16, 128, 2048], BF16)
    t256_d = din("t256", [128, 1024], BF16)
    cs64p_d = din("cs64p", [64, 512])
    sl_d = din("sl", [16, 128, 2048], BF16)
    rw_d = din("rw", [nlayers, 128, 256])
    b1t_d = din("b1t", [nlayers, 128, NE * 16])
    b2_d = din("b2", [nlayers, NE, D])
    w1_d = din("w1", [nlayers, ne_decl, D, 2 * D])
    w2_d = din("w2", [nlayers, ne_decl, D, D])
    out_d = nc.dram_tensor("out", [L, D], F32, kind="ExternalOutput").ap()

    TOTAL = 212000
    ALL = nc.alloc_sbuf_tensor("allsb", [128, TOTAL // 2], BF16)
    OFF_X = 0
    OFF_HT = 73728
    OFF_B = OFF_HT + 36864
    OFF_RING = OFF_B + 36864
    OFF_MISC = OFF_RING + 16384
    MISC_SZ = 7168
    OFF_S = OFF_MISC + MISC_SZ
    S_SZ = TOTAL - OFF_S

    def carve(off, shape, dt, parts=128):
        n = 1
        for s_ in shape:
            n *= s_
        assert off % 4 == 0
        if dt == F32:
            ap = ALL[0:parts, off // 2: off // 2 + 2 * n].bitcast(F32)
        else:
            ap = ALL[0:parts, off // 2: off // 2 + n]
        if len(shape) == 2:
            ap = ap.rearrange("p (a b) -> p a b", a=shape[0])
        elif len(shape) == 3:
            ap = ap.rearrange("p (a b c) -> p a b c", a=shape[0], b=shape[1])
        elif len(shape) == 4:
            ap = ap.rearrange("p (a b c d) -> p a b c d", a=shape[0], b=shape[1], c=shape[2])
        return ap

    def nbytes(shape, dt):
        n = 4 if dt == F32 else 2
        for s_ in shape:
            n *= s_
        return (n + 31) // 32 * 32

    class Bump:
        def __init__(self, regions):
            self.regions = regions
            self.cur = [r[0] for r in regions]

        def get(self, shape, dt, parts=128):
            nb = nbytes(shape, dt)
            for i, (o, sz) in enumerate(self.regions):
                if self.cur[i] + nb <= o + sz:
                    a = carve(self.cur[i], shape, dt, parts)
                    self.cur[i] += nb
                    return a
            raise RuntimeError("scratch overflow %s" % (shape,))

        def mark(self):
            return list(self.cur)

        def reset(self, m):
            self.cur = list(m)

    X = carve(OFF_X, [18, D], F32)
    HT = carve(OFF_HT, [8, 2304], BF16)
    YG = carve(OFF_B, [2, 2304], BF16)
    ACTB = carve(OFF_B, [8, 2304], BF16)
    misc = Bump([(OFF_MISC, MISC_SZ)])
    CB = misc.get([NCB], BF16)
    CF = misc.get([NCF], F32)
    PF = misc.get([NPF], F32)
    MOD = misc.get([nlayers, 48, 2], F32)
    CS = misc.get([8, 2], F32)
    GS = misc.get([8, 2], F32)
    SS = misc.get([18], F32)
    RSTD = misc.get([18], F32)
    NLAM = misc.get([2], F32)
    SM = misc.get([16], F32)

    def cbv(name):
        o, n = CB_OFF[name]
        return CB[:, o:o + n]

    def cfv(name, parts=128):
        o, n = CF_OFF[name]
        return CF[0:parts, o:o + n]

    def pfv(name):
        o, n = PF_OFF[name]
        return PF[:, o:o + n]

    IDB, BD64, BD32, PERM = cbv("ident"), cbv("bd64"), cbv("bd32"), cbv("perm")
    IDF, ONESF, EPSC = cfv("identf"), cfv("onesf"), cfv("eps")
    MASK12 = cfv("mask12")
    CVEC = pfv("cvec").rearrange("p (a b) -> p a b", a=8)
    BADA = pfv("bada").rearrange("p (a b) -> p a b", a=nlayers)
    G1T = pfv("g1").rearrange("p (a b) -> p a b", a=nlayers)
    G2T = pfv("g2").rearrange("p (a b) -> p a b", a=nlayers)
    LAMV = pfv("lam").rearrange("p (a b c) -> p a b c", a=nlayers, b=4)
    SUBLN = pfv("subln").rearrange("p (a b) -> p a b", a=nlayers)
    PSCALE = pfv("pscale").rearrange("p (a b) -> p a b", a=nlayers)
    RB = pfv("rb").rearrange("p (a b) -> p a b", a=nlayers)

    PS = [nc.alloc_psum_tensor("ps%d" % i, [128, 512], F32) for i in range(8)]
    psc = {"a": 0, "b": 0, "c": 0}
    psr = {"a": (0, 4), "b": (4, 2), "c": (6, 2)}

    def psum(role):
        base, n = psr[role]
        i = base + psc[role] % n
        psc[role] += 1
        return PS[i], ("ps", i)

    def MM(out, lhsT, rhs, start, stop, r, w, skip=False):
        if skip:
            P.op("pe", lambda e: e.matmul(out, lhsT=lhsT, rhs=rhs, start=start, stop=stop, skip_group_check=True), r, w)
        else:
            P.op("pe", lambda e: e.matmul(out, lhsT=lhsT, rhs=rhs, start=start, stop=stop), r, w)

    def TR(out, in_, ident, r, w):
        P.op("pe", lambda e: e.transpose(out=out, in_=in_, identity=ident), r, w)

    def ACT(out, in_, func, r, w, bias=None, scale=None, accum=None):
        kw = {}
        if bias is not None:
            kw["bias"] = bias
        if scale is not None:
            kw["scale"] = scale
        if accum is not None:
            kw["accum_out"] = accum
        P.op("act", lambda e: e.activation(out=out, in_=in_, func=func, **kw), r, w)

    def TS(out, in0, s1, s2, op0, op1, r, w, eng="dve"):
        if op1 is None:
            P.op(eng, lambda e: e.tensor_scalar(out=out, in0=in0, scalar1=s1, scalar2=None, op0=op0), r, w)
        else:
            P.op(eng, lambda e: e.tensor_scalar(out=out, in0=in0, scalar1=s1, scalar2=s2, op0=op0, op1=op1), r, w)

    def TT(out, in0, in1, op, r, w, eng="dve"):
        P.op(eng, lambda e: e.tensor_tensor(out=out, in0=in0, in1=in1, op=op), r, w)

    def STT(out, in0, scalar, in1, op0, op1, r, w, eng="dve"):
        P.op(eng, lambda e: e.scalar_tensor_tensor(out=out, in0=in0, scalar=scalar, in1=in1, op0=op0, op1=op1), r, w)

    def CPY(out, in_, r, w, eng="dve"):
        if eng == "act":
            P.op("act", lambda e: e.copy(out=out, in_=in_), r, w)
        else:
            P.op(eng, lambda e: e.tensor_copy(out=out, in_=in_), r, w)

    def RECIP(out, in_, r, w):
        P.op("dve", lambda e: e.reciprocal(out=out, in_=in_), r, w)

    def MEMSET(out, val, w, eng="pool"):
        P.op(eng, lambda e: e.memset(out, val), (), w)

    def DMA(eng, out, in_, stream, r, w):
        P.op(eng, lambda e: e.dma_start(out=out, in_=in_), r, w, dma=stream)

    ring_n = [0]

    def ring_load(shape, dt, dram_ap, parts=128):
        s = ring_n[0] % 4
        ring_n[0] += 1
        v = carve(OFF_RING + 4096 * s, shape, dt, parts)
        DMA("pool", v, dram_ap, "ring%d" % s, (), [("ring", s)])
        return v, ("ring", s)

    def run_units(units, depth=3):
        loaded = []
        for i in range(len(units)):
            while len(loaded) < min(len(units), i + depth):
                loaded.append(units[len(loaded)][0]())
            units[i][1](*loaded[i])

    def run_pipe(its, d=2):
        n = len(its)
        for i in range(n + d):
            if i < n:
                its[i][0]()
            if i >= d:
                its[i - d][1]()

    def tkeys(name, c, t0, n):
        return [(name, c, t) for t in range(t0 // 128, (t0 + n + 127) // 128)]

    DMA("sp", CB, cb_d, "const", (), ["CB"])
    DMA("sp", CF, cf_d, "const", (), ["CF"])
    DMA("sp", PF, pf_d, "const", (), ["PF"])
    for j in range(16):
        DMA("sp", X[:, j, :], x_d[j * 128:(j + 1) * 128, :], "xin", (), [("X", j)])
    for j in range(2):
        DMA("sp", X[:, 16 + j, :], cx_d[j * 128:(j + 1) * 128, :], "xin", (), [("X", 16 + j)])
    CONSTS = ["CB", "CF", "PF"]
    ACT(CS, CVEC, AF.Silu, CONSTS, ["CS"])
    sA = Bump([(OFF_HT, 36864)])
    WA = [sA.get([8, 256], F32) for _ in range(2)]
    for l in range(nlayers):
        wv = wada_d[l].rearrange("(k p) n -> p k n", p=128)
        for u in range(24):
            b_ = u % 2
            DMA("sp", WA[b_], wv[:, :, u * 256:(u + 1) * 256], "wa%d" % b_, (), [("WA", b_)])
            for jj in range(2):
                j = u * 2 + jj
                ps, pk = psum("c")
                for k in range(8):
                    MM(ps[:, 0:2], WA[b_][:, k, jj * 128:(jj + 1) * 128], CS[:, k, :], k == 0, k == 7,
                       [("WA", b_), "CS"], [pk])
                TS(MOD[:, l, j, :], ps[:, 0:2], BADA[:, l, j:j + 1], None, ALU.add, None, [pk, "PF"], [("MOD", l)])
    P.barrier()

    def mod_ap(l, which, c, src):
        return MOD[:, l, which * 8 + c, src:src + 1]

    def bcast_vec(dst, l, which, src, tmp):
        for c in range(8):
            TS(tmp, IDF, mod_ap(l, which, c, src), None, ALU.mult, None, ["CF", ("MOD", l)], ["bctmp"])
            ps, pk = psum("c")
            MM(ps[:, 0:128], ONESF, tmp, True, True, ["CF", "bctmp"], [pk])
            CPY(dst[:, c * 128:(c + 1) * 128], ps[:, 0:128], [pk], ["GB"], eng="act")

    def norm_phase(l, which0, gT, tiles, scr, router=None, pre_router=None):
        XNs = [scr.get([D], F32) for _ in range(2)]
        JUNK = scr.get([D], BF16)
        for src in range(2):
            TS(GS[:, :, src], MOD[:, l, (which0 + 1) * 8:(which0 + 2) * 8, src], 1.0, None, ALU.add, None,
               [("MOD", l)], ["GS"])
            TT(GS[:, :, src], GS[:, :, src], gT[:, l, :], ALU.mult, ["GS", "PF"], ["GS"])
        if pre_router is not None:
            pre_router()
        MEMSET(SS, 0.0, [("SS", j) for j in range(18)], eng="dve")
        for j in tiles:
            src = 0 if j < 16 else 1
            XN = XNs[j % 2]
            xnk = ("XN", j % 2)
            xk = [("X", j)] + [("X", j, q) for q in range(4)]
            ACT(JUNK, X[:, j, :], AF.Square, xk, [("SS", j)], accum=SS[:, j:j + 1])
            ACT(RSTD[:, j:j + 1], SS[:, j:j + 1], AF.Sqrt, [("SS", j), "CF"], [("RSTD", j)], bias=EPSC, scale=1.0 / D)
            RECIP(RSTD[:, j:j + 1], RSTD[:, j:j + 1], [("RSTD", j)], [("RSTD", j)])
            TS(XN, X[:, j, :], RSTD[:, j:j + 1], None, ALU.mult, None, xk + [("RSTD", j)], [xnk])
            pss = [psum("a"), psum("a")]
            for c in range(8):
                ps, pk = pss[c // 4]
                TR(ps[:, (c % 4) * 128:(c % 4 + 1) * 128], XN[:, c * 128:(c + 1) * 128], IDF, [xnk, "CF"], [pk])
            if router is not None:
                router(j, src, pss)
            for c in range(8):
                ps, pk = pss[c // 4]
                ACT(HT[:, c, j * 128:(j + 1) * 128], ps[:, (c % 4) * 128:(c % 4 + 1) * 128], AF.Identity,
                    [pk, "GS", ("MOD", l)], [("HT", c, j)], bias=mod_ap(l, which0, c, src), scale=GS[:, c, src:src + 1])

    def proj_units(l, col0, ncols):
        wv = win_d[l].rearrange("(k p) n -> p k n", p=128)
        return [(lambda c0=c0: ring_load([8, 256], BF16, wv[:, :, c0:c0 + 256])) for c0 in range(col0, col0 + ncols, 256)]

    def tok_blocks(ntok):
        return [(t0, min(512, ntok - t0)) for t0 in range(0, ntok, 512)]

    def proj_fm(unit, ukey, ntok, evac):
        for cc in range(2):
            for (t0, n) in tok_blocks(ntok):
                ps, pk = psum("a")
                for k in range(8):
                    MM(ps[:, 0:n], unit[:, k, cc * 128:(cc + 1) * 128], HT[:, k, t0:t0 + n], k == 0, k == 7,
                       [ukey] + tkeys("HT", k, t0, n), [pk])
                evac(cc, t0, n, ps, pk)

    def proj_tm(unit, ukey, t0, evac):
        ps, pk = psum("a")
        for k in range(8):
            MM(ps[:, 0:256], HT[:, k, t0:t0 + 128], unit[:, k, :], k == 0, k == 7, [ukey] + tkeys("HT", k, t0, 128), [pk])
        evac(ps, pk)

    def wout_apply(l, g, ntiles, G1B, scr):
        TMP = [scr.get([512], F32) for _ in range(2)]
        wv = wout_d[l][g * 256:(g + 1) * 256, :].rearrange("(k p) n -> p k n", p=128)
        unit, ukey = ring_load([2, D], BF16, wv)
        n = 0
        for j in range(ntiles):
            src = 0 if j < 16 else 1
            for fb in range(2):
                ps, pk = psum("a")
                for k in range(2):
                    MM(ps[:, :], YG[:, k, j * 128:(j + 1) * 128], unit[:, k, fb * 512:(fb + 1) * 512], k == 0, k == 1,
                       [ukey, ("YG", k, j)], [pk])
                t = TMP[n % 2]
                tk = ("wtmp", n % 2)
                n += 1
                TT(t, ps[:, :], G1B[:, src, fb * 512:(fb + 1) * 512], ALU.mult, [pk, "GB"], [tk])
                TT(X[:, j, fb * 512:(fb + 1) * 512], X[:, j, fb * 512:(fb + 1) * 512], t, ALU.add, [("X", j), tk], [("X", j)],
                   eng="pool")

    def qk_norm_evac(SQ, RS, gain_ap, bd, out_fn):
        def evac(cc, t0, n, ps, pk):
            ACT(SQ[:, 0:n], ps[:, 0:n], AF.Square, [pk], ["SQ"])
            ps2, pk2 = psum("c")
            MM(ps2[:, 0:n], bd, SQ[:, 0:n], True, True, ["SQ", "CB"], [pk2])
            ACT(RS[:, 0:n], ps2[:, 0:n], AF.Sqrt, [pk2, "CF"], ["RS"], bias=EPSC, scale=1.0)
            RECIP(RS[:, 0:n], RS[:, 0:n], ["RS"], ["RS"])
            out_fn(cc, t0, n, ps, pk)
        return evac

    def mixer_na(l, ctx_out, G1B, scr):
        ntok = 2304
        QT = scr.get([2, 2304], BF16)
        KT = scr.get([2, 2304], BF16)
        VA = scr.get([18, 4, 65], BF16)
        VS = scr.get([15, 4, 65], BF16)
        DB = scr.get([4, 14, 64], BF16)
        SQ = scr.get([512], BF16)
        RS = scr.get([512], F32)
        GQ = scr.get([2], F32)
        PT = [scr.get([384], BF16) for _ in range(2)]
        PTC = scr.get([2, 256], BF16)
        ON = scr.get([128], F32)
        RZ = scr.get([2], F32)
        ONC = scr.get([128], F32)
        DMA("pool", DB, nab_d[l].rearrange("p (a b c) -> p a b c", a=4, b=14), "nab", (), ["DB"])
        MEMSET(VA[:, :, :, 64:65], 1.0, ["VA"])
        MEMSET(VS[:, :, :, 64:65], 1.0, ["VS"])
        naq = pfv("naq")
        nak = pfv("nak")
        TS(GQ[:, 0:1], naq[:, l:l + 1], 0.125, None, ALU.mult, None, ["PF"], ["GQ"])

        def q_out(cc, t0, n, ps, pk):
            STT(QT[:, cc, t0:t0 + n], ps[:, 0:n], GQ[:, 0:1], RS[:, 0:n], ALU.mult, ALU.mult, [pk, "RS", "GQ"], ["QT"])

        def k_out(cc, t0, n, ps, pk):
            STT(KT[:, cc, t0:t0 + n], ps[:, 0:n], nak[:, l:l + 1], RS[:, 0:n], ALU.mult, ALU.mult, [pk, "RS", "PF"], ["KT"])

        def v_comp(unit, ukey):
            for j in range(18):
                def ev(ps, pk, j=j):
                    CPY(VA[:, j, :, 0:64], ps[:, 0:256].rearrange("p (h d) -> p h d", h=4), [pk], ["VA"], eng="act")
                proj_tm(unit, ukey, j * 128, ev)
            for i in range(15):
                def ev(ps, pk, i=i):
                    CPY(VS[:, i, :, 0:64], ps[:, 0:256].rearrange("p (h d) -> p h d", h=4), [pk], ["VS"], eng="act")
                proj_tm(unit, ukey, 64 + i * 128, ev)

        lq, lk, lv = proj_units(l, 0, 256)[0], proj_units(l, 256, 256)[0], proj_units(l, 512, 256)[0]
        units = [
            (lq, lambda u, uk: proj_fm(u, uk, ntok if ctx_out else L, qk_norm_evac(SQ, RS, None, BD64, q_out))),
            (lk, lambda u, uk: proj_fm(u, uk, ntok, qk_norm_evac(SQ, RS, None, BD64, k_out))),
            (lv, v_comp),
        ]
        run_units(units)
        NPT = 4
        PT = PT + [scr.get([384], BF16) for _ in range(NPT - 2)]
        itc = [0]
        for hp in range(2):
            its = []
            state = {}
            for r in range(32):
                for hh in range(2):
                    def AB(r=r, hh=hh, hp=hp):
                        rs = min(max(r - 4, 0), 24)
                        m0b = 7 - (r - rs)
                        h = hp * 2 + hh
                        b0 = 64 * hh
                        ps, pk = psum("a")
                        q_ap = QT[b0:b0 + 64, hp, r * 64:(r + 1) * 64]
                        for c in range(6):
                            kt0 = (rs + 2 * c) * 64 if c < 4 else 2048 + (c - 4) * 128
                            MM(ps[:, c * 64:(c + 1) * 64], KT[b0:b0 + 64, hp, kt0:kt0 + 128], q_ap, True, True, ["KT", "QT"], [pk])
                        dv = DB[:, h, m0b:m0b + 7:2, :]
                        pv = ps[:, 0:256].rearrange("p (a b) -> p a b", a=4)
                        TT(pv, pv, dv, ALU.add, [pk, "DB"], [pk])
                        i_ = itc[0] % NPT
                        itc[0] += 1
                        ACT(PT[i_], ps[:, 0:384], AF.Exp, [pk], [("PT", i_)])
                        state[(r, hh)] = i_

                    def C(r=r, hh=hh, hp=hp):
                        rs = min(max(r - 4, 0), 24)
                        h = hp * 2 + hh
                        if hh == 0:
                            state[("po", r)] = psum("b")
                        po, pok = state[("po", r)]
                        i_ = state[(r, hh)]
                        pt, ptk = PT[i_], ("PT", i_)
                        for c in range(6):
                            if c < 4:
                                kr = rs + 2 * c
                                vap = VA[:, kr // 2, h, :] if kr % 2 == 0 else VS[:, (kr - 1) // 2, h, :]
                            else:
                                vap = VA[:, 16 + (c - 4), h, :]
                            MM(po[0:64, hh * 65:hh * 65 + 65], pt[:, c * 64:(c + 1) * 64], vap, c == 0, c == 5,
                               [ptk, "VA", "VS"], [pok])
                        if hh == 1:
                            RECIP(RZ[0:64, :], po[0:64, 0:130].rearrange("p (a b) -> p a b", a=2)[:, :, 64], [pok], ["RZ"])
                            for h2 in range(2):
                                TS(ON[0:64, h2 * 64:(h2 + 1) * 64], po[0:64, h2 * 65:h2 * 65 + 64], RZ[0:64, h2:h2 + 1], None, ALU.mult, None,
                                   [pok, "RZ"], ["ON"])
                            pt2, pk2 = psum("c")
                            TR(pt2[:, 0:64], ON[0:64, :], IDF[0:64, 0:64], ["ON", "CF"], [pk2])
                            CPY(YG[:, hp, r * 64:(r + 1) * 64], pt2[:, 0:64], [pk2], [("YG", hp, r // 2)], eng="act")
                    its.append((AB, C))
            run_pipe(its, d=2)
            if ctx_out:
                for qt in range(2):
                    po, pok = psum("b")
                    for hh in range(2):
                        h = hp * 2 + hh
                        b0 = 64 * hh
                        ps, pk = psum("a")
                        for kc in range(2):
                            MM(ps[:, kc * 128:(kc + 1) * 128], KT[b0:b0 + 64, hp, 2048 + kc * 128:2048 + (kc + 1) * 128],
                               QT[b0:b0 + 64, hp, 2048 + qt * 128:2048 + (qt + 1) * 128], True, True, ["KT", "QT"], [pk])
                        ACT(PTC[:, hh, :], ps[:, 0:256], AF.Exp, [pk], [("PTC", hh)])
                        for kc in range(2):
                            MM(po[:, hh * 65:hh * 65 + 65], PTC[:, hh, kc * 128:(kc + 1) * 128], VA[:, 16 + kc, h, :], kc == 0, kc == 1,
                               [("PTC", hh), "VA"], [pok])
                    RECIP(RZ[:, :], po[:, 0:130].rearrange("p (a b) -> p a b", a=2)[:, :, 64], [pok], ["RZ"])
                    for hh in range(2):
                        TS(ONC[:, hh * 64:(hh + 1) * 64], po[:, hh * 65:hh * 65 + 64], RZ[:, hh:hh + 1], None, ALU.mult, None,
                           [pok, "RZ"], ["ONC"])
                    pt2, pk2 = psum("c")
                    TR(pt2[:, 0:128], ONC[:, :], IDF, ["ONC", "CF"], [pk2])
                    CPY(YG[:, hp, 2048 + qt * 128:2048 + (qt + 1) * 128], pt2[:, 0:128], [pk2], [("YG", hp, 16 + qt)], eng="act")
        wout_apply(l, 0, 18 if ctx_out else 16, G1B, scr)


    def mixer_diff(l, ctx_out, G1B, scr):
        lam_init = 0.8 - 0.6 * math.exp(-0.3 * l)
        ROPE = scr.get([2, L], BF16)
        DMA("sp", ROPE, rope_d.rearrange("p (a b) -> p a b", a=2), "rope", (), ["ROPE"])
        LT = scr.get([2, 32], F32)
        for i in range(2):
            TT(LT[:, i, :], LAMV[:, l, 2 * i, :], LAMV[:, l, 2 * i + 1, :], ALU.mult, ["PF"], ["LT"])
            P.op("dve", lambda e, i=i: e.reduce_sum(out=SM[:, i:i + 1], in_=LT[:, i, :], axis=mybir.AxisListType.X), ["LT"], ["SM"])
        ACT(SM[:, 0:2], SM[:, 0:2], AF.Exp, ["SM"], ["SM"])
        TT(SM[:, 2:3], SM[:, 1:2], SM[:, 0:1], ALU.subtract, ["SM"], ["SM"])
        TS(NLAM[:, 0:1], SM[:, 2:3], -lam_init, None, ALU.add, None, ["SM"], ["NLAM"])
        SLG = scr.get([64], F32)
        TS(SLG, SUBLN[:, l, :], 1.0 - lam_init, None, ALU.mult, None, ["PF"], ["SLG"])
        GQ = scr.get([2], F32)
        dfq, dfk = pfv("dfq"), pfv("dfk")
        TS(GQ[:, 0:1], dfq[:, l:l + 1], 32.0 ** -0.5, None, ALU.mult, None, ["PF"], ["GQ"])
        SQ = scr.get([512], BF16)
        QG = scr.get([512], BF16)
        RS = scr.get([512], F32)
        A_ = scr.get([512], F32)
        B_ = scr.get([512], F32)
        Q1 = scr.get([2304], BF16)
        Q2 = scr.get([2304], BF16)
        KT = scr.get([2304], BF16)
        VA = scr.get([18, 2, 65], BF16)
        PT = [scr.get([512], BF16) for _ in range(2)]
        OO = scr.get([2, 4, 65], F32)
        RR = scr.get([2, 4], F32)
        TQ = scr.get([64], F32)
        JK = scr.get([64], F32)
        YDT = scr.get([4, 128], F32)
        OT = [scr.get([512], F32) for _ in range(2)]
        MEMSET(VA[:, :, :, 64:65], 1.0, ["VA"])
        ntq = 2304 if ctx_out else L
        mark = scr.mark()
        for hp in range(2):
            def prep(gain_ap, gkeys, outs):
                def evac(cc_unused, t0, n, ps, pk):
                    ACT(SQ[:, 0:n], ps[:, 0:n], AF.Square, [pk], ["SQ"])
                    ACT(QG[:, 0:n], ps[:, 0:n], AF.Identity, [pk] + gkeys, ["QG"], scale=gain_ap)
                    ps2, pk2 = psum("c")
                    MM(ps2[:, 0:n], BD32, SQ[:, 0:n], True, True, ["SQ", "CB"], [pk2])
                    ACT(RS[:, 0:n], ps2[:, 0:n], AF.Sqrt, [pk2, "CF"], ["RS"], bias=EPSC, scale=1.0)
                    RECIP(RS[:, 0:n], RS[:, 0:n], ["RS"], ["RS"])
                    if t0 < L:
                        ps3, pk3 = psum("c")
                        MM(ps3[:, 0:n], PERM, QG[:, 0:n], True, True, ["QG", "CB"], [pk3])
                        TT(A_[:, 0:n], QG[:, 0:n], ROPE[:, 0, t0:t0 + n], ALU.mult, ["QG", "ROPE"], ["A"])
                        TT(B_[:, 0:n], ps3[:, 0:n], ROPE[:, 1, t0:t0 + n], ALU.mult, [pk3, "ROPE"], ["B"])
                        TT(A_[:, 0:n], A_[:, 0:n], B_[:, 0:n], ALU.add, ["A", "B"], ["A"], eng="pool")
                        src_ap = A_
                        sk = "A"
                    else:
                        src_ap = QG
                        sk = "QG"
                    for (dst, dk, mk) in outs:
                        if mk is None:
                            TT(dst[:, t0:t0 + n], src_ap[:, 0:n], RS[:, 0:n], ALU.mult, [sk, "RS"], [dk])
                        else:
                            STT(dst[:, t0:t0 + n], src_ap[:, 0:n], mk, RS[:, 0:n], ALU.mult, ALU.mult, [sk, "RS", "CF"], [dk])
                return evac

            def one_chunk(unit, ukey, ntok, evac, cc):
                for (t0, n) in tok_blocks(ntok):
                    ps, pk = psum("a")
                    for k in range(8):
                        MM(ps[:, 0:n], unit[:, k, cc * 128:(cc + 1) * 128], HT[:, k, t0:t0 + n], k == 0, k == 7,
                           [ukey] + tkeys("HT", k, t0, n), [pk])
                    evac(cc, t0, n, ps, pk)

            def v_comp(unit, ukey, hp=hp):
                for j in range(18):
                    def ev(ps, pk, j=j):
                        CPY(VA[:, j, :, 0:64], ps[:, hp * 128:(hp + 1) * 128].rearrange("p (h d) -> p h d", h=2), [pk], ["VA"], eng="act")
                    proj_tm(unit, ukey, j * 128, ev)

            units = [
                (proj_units(l, 768, 256)[0], lambda u, uk, hp=hp: one_chunk(u, uk, ntq, prep(GQ[:, 0:1], ["GQ"], [(Q1, "Q1", MASK12[:, 0:1]), (Q2, "Q2", MASK12[:, 1:2])]), hp)),
                (proj_units(l, 1024, 256)[0], lambda u, uk, hp=hp: one_chunk(u, uk, 2304, prep(dfk[:, l:l + 1], ["PF"], [(KT, "KT", None)]), hp)),
                (proj_units(l, 1280, 256)[0], v_comp),
            ]
            run_units(units)
            NPT = 4
            if hp == 0:
                PT = PT + [scr.get([512], BF16) for _ in range(NPT - 2)]
            itc = [0]
            state = {}
            its = []
            qblocks = [(qb * 512, 512, list(range(16, 18)) + list(range(16))) for qb in range(4)]
            if ctx_out:
                qblocks.append((2048, 256, [16, 17]))
            for (q0, qn, kcs) in qblocks:
                nqt = qn // 128
                for hh in range(2):
                    for sub in range(2):
                        for ci, kc in enumerate(kcs):
                            def AB(q0=q0, qn=qn, hh=hh, sub=sub, kc=kc, ci=ci):
                                b0 = 64 * hh
                                Qs, qk_ = (Q1, "Q1") if sub == 0 else (Q2, "Q2")
                                ps, pk = psum("a")
                                MM(ps[:, 0:qn], KT[b0:b0 + 64, kc * 128:(kc + 1) * 128], Qs[b0:b0 + 64, q0:q0 + qn], True, True, ["KT", qk_], [pk])
                                i_ = itc[0] % NPT
                                itc[0] += 1
                                ACT(PT[i_][:, 0:qn], ps[:, 0:qn], AF.Exp, [pk], [("PT", i_)])
                                state[(q0, hh, sub, ci)] = i_

                            def C(q0=q0, qn=qn, nqt=nqt, hh=hh, sub=sub, kc=kc, ci=ci, nk=len(kcs), hp=hp):
                                if ci == 0:
                                    state[("po", q0, hh, sub)] = psum("b")
                                po, pok = state[("po", q0, hh, sub)]
                                i_ = state[(q0, hh, sub, ci)]
                                pt, ptk = PT[i_], ("PT", i_)
                                for qt in range(nqt):
                                    MM(po[:, qt * 65:qt * 65 + 65], pt[:, qt * 128:(qt + 1) * 128], VA[:, kc, hh, :], ci == 0 and qt == 0,
                                       ci == nk - 1, [ptk, "VA"], [pok], skip=True)
                                if ci != nk - 1:
                                    return
                                CPY(OO[:, sub, 0:nqt, :], po[:, 0:nqt * 65].rearrange("p (a b) -> p a b", a=nqt), [pok], [("OO", sub)], eng="act")
                                if sub != 1:
                                    return
                                RECIP(RR[:, :, 0:nqt], OO[:, :, 0:nqt, 64], [("OO", 0), ("OO", 1)], ["RR"])
                                TS(RR[:, 1, 0:nqt], RR[:, 1, 0:nqt], NLAM[:, 0:1], None, ALU.mult, None, ["RR", "NLAM"], ["RR"])
                                for qt in range(nqt):
                                    TS(TQ, OO[:, 0, qt, 0:64], RR[:, 0, qt:qt + 1], None, ALU.mult, None, [("OO", 0), "RR"], ["TQ"])
                                    STT(TQ, OO[:, 1, qt, 0:64], RR[:, 1, qt:qt + 1], TQ, ALU.mult, ALU.add, [("OO", 1), "RR", "TQ"], ["TQ"])
                                    MEMSET(SM[:, 4:5], 0.0, ["SM4"], eng="dve")
                                    ACT(JK, TQ, AF.Square, ["TQ", "SM4"], ["JK", "SM4"], accum=SM[:, 4:5])
                                    ACT(SM[:, 5:6], SM[:, 4:5], AF.Sqrt, ["SM4", "CF"], ["SM5"], bias=EPSC, scale=1.0 / 64)
                                    RECIP(SM[:, 5:6], SM[:, 5:6], ["SM5"], ["SM5"])
                                    STT(YDT[:, qt, hh * 64:(hh + 1) * 64], TQ, SM[:, 5:6], SLG, ALU.mult, ALU.mult, ["TQ", "SM5", "SLG"], [("YDT", qt)])
                                if hh != 1:
                                    return
                                for qt in range(nqt):
                                    pt2, pk2 = psum("c")
                                    TR(pt2[:, 0:128], YDT[:, qt, :], IDF, [("YDT", qt), "CF"], [pk2])
                                    tt = q0 // 128 + qt
                                    CPY(YG[:, hp, tt * 128:(tt + 1) * 128], pt2[:, 0:128], [pk2], [("YG", hp, tt)], eng="act")
                            its.append((AB, C))
            run_pipe(its, d=2)
        wout_apply(l, 1, 18 if ctx_out else 16, G1B, scr)

    def mixer_pool(l, ctx_out, G1B, scr):
        ntiles = 18 if ctx_out else 16
        ntok = ntiles * 128
        TTt = scr.get([2, 2304], BF16)
        ZP = scr.get([18, 4, 128], BF16)
        PB = scr.get([4, 5, 128], BF16)
        PW = scr.get([4, 128], BF16)
        DMA("sp", PB, pband_d.rearrange("p (a b c) -> p a b c", a=4, b=5), "pband", (), ["PB"])
        DMA("pool", PW, poolw_d[l].rearrange("p (a b) -> p a b", a=4), "poolw", (), ["PW"])

        def t_evac(cc, t0, n, ps, pk):
            CPY(TTt[:, cc, t0:t0 + n], ps[:, 0:n], [pk], [("TT", cc)], eng="act")

        run_units([(proj_units(l, 1536, 256)[0], lambda u, uk: proj_fm(u, uk, ntok, t_evac))])
        for j in range(ntiles):
            ps, pk = psum("a")
            for g in range(4):
                MM(ps[:, g * 128:(g + 1) * 128], TTt[:, g // 2, j * 128:(j + 1) * 128], PW[:, g, :], True, True, [("TT", g // 2), "PW"], [pk])
            CPY(ZP[:, j, :, :], ps[:, :].rearrange("p (a b) -> p a b", a=4), [pk], [("ZP", j)], eng="dve")
        seqs = [(0, 16)] + ([(16, 2)] if ctx_out else [])
        for (j0, nt) in seqs:
            for jo in range(nt):
                for pc in range(2):
                    terms = []
                    for g in (2 * pc, 2 * pc + 1):
                        if jo > 0:
                            terms.append((j0 + jo - 1, g, 0))
                        terms.append((j0 + jo, g, 3 if jo == 0 else (4 if jo == nt - 1 else 1)))
                        if jo < nt - 1:
                            terms.append((j0 + jo + 1, g, 2))
                    ps, pk = psum("a")
                    for i, (ji, g, kind) in enumerate(terms):
                        MM(ps[:, 0:128], ZP[:, ji, g, :], PB[:, g, kind, :], i == 0, i == len(terms) - 1, [("ZP", ji), "PB"], [pk])
                    j = j0 + jo
                    ACT(YG[:, pc, j * 128:(j + 1) * 128], ps[:, 0:128], AF.Identity, [pk, "PF"], [("YG", pc, j)], scale=PSCALE[:, l, pc:pc + 1])
        wout_apply(l, 2, ntiles, G1B, scr)

    def mixer_fft(l, ctx_out, G1B, scr):
        ntiles = 18 if ctx_out else 16
        ntok = ntiles * 128
        TTt = scr.get([2, 2304], BF16)
        TCS = scr.get([18, 4, 128], BF16)
        FW = scr.get([4, 128], F32, parts=64)
        WCS = scr.get([4, 128], BF16)
        DMA("sp", FW, fftw_d[l].rearrange("p (a b) -> p a b", a=4), "fftw", (), ["FW"])
        CS64P = scr.get([2, 2, 128], F32, parts=64)
        DMA("sp", CS64P, cs64p_d.rearrange("p (a b c) -> p a b c", a=2, b=2), "fftw", (), ["CS64P"])
        T256 = scr.get([2, 2, 256], BF16)
        DMA("sp", T256, t256_d.rearrange("p (a b c) -> p a b c", a=2, b=2), "fftw", (), ["T256"])
        C256 = T256[:, 0, :, :]
        S256 = T256[:, 1, :, :]
        for pc in range(2):
            for cs in range(2):
                ps, pk = psum("c")
                for pos in range(2):
                    MM(ps[:, 0:128], CS64P[:, cs, pos, :], FW[:, 2 * pc + pos, :], pos == 0, pos == 1, ["CS64P", "FW"], [pk])
                CPY(WCS[:, pc * 2 + cs, :], ps[:, 0:128], [pk], ["WCS"], eng="act")

        def t_evac(cc, t0, n, ps, pk):
            CPY(TTt[:, cc, t0:t0 + n], ps[:, 0:n], [pk], [("TT", cc)], eng="act")

        run_units([(proj_units(l, 1792, 256)[0], lambda u, uk: proj_fm(u, uk, ntok, t_evac))])
        for j in range(ntiles):
            ps, pk = psum("a")
            for q in range(4):
                MM(ps[:, q * 128:(q + 1) * 128], TTt[:, q // 2, j * 128:(j + 1) * 128], WCS[:, q, :], True, True, [("TT", q // 2), "WCS"], [pk])
            CPY(TCS[:, j, :, :], ps[:, :].rearrange("p (a b) -> p a b", a=4), [pk], ["TCS"], eng="dve")
        units = []
        for kb in range(16):
            def ld(kb=kb):
                c_, ck_ = ring_load([16, 128], BF16, cl_d[kb].rearrange("p (a b) -> p a b", a=16))
                s_, sk_ = ring_load([16, 128], BF16, sl_d[kb].rearrange("p (a b) -> p a b", a=16))
                return (c_, s_), (ck_, sk_)

            def comp(tabs, keys, kb=kb):
                for pc in range(2):
                    ps, pk = psum("a")
                    n = 0
                    for cs in range(2):
                        for lt in range(16):
                            MM(ps[:, 0:128], TCS[:, lt, pc * 2 + cs, :], tabs[cs][:, lt, :], n == 0, n == 31, ["TCS", keys[cs]], [pk])
                            n += 1
                    CPY(YG[:, pc, kb * 128:(kb + 1) * 128], ps[:, 0:128], [pk], [("YG", pc, kb)], eng="act")
            units.append((ld, comp))
        run_units(units, depth=2)
        if ctx_out:
            for pc in range(2):
                ps, pk = psum("a")
                n = 0
                for cs in range(2):
                    tab = C256 if cs == 0 else S256
                    for lt in range(2):
                        MM(ps[:, 0:256], TCS[:, 16 + lt, pc * 2 + cs, :], tab[:, lt, :], n == 0, n == 3, ["TCS", "T256"], [pk])
                        n += 1
                CPY(YG[:, pc, 2048:2304], ps[:, 0:256], [pk], [("YG", pc, 16), ("YG", pc, 17)], eng="act")
        wout_apply(l, 3, ntiles, G1B, scr)

    def moe_phase(l, ctx_out, scr):
        ntiles = 18 if ctx_out else 16
        ntok = ntiles * 128
        G2B = scr.get([2, D], F32)
        BT = scr.get([128], F32)
        for src in range(2 if ctx_out else 1):
            bcast_vec(G2B[:, src, :], l, 5, src, BT)
        WR = scr.get([8, 32], F32)
        WRS = scr.get([2, 8, 32], F32)
        CROW = scr.get([2, 32], F32, parts=1)
        DMA("sp", WR, rw_d[l].rearrange("p (a b) -> p a b", a=8), "rw", (), ["WR"])
        B1T = scr.get([NE, 8, 2], F32)
        DMA("sp", B1T, b1t_d[l].rearrange("p (a b c) -> p a b c", a=NE, b=8), "b1t", (), ["B1T"])
        B2 = scr.get([D], F32, parts=NE)
        DMA("sp", B2, b2_d[l], "b2", (), ["B2"])
        TS(B1T[:, :, :, 1], B1T[:, :, :, 1], 1.0, None, ALU.add, None, ["B1T"], ["B1T"])
        G = scr.get([18, 32], F32)
        GA = scr.get([18, 32], F32)
        moe_mark = scr.mark()
        XNT = scr.get([8, 128], F32)
        LG = scr.get([32], F32)
        T8 = scr.get([8], F32)
        EX = scr.get([32], F32)
        MK = scr.get([32], F32)
        GT = scr.get([128], F32, parts=NE)
        def router(j, src, pss):
            for c in range(8):
                ps, pk = pss[c // 4]
                CPY(XNT[:, c, :], ps[:, (c % 4) * 128:(c % 4 + 1) * 128], [pk], ["XNT"], eng="dve")
            pl, plk = psum("c")
            for c in range(8):
                MM(pl[:, 0:32], XNT[:, c, :], WRS[:, src, c, :], c == 0, False, ["XNT", "WRS"], [plk])
            MM(pl[:, 0:32], ONESF[0:1, :], CROW[0:1, src, :], False, True, ["CF", "CROW"], [plk])
            CPY(LG, pl[:, 0:32], [plk], ["LG"], eng="dve")
            P.op("dve", lambda e: e.max(out=T8, in_=LG), ["LG"], ["T8"])
            TS(MK, LG, T8[:, 3:4], None, ALU.is_ge, None, ["LG", "T8"], ["MK"])
            TS(SM[:, 6:7], T8[:, 0:1], -1.0, None, ALU.mult, None, ["T8"], ["SM6"])
            ACT(EX, LG, AF.Exp, ["LG", "SM6"], ["EX"], bias=SM[:, 6:7], scale=1.0)
            TT(EX, EX, MK, ALU.mult, ["EX", "MK"], ["EX"])
            P.op("dve", lambda e: e.reduce_sum(out=SM[:, 7:8], in_=EX, axis=mybir.AxisListType.X), ["EX"], ["SM7"])
            RECIP(SM[:, 7:8], SM[:, 7:8], ["SM7"], ["SM7"])
            TS(G[:, j, :], EX, SM[:, 7:8], None, ALU.mult, None, ["EX", "SM7"], [("G", j)])
            TS(GA[:, j, :], G[:, j, :], 1.0 / ALPHA, None, ALU.mult, None, [("G", j)], [("GA", j)])

        def pre_router():
            for src in range(2 if ctx_out else 1):
                for c in range(8):
                    TS(WRS[:, src, c, :], WR[:, c, :], GS[:, c, src:src + 1], None, ALU.mult, None, ["WR", "GS"], ["WRS"])
                pc_, pck = psum("c")
                for c in range(8):
                    MM(pc_[0:1, 0:32], mod_ap(l, 3, c, src), WR[:, c, :], c == 0, c == 7, [("MOD", l), "WR"], [pck])
                TT(CROW[0:1, src, :], pc_[0:1, 0:32], RB[0:1, l, :], ALU.add, [pck, "PF"], ["CROW"])

        norm_phase(l, 3, G2T, list(range(ntiles)), scr, router, pre_router)

        TMPB = scr.get([512], F32)
        for j in range(ntiles):
            src = 0 if j < 16 else 1
            pt_, ptk_ = psum("c")
            TR(pt_[0:32, 0:128], G[:, j, :], IDF, [("G", j), "CF"], [ptk_])
            CPY(GT[:, :], pt_[0:32, 0:128], [ptk_], ["GT"], eng="act")
            for fb in range(2):
                ps, pk = psum("a")
                MM(ps[:, :], GT[:, :], B2[:, fb * 512:(fb + 1) * 512], True, True, ["GT", "B2"], [pk])
                TT(TMPB, ps[:, :], G2B[:, src, fb * 512:(fb + 1) * 512], ALU.mult, [pk, "GB"], ["TMPB"])
                TT(X[:, j, fb * 512:(fb + 1) * 512], X[:, j, fb * 512:(fb + 1) * 512], TMPB, ALU.add, [("X", j), "TMPB"], [("X", j)], eng="pool")

        P.barrier()
        scr.reset(moe_mark)
        NB = 2
        GC = [scr.get([512], F32) for _ in range(NB)]
        SI = [scr.get([512], F32) for _ in range(NB)]
        L1 = [scr.get([512], F32) for _ in range(NB)]
        TO = [scr.get([256], F32) for _ in range(NB)]
        blocks = tok_blocks(ntok)
        units = []
        cnt = [0, 0]
        for e in range(n_exp):
            w1v = w1_d[l, e].rearrange("(k p) n -> p k n", p=128)
            w2v = w2_d[l, e].rearrange("(k p) n -> p k n", p=128)
            for p in range(8):
                def ld(p=p, w1v=w1v):
                    return ring_load([8, 256], BF16, w1v[:, :, p * 256:(p + 1) * 256])

                def comp(unit, ukey, e=e, p=p):
                    uv = unit.rearrange("p k (f two) -> p k two f", two=2)
                    for (t0, n) in blocks:
                        pg, pgk = psum("a")
                        pl, plk = psum("a")
                        for k in range(8):
                            MM(pg[:, 0:n], uv[:, k, 0, :], HT[:, k, t0:t0 + n], k == 0, k == 7, [ukey] + tkeys("HT", k, t0, n), [pgk])
                        for k in range(8):
                            MM(pl[:, 0:n], uv[:, k, 1, :], HT[:, k, t0:t0 + n], k == 0, k == 7, [ukey] + tkeys("HT", k, t0, n), [plk])
                        i = cnt[0] % NB
                        cnt[0] += 1
                        TS(GC[i][:, 0:n], pg[:, 0:n], B1T[:, e, p, 0:1], 7.0, ALU.add, ALU.min, [pgk, "B1T"], [("GC", i)])
                        ACT(SI[i][:, 0:n], GC[i][:, 0:n], AF.Silu, [("GC", i)], [("SI", i)], scale=ALPHA)
                        TS(L1[i][:, 0:n], pl[:, 0:n], B1T[:, e, p, 1:2], -6.0, ALU.add, ALU.max, [plk, "B1T"], [("L1", i)])
                        STT(ACTB[:, p, t0:t0 + n], L1[i][:, 0:n], 8.0, SI[i][:, 0:n], ALU.min, ALU.mult, [("L1", i), ("SI", i)],
                            [("ACTB", p, t0 // 512)])
                units.append((ld, comp))
            for q in range(4):
                def ld(q=q, w2v=w2v):
                    return ring_load([8, 256], BF16, w2v[:, :, q * 256:(q + 1) * 256])

                def comp(unit, ukey, e=e, q=q):
                    for j in range(ntiles):
                        src = 0 if j < 16 else 1
                        po, pok = psum("b")
                        for k in range(8):
                            MM(po[:, 0:256], ACTB[:, k, j * 128:(j + 1) * 128], unit[:, k, :], k == 0, k == 7, [ukey, ("ACTB", k, j // 4)], [pok])
                        i = cnt[1] % NB
                        cnt[1] += 1
                        STT(TO[i], po[:, 0:256], GA[:, j, e:e + 1], G2B[:, src, q * 256:(q + 1) * 256], ALU.mult, ALU.mult,
                            [pok, ("GA", j), "GB"], [("TO", i)])
                        TT(X[:, j, q * 256:(q + 1) * 256], X[:, j, q * 256:(q + 1) * 256], TO[i], ALU.add, [("X", j), ("X", j, q), ("TO", i)], [("X", j, q)],
                           eng="pool")
                units.append((ld, comp))
        psr["b"] = (4, 4)
        run_units(units, depth=3)
        psr["b"] = (4, 2)

    for l in range(nlayers):
        ctx_out = l < nlayers - 1
        ntiles_all = 18
        scr = Bump([(OFF_B + 9216, 36864 - 9216), (OFF_S, S_SZ)])
        G1B = scr.get([2, D], F32)
        BT = scr.get([128], F32)
        base_mark = scr.mark()
        norm_phase(l, 0, G1T, list(range(18)), scr)
        for src in range(2 if ctx_out else 1):
            bcast_vec(G1B[:, src, :], l, 2, src, BT)
        P.barrier()
        for mi, fn in enumerate((mixer_na, mixer_diff, mixer_pool, mixer_fft)):
            if mi not in mixers:
                continue
            scr.reset(base_mark)
            fn(l, ctx_out, G1B, scr)
            P.barrier()
        if stop == ("mix", l):
            break
        if do_moe:
            scr = Bump([(OFF_S, S_SZ)])
            moe_phase(l, ctx_out, scr)
            P.barrier()
        if stop == ("moe", l):
            break

    for j in range(16):
        DMA("sp", out_d[j * 128:(j + 1) * 128, :], X[:, j, :], "st", [("X", j)] + [("X", j, q) for q in range(4)], [])
    if stop is not None:
        dbg = nc.dram_tensor("dbgc", [LC, D], F32, kind="ExternalOutput").ap()
        for j in range(2):
            DMA("sp", dbg[j * 128:(j + 1) * 128, :], X[:, 16 + j, :], "st", [("X", 16 + j)] + [("X", 16 + j, q) for q in range(4)], [])
    P.emit(nc, final_waits=["st"])
    return nc, len(P.ops)


def make_in_maps(inp, nlayers=2, cores=range(8)):
    f = lambda a: np.ascontiguousarray(np.asarray(a, np.float32))
    cst = host_constants()
    lay = host_layouts(inp, nlayers)
    shared = dict(cb=cst["cb"], cf=cst["cf"], t256=cst["t256"], cs64p=cst["cs64p"], rope=cst["rope"], pband=cst["pband"], cl=cst["cl"], sl=cst["sl"],
                  wada=f(inp["w_ada"]), win=f(inp["w_in"]), wout=f(inp["w_out"]),
                  nab=lay["nab"], poolw=lay["poolw"], fftw=lay["fftw"], rw=lay["rw"], b1t=lay["b1t"], b2=lay["b2"],
                  w1=f(inp["moe_w1"]), w2=f(inp["moe_w2"]))
    x = f(inp["x"])
    cx = f(inp["ctx"])
    maps = []
    for b in cores:
        m = dict(shared)
        m["x"] = x[b]
        m["cx"] = cx[b]
        m["pf"] = host_pf(inp, b, nlayers)
        maps.append(m)
    return maps


_NC_CACHE = {}


def kernel(**inputs):
    if "nc" not in _NC_CACHE:
        _NC_CACHE["nc"] = build_nc()[0]
    nc = _NC_CACHE["nc"]
    maps = make_in_maps(inputs)
    res = run_bass_kernel_spmd(nc, maps, core_ids=list(range(8)))
    return np.stack([np.asarray(r["out"], np.float32) for r in res.results], axis=0)
```

```python
import math
import numpy as np
import ml_dtypes
import concourse.bass as bass
import concourse.mybir as mybir
from concourse.bass_utils import run_bass_kernel_spmd

F32 = mybir.dt.float32
BF16 = mybir.dt.bfloat16
ALU = mybir.AluOpType
AF = mybir.ActivationFunctionType
ENGS = ("pe", "act", "dve", "pool", "sp")

D = 1024
L = 2048
LC = 256
NE = 32
ALPHA = 1.702
EPS = 1e-6
MASKV = -30000.0


class Op:
    __slots__ = ("eng", "fn", "reads", "writes", "dma", "waits", "signal", "sig_idx", "dma_val", "deps")

    def __init__(self, eng, fn, reads, writes, dma):
        self.eng = eng
        self.fn = fn
        self.reads = reads
        self.writes = writes
        self.dma = dma
        self.waits = []
        self.signal = False
        self.sig_idx = 0
        self.dma_val = 0
        self.deps = None


class Prog:
    def __init__(self):
        self.ops = []
        self.last_w = {}
        self.readers = {}
        self.dma_counts = {}
        self.group_streams = set()
        self.pending_barrier = {}

    def op(self, eng, fn, reads=(), writes=(), dma=None):
        o = Op(eng, fn, tuple(reads), tuple(writes), dma)
        idx = len(self.ops)
        deps = set()
        for k in o.reads:
            w = self.last_w.get(k)
            if w is not None:
                deps.add(w)
        for k in o.writes:
            w = self.last_w.get(k)
            if w is not None:
                deps.add(w)
            rs = self.readers.get(k)
            if rs:
                deps.update(rs)
        for k in o.reads:
            self.readers.setdefault(k, []).append(idx)
        for k in o.writes:
            self.last_w[k] = idx
            self.readers[k] = []
        if eng in self.pending_barrier:
            deps.update(self.pending_barrier.pop(eng))
        if dma is not None:
            self.dma_counts[dma] = self.dma_counts.get(dma, 0) + 16
            o.dma_val = self.dma_counts[dma]
        o.deps = deps
        self.ops.append(o)
        return idx

    def barrier(self):
        last = {}
        for i, o in enumerate(self.ops):
            last[o.eng] = i
        lastd = {}
        for i, o in enumerate(self.ops):
            if o.dma is not None:
                lastd[o.dma] = i
        s = set(last.values()) | set(lastd.values())
        for e in ENGS:
            self.pending_barrier[e] = set(s) | self.pending_barrier.get(e, set())

    def finalize(self):
        need = {}
        for ci, c in enumerate(self.ops):
            for pi in c.deps:
                p = self.ops[pi]
                if p.dma is None:
                    if p.eng == c.eng and p.eng in ("pe", "sp"):
                        continue
                    p.signal = True
                need.setdefault(ci, []).append(pi)
        cnt = {e: 0 for e in ENGS}
        for o in self.ops:
            if o.signal:
                cnt[o.eng] += 1
                o.sig_idx = cnt[o.eng]
        waited = {e: {} for e in ENGS}
        for ci, c in enumerate(self.ops):
            ws = {}
            for pi in need.get(ci, ()):
                p = self.ops[pi]
                if p.dma is not None:
                    key = ("dma", p.dma)
                    val = self.dma_counts[p.dma] if p.dma in self.group_streams else p.dma_val
                else:
                    key, val = ("eng", p.eng), p.sig_idx
                if ws.get(key, 0) < val:
                    ws[key] = val
            wd = waited[c.eng]
            for key, val in ws.items():
                if wd.get(key, 0) >= val:
                    continue
                wd[key] = val
                c.waits.append((key, val))

    def emit(self, nc, final_waits=()):
        import contextlib
        self.finalize()
        with contextlib.ExitStack() as es:
            sems = {}
            for e in ENGS:
                sems[("eng", e)] = es.enter_context(nc.semaphore("s_" + e))
            for d in self.dma_counts:
                sems[("dma", d)] = es.enter_context(nc.semaphore("d_" + d))
            block = es.enter_context(nc.Block())
            ops = self.ops
            counts = self.dma_counts

            def body(engname):
                def run(eng):
                    for o in ops:
                        if o.eng != engname:
                            continue
                        for key, val in o.waits:
                            eng.wait_ge(sems[key], val)
                        ins = o.fn(eng)
                        if o.dma is not None:
                            ins.then_inc(sems[("dma", o.dma)], 16)
                        elif o.signal:
                            ins.then_inc(sems[("eng", engname)], 1)
                    if engname == "sp":
                        for d in final_waits:
                            eng.wait_ge(sems[("dma", d)], counts[d])
                return run

            block.tensor(body("pe"))
            block.scalar(body("act"))
            block.vector(body("dve"))
            block.gpsimd(body("pool"))
            block.sync(body("sp"))


def _bf(a):
    return np.ascontiguousarray(a.astype(ml_dtypes.bfloat16))


CB_OFF = {}
CF_OFF = {}
PF_OFF = {}


def _layout(offs, items):
    o = 0
    for name, n in items:
        offs[name] = (o, n)
        o += n
    return o


NCB = _layout(CB_OFF, [("ident", 128), ("bd64", 128), ("bd32", 128), ("perm", 128)])
NCF = _layout(CF_OFF, [("identf", 128), ("onesf", 128), ("eps", 1), ("mask12", 2), ("seven", 1), ("mask4", 4)])
NPF = _layout(PF_OFF, [("cvec", 16), ("bada", 96), ("g1", 16), ("g2", 16), ("naq", 2), ("nak", 2), ("dfq", 2),
                       ("dfk", 2), ("lam", 256), ("subln", 128), ("pscale", 4), ("rb", 64)])

_CONST_CACHE = {}


def host_constants():
    if _CONST_CACHE:
        return _CONST_CACHE
    p = np.arange(128)
    cb = np.zeros((128, NCB), np.float32)
    cb[:, 0:128] = np.eye(128)
    cb[:, 128:256] = (p[:, None] // 64 == p[None, :] // 64) / 64.0
    cb[:, 256:384] = (p[:, None] // 32 == p[None, :] // 32) / 32.0
    partner = np.where((p % 16) < 8, p + 8, p - 8)
    perm = np.zeros((128, 128), np.float32)
    perm[partner, p] = 1.0
    cb[:, 384:512] = perm
    lt = np.arange(2)[None, :, None]
    lin = p[:, None, None]
    k = np.arange(256)[None, None, :]
    ang = 2 * np.pi * ((lt * 128 + lin) * k % 256) / 256.0
    t256 = np.zeros((128, 1024), np.float32)
    t256[:, 0:512] = (np.cos(ang) / 16.0).reshape(128, 512)
    t256[:, 512:1024] = (-np.sin(ang) / 16.0).reshape(128, 512)
    cf = np.zeros((128, NCF), np.float32)
    cf[:, 0:128] = np.eye(128)
    cf[:, 128:256] = 1.0
    cf[:, 256] = EPS
    cf[:, 257] = (p % 64 < 32)
    cf[:, 258] = (p % 64 >= 32)
    m = np.arange(64)[:, None]
    c = np.arange(64)[None, :]
    a64 = 2 * np.pi * (m * c % 64) / 64.0
    cs = np.zeros((64, 2, 2, 128), np.float32)
    cs[:, 0, 0, 0:64] = np.cos(a64) / 8.0
    cs[:, 0, 1, 64:128] = np.cos(a64) / 8.0
    cs[:, 1, 0, 0:64] = np.sin(a64) / 8.0
    cs[:, 1, 1, 64:128] = np.sin(a64) / 8.0
    cf[:, 259] = 7.0
    for j_ in range(4):
        cf[:, 260 + j_] = (p // 32 == j_)
    d = p % 32
    seg = d // 16
    i = d % 16
    j = i % 8
    inv = 10000.0 ** (-(2.0 * j) / 16.0)
    t = np.arange(L)
    pos = np.where(seg[:, None] == 0, (t // 64)[None, :], (t % 64)[None, :]).astype(np.float64)
    angr = pos * inv[:, None]
    rope = np.zeros((128, 2, L), np.float32)
    rope[:, 0, :] = np.cos(angr)
    rope[:, 1, :] = np.where((i < 8)[:, None], -np.sin(angr), np.sin(angr))
    pband = np.zeros((128, 4, 5, 128), np.float32)
    Lp = 512
    posp = np.arange(Lp)
    for g, win in enumerate((2, 4, 8, 16)):
        lo = np.clip(posp - win // 2, 0, Lp)
        hi = np.clip(posp - win // 2 + win, 0, Lp)
        M = np.zeros((Lp, Lp), np.float64)
        for o in range(Lp):
            M[o, lo[o]:hi[o]] = 1.0 / (hi[o] - lo[o])
        M -= np.eye(Lp)
        pband[:, g, 0, :] = M[128:256, 0:128].T
        pband[:, g, 1, :] = M[128:256, 128:256].T
        pband[:, g, 2, :] = M[128:256, 256:384].T
        pband[:, g, 3, :] = M[0:128, 0:128].T
        pband[:, g, 4, :] = M[384:512, 384:512].T
    kb = np.arange(16)[:, None, None, None]
    lin4 = np.arange(128)[None, :, None, None]
    lt4 = np.arange(16)[None, None, :, None]
    kk = np.arange(128)[None, None, None, :]
    prod = ((lt4 * 128 + lin4) * (kb * 128 + kk)) % L
    angL = 2 * np.pi * prod / float(L)
    s = 1.0 / math.sqrt(L)
    cl = (np.cos(angL) * s).reshape(16, 128, 2048)
    sl = (-np.sin(angL) * s).reshape(16, 128, 2048)
    _CONST_CACHE.update(dict(cb=_bf(cb), cf=cf, t256=_bf(t256), cs64p=np.ascontiguousarray(cs.reshape(64, 512)), rope=_bf(rope.reshape(128, 2 * L)),
                             pband=_bf(pband.reshape(128, 4 * 5 * 128)), cl=_bf(cl), sl=_bf(sl)))
    return _CONST_CACHE


def host_layouts(inp, nlayers=2):
    f = lambda a: np.asarray(a, np.float32)
    out = {}
    p = np.arange(128)
    rpb = f(inp["na_rpb"])
    ck = np.arange(64)[:, None]
    cq = np.arange(64)[None, :]
    col_start = np.clip(np.arange(64) - 8, 0, 48)
    inwin = (ck >= col_start[None, :]) & (ck < col_start[None, :] + 16)
    relc = np.clip(ck - cq, -15, 15) + 15
    nab = np.full((nlayers, 2, 64, 4, 14, 64), MASKV, np.float32)
    for l in range(nlayers):
        for h in range(4):
            for m0 in range(14):
                for jj in range(2):
                    blk = rpb[l, h, m0 + jj][relc]
                    nab[l, jj, :, h, m0, :] = np.where(inwin, blk, MASKV)
    out["nab"] = nab.reshape(nlayers, 128, 4 * 14 * 64)
    pw = f(inp["pool_w"])
    poolw = np.zeros((nlayers, 128, 4, 128), np.float32)
    fw = f(inp["fft_w"])
    fftw = np.zeros((nlayers, 64, 4, 128), np.float32)
    for l in range(nlayers):
        for g in range(4):
            o = (g % 2) * 64
            poolw[l, o:o + 64, g, o:o + 64] = pw[l, g]
            fftw[l, :, g, o:o + 64] = fw[l, g]
    out["poolw"] = poolw.reshape(nlayers, 128, 512)
    out["fftw"] = fftw.reshape(nlayers, 64, 512)
    rw = f(inp["router_w"])
    out["rw"] = np.ascontiguousarray(rw.reshape(nlayers, 8, 128, 32).transpose(0, 2, 1, 3)).reshape(nlayers, 128, 256)
    b1 = f(inp["moe_b1"])
    b1t = b1.reshape(nlayers, NE, 8, 128, 2).transpose(0, 3, 1, 2, 4)
    out["b1t"] = np.ascontiguousarray(b1t).reshape(nlayers, 128, NE * 16)
    out["b2"] = np.ascontiguousarray(f(inp["moe_b2"]))
    return out


def host_pf(inp, b, nlayers=2):
    f = lambda a: np.asarray(a, np.float32)
    p = np.arange(128)
    pf = np.zeros((128, NPF), np.float32)

    def put(name, arr):
        o, n = PF_OFF[name]
        pf[:, o:o + n] = arr.reshape(128, n)

    cv = np.zeros((128, 8, 2), np.float32)
    cv[:, :, 0] = f(inp["c"])[b].reshape(8, 128).T
    cv[:, :, 1] = f(inp["c_ctx"]).reshape(8, 128).T
    put("cvec", cv)
    put("bada", f(inp["b_ada"]).reshape(nlayers, 48, 128).transpose(2, 0, 1))
    put("g1", f(inp["g_norm1"]).reshape(nlayers, 8, 128).transpose(2, 0, 1))
    put("g2", f(inp["g_norm2"]).reshape(nlayers, 8, 128).transpose(2, 0, 1))
    put("naq", f(inp["na_q_gain"])[:, p % 64].T)
    put("nak", f(inp["na_k_gain"])[:, p % 64].T)
    put("dfq", f(inp["diff_q_gain"])[:, p % 32].T)
    put("dfk", f(inp["diff_k_gain"])[:, p % 32].T)
    lam = np.stack([f(inp["diff_lambda_q1"]), f(inp["diff_lambda_k1"]), f(inp["diff_lambda_q2"]),
                    f(inp["diff_lambda_k2"])], axis=1)
    put("lam", np.broadcast_to(lam[None], (128, nlayers, 4, 32)).copy())
    put("subln", np.broadcast_to(f(inp["diff_subln"])[None], (128, nlayers, 64)).copy())
    put("pscale", f(inp["pool_scale"]).reshape(nlayers, 2, 128).transpose(2, 0, 1))
    put("rb", np.broadcast_to(f(inp["router_b"])[None], (128, nlayers, 32)).copy())
    return pf


def build_nc(nlayers=2, do_moe=True, n_exp=NE, stop=None, mixers=(0, 1, 2, 3), ne_decl=NE):
    nc = bass.Bass("TRN2", target_bir_lowering=False)
    P = Prog()
    P.group_streams = {"const", "xin"}

    def din(name, shape, dt=F32):
        return nc.dram_tensor(name, list(shape), dt, kind="ExternalInput").ap()

    x_d = din("x", [L, D])
    cx_d = din("cx", [LC, D])
    pf_d = din("pf", [128, NPF])
    cb_d = din("cb", [128, NCB], BF16)
    cf_d = din("cf", [128, NCF])
    wada_d = din("wada", [nlayers, D, 6 * D])
    win_d = din("win", [nlayers, D, 2048])
    wout_d = din("wout", [nlayers, D, D])
    nab_d = din("nab", [nlayers, 128, 4 * 14 * 64])
    rope_d = din("rope", [128, 2 * L], BF16)
    pband_d = din("pband", [128, 2560], BF16)
    poolw_d = din("poolw", [nlayers, 128, 512])
    fftw_d = din("fftw", [nlayers, 64, 512])
    cl_d = din("cl", [16, 128, 2048], BF16)
    t256_d = din("t256", [128, 1024], BF16)
    cs64p_d = din("cs64p", [64, 512])
    sl_d = din("sl", [16, 128, 2048], BF16)
    rw_d = din("rw", [nlayers, 128, 256])
    b1t_d = din("b1t", [nlayers, 128, NE * 16])
    b2_d = din("b2", [nlayers, NE, D])
    w1_d = din("w1", [nlayers, ne_decl, D, 2 * D])
    w2_d = din("w2", [nlayers, ne_decl, D, D])
    out_d = nc.dram_tensor("out", [L, D], F32, kind="ExternalOutput").ap()

    TOTAL = 212000
    ALL = nc.alloc_sbuf_tensor("allsb", [128, TOTAL // 2], BF16)
    OFF_X = 0
    OFF_HT = 73728
    OFF_B = OFF_HT + 36864
    OFF_RING = OFF_B + 36864
    OFF_MISC = OFF_RING + 16384
    MISC_SZ = 7168
    OFF_S = OFF_MISC + MISC_SZ
    S_SZ = TOTAL - OFF_S

    def carve(off, shape, dt, parts=128):
        n = 1
        for s_ in shape:
            n *= s_
        assert off % 4 == 0
        if dt == F32:
            ap = ALL[0:parts, off // 2: off // 2 + 2 * n].bitcast(F32)
        else:
            ap = ALL[0:parts, off // 2: off // 2 + n]
        if len(shape) == 2:
            ap = ap.rearrange("p (a b) -> p a b", a=shape[0])
        elif len(shape) == 3:
            ap = ap.rearrange("p (a b c) -> p a b c", a=shape[0], b=shape[1])
        elif len(shape) == 4:
            ap = ap.rearrange("p (a b c d) -> p a b c d", a=shape[0], b=shape[1], c=shape[2])
        return ap

    def nbytes(shape, dt):
        n = 4 if dt == F32 else 2
        for s_ in shape:
            n *= s_
        return (n + 31) // 32 * 32

    class Bump:
        def __init__(self, regions):
            self.regions = regions
            self.cur = [r[0] for r in regions]

        def get(self, shape, dt, parts=128):
            nb = nbytes(shape, dt)
            for i, (o, sz) in enumerate(self.regions):
                if self.cur[i] + nb <= o + sz:
                    a = carve(self.cur[i], shape, dt, parts)
                    self.cur[i] += nb
                    return a
            raise RuntimeError("scratch overflow %s" % (shape,))

        def mark(self):
            return list(self.cur)

        def reset(self, m):
            self.cur = list(m)

    X = carve(OFF_X, [18, D], F32)
    HT = carve(OFF_HT, [8, 2304], BF16)
    YG = carve(OFF_B, [2, 2304], BF16)
    ACTB = carve(OFF_B, [8, 2304], BF16)
    misc = Bump([(OFF_MISC, MISC_SZ)])
    CB = misc.get([NCB], BF16)
    CF = misc.get([NCF], F32)
    PF = misc.get([NPF], F32)
    MOD = misc.get([nlayers, 48, 2], F32)
    CS = misc.get([8, 2], F32)
    GS = misc.get([8, 2], F32)
    SS = misc.get([18], F32)
    RSTD = misc.get([18], F32)
    NLAM = misc.get([2], F32)
    SM = misc.get([16], F32)

    def cbv(name):
        o, n = CB_OFF[name]
        return CB[:, o:o + n]

    def cfv(name, parts=128):
        o, n = CF_OFF[name]
        return CF[0:parts, o:o + n]

    def pfv(name):
        o, n = PF_OFF[name]
        return PF[:, o:o + n]

    IDB, BD64, BD32, PERM = cbv("ident"), cbv("bd64"), cbv("bd32"), cbv("perm")
    IDF, ONESF, EPSC = cfv("identf"), cfv("onesf"), cfv("eps")
    MASK12 = cfv("mask12")
    MASK4 = cfv("mask4")
    CVEC = pfv("cvec").rearrange("p (a b) -> p a b", a=8)
    BADA = pfv("bada").rearrange("p (a b) -> p a b", a=nlayers)
    G1T = pfv("g1").rearrange("p (a b) -> p a b", a=nlayers)
    G2T = pfv("g2").rearrange("p (a b) -> p a b", a=nlayers)
    LAMV = pfv("lam").rearrange("p (a b c) -> p a b c", a=nlayers, b=4)
    SUBLN = pfv("subln").rearrange("p (a b) -> p a b", a=nlayers)
    PSCALE = pfv("pscale").rearrange("p (a b) -> p a b", a=nlayers)
    RB = pfv("rb").rearrange("p (a b) -> p a b", a=nlayers)

    PS = [nc.alloc_psum_tensor("ps%d" % i, [128, 512], F32) for i in range(8)]
    psc = {"a": 0, "b": 0, "c": 0}
    psr = {"a": (0, 4), "b": (4, 2), "c": (6, 2)}

    def psum(role):
        base, n = psr[role]
        i = base + psc[role] % n
        psc[role] += 1
        return PS[i], ("ps", i)

    def MM(out, lhsT, rhs, start, stop, r, w, skip=False):
        if skip:
            P.op("pe", lambda e: e.matmul(out, lhsT=lhsT, rhs=rhs, start=start, stop=stop, skip_group_check=True), r, w)
        else:
            P.op("pe", lambda e: e.matmul(out, lhsT=lhsT, rhs=rhs, start=start, stop=stop), r, w)

    def TR(out, in_, ident, r, w):
        P.op("pe", lambda e: e.transpose(out=out, in_=in_, identity=ident), r, w)

    def ACT(out, in_, func, r, w, bias=None, scale=None, accum=None):
        kw = {}
        if bias is not None:
            kw["bias"] = bias
        if scale is not None:
            kw["scale"] = scale
        if accum is not None:
            kw["accum_out"] = accum
        P.op("act", lambda e: e.activation(out=out, in_=in_, func=func, **kw), r, w)

    def TS(out, in0, s1, s2, op0, op1, r, w, eng="dve"):
        if op1 is None:
            P.op(eng, lambda e: e.tensor_scalar(out=out, in0=in0, scalar1=s1, scalar2=None, op0=op0), r, w)
        else:
            P.op(eng, lambda e: e.tensor_scalar(out=out, in0=in0, scalar1=s1, scalar2=s2, op0=op0, op1=op1), r, w)

    def TT(out, in0, in1, op, r, w, eng="dve"):
        P.op(eng, lambda e: e.tensor_tensor(out=out, in0=in0, in1=in1, op=op), r, w)

    def STT(out, in0, scalar, in1, op0, op1, r, w, eng="dve"):
        P.op(eng, lambda e: e.scalar_tensor_tensor(out=out, in0=in0, scalar=scalar, in1=in1, op0=op0, op1=op1), r, w)

    def CPY(out, in_, r, w, eng="dve"):
        if eng == "act":
            P.op("act", lambda e: e.copy(out=out, in_=in_), r, w)
        else:
            P.op(eng, lambda e: e.tensor_copy(out=out, in_=in_), r, w)

    def RECIP(out, in_, r, w):
        P.op("dve", lambda e: e.reciprocal(out=out, in_=in_), r, w)

    def MEMSET(out, val, w, eng="pool"):
        P.op(eng, lambda e: e.memset(out, val), (), w)

    def DMA(eng, out, in_, stream, r, w):
        P.op(eng, lambda e: e.dma_start(out=out, in_=in_), r, w, dma=stream)

    ring_n = [0]

    def ring_load(shape, dt, dram_ap, parts=128):
        s = ring_n[0] % 4
        ring_n[0] += 1
        v = carve(OFF_RING + 4096 * s, shape, dt, parts)
        DMA("pool", v, dram_ap, "ring%d" % s, (), [("ring", s)])
        return v, ("ring", s)

    def run_units(units, depth=3):
        loaded = []
        for i in range(len(units)):
            while len(loaded) < min(len(units), i + depth):
                loaded.append(units[len(loaded)][0]())
            units[i][1](*loaded[i])

    def run_pipe(its, d=2):
        n = len(its)
        for i in range(n + d):
            if i < n:
                its[i][0]()
            if i >= d:
                its[i - d][1]()

    def tkeys(name, c, t0, n):
        return [(name, c, t) for t in range(t0 // 128, (t0 + n + 127) // 128)]

    DMA("sp", CB, cb_d, "const", (), ["CB"])
    DMA("sp", CF, cf_d, "const", (), ["CF"])
    DMA("sp", PF, pf_d, "const", (), ["PF"])
    for j in range(16):
        DMA("sp", X[:, j, :], x_d[j * 128:(j + 1) * 128, :], "xin", (), [("X", j)])
    for j in range(2):
        DMA("sp", X[:, 16 + j, :], cx_d[j * 128:(j + 1) * 128, :], "xin", (), [("X", 16 + j)])
    CONSTS = ["CB", "CF", "PF"]
    ACT(CS, CVEC, AF.Silu, CONSTS, ["CS"])
    sA = Bump([(OFF_HT, 36864)])
    WA = [sA.get([8, 256], F32) for _ in range(2)]
    for l in range(nlayers):
        wv = wada_d[l].rearrange("(k p) n -> p k n", p=128)
        for u in range(24):
            b_ = u % 2
            DMA("sp", WA[b_], wv[:, :, u * 256:(u + 1) * 256], "wa%d" % b_, (), [("WA", b_)])
            for jj in range(2):
                j = u * 2 + jj
                ps, pk = psum("c")
                for k in range(8):
                    MM(ps[:, 0:2], WA[b_][:, k, jj * 128:(jj + 1) * 128], CS[:, k, :], k == 0, k == 7,
                       [("WA", b_), "CS"], [pk])
                TS(MOD[:, l, j, :], ps[:, 0:2], BADA[:, l, j:j + 1], None, ALU.add, None, [pk, "PF"], [("MOD", l)])
    P.barrier()

    def mod_ap(l, which, c, src):
        return MOD[:, l, which * 8 + c, src:src + 1]

    def bcast_vec(dst, l, which, src, tmp):
        for c in range(8):
            TS(tmp, IDF, mod_ap(l, which, c, src), None, ALU.mult, None, ["CF", ("MOD", l)], ["bctmp"])
            ps, pk = psum("c")
            MM(ps[:, 0:128], ONESF, tmp, True, True, ["CF", "bctmp"], [pk])
            CPY(dst[:, c * 128:(c + 1) * 128], ps[:, 0:128], [pk], ["GB"], eng="act")

    def norm_phase(l, which0, gT, tiles, scr, router=None, pre_router=None):
        XNs = [scr.get([D], F32) for _ in range(2)]
        JUNK = scr.get([D], BF16)
        for src in range(2):
            TS(GS[:, :, src], MOD[:, l, (which0 + 1) * 8:(which0 + 2) * 8, src], 1.0, None, ALU.add, None,
               [("MOD", l)], ["GS"])
            TT(GS[:, :, src], GS[:, :, src], gT[:, l, :], ALU.mult, ["GS", "PF"], ["GS"])
        if pre_router is not None:
            pre_router()
        MEMSET(SS, 0.0, [("SS", j) for j in range(18)], eng="dve")
        for j in tiles:
            src = 0 if j < 16 else 1
            XN = XNs[j % 2]
            xnk = ("XN", j % 2)
            xk = [("X", j)] + [("X", j, q) for q in range(4)]
            ACT(JUNK, X[:, j, :], AF.Square, xk, [("SS", j)], accum=SS[:, j:j + 1])
            ACT(RSTD[:, j:j + 1], SS[:, j:j + 1], AF.Sqrt, [("SS", j), "CF"], [("RSTD", j)], bias=EPSC, scale=1.0 / D)
            RECIP(RSTD[:, j:j + 1], RSTD[:, j:j + 1], [("RSTD", j)], [("RSTD", j)])
            TS(XN, X[:, j, :], RSTD[:, j:j + 1], None, ALU.mult, None, xk + [("RSTD", j)], [xnk])
            pss = [psum("a"), psum("a")]
            for c in range(8):
                ps, pk = pss[c // 4]
                TR(ps[:, (c % 4) * 128:(c % 4 + 1) * 128], XN[:, c * 128:(c + 1) * 128], IDF, [xnk, "CF"], [pk])
            if router is not None:
                router(j, src, pss)
            for c in range(8):
                ps, pk = pss[c // 4]
                ACT(HT[:, c, j * 128:(j + 1) * 128], ps[:, (c % 4) * 128:(c % 4 + 1) * 128], AF.Identity,
                    [pk, "GS", ("MOD", l)], [("HT", c, j)], bias=mod_ap(l, which0, c, src), scale=GS[:, c, src:src + 1])

    def proj_units(l, col0, ncols):
        wv = win_d[l].rearrange("(k p) n -> p k n", p=128)
        return [(lambda c0=c0: ring_load([8, 256], BF16, wv[:, :, c0:c0 + 256])) for c0 in range(col0, col0 + ncols, 256)]

    def tok_blocks(ntok):
        return [(t0, min(512, ntok - t0)) for t0 in range(0, ntok, 512)]

    def proj_fm(unit, ukey, ntok, evac):
        for cc in range(2):
            for (t0, n) in tok_blocks(ntok):
                ps, pk = psum("a")
                for k in range(8):
                    MM(ps[:, 0:n], unit[:, k, cc * 128:(cc + 1) * 128], HT[:, k, t0:t0 + n], k == 0, k == 7,
                       [ukey] + tkeys("HT", k, t0, n), [pk])
                evac(cc, t0, n, ps, pk)

    def proj_tm(unit, ukey, t0, evac):
        ps, pk = psum("a")
        for k in range(8):
            MM(ps[:, 0:256], HT[:, k, t0:t0 + 128], unit[:, k, :], k == 0, k == 7, [ukey] + tkeys("HT", k, t0, 128), [pk])
        evac(ps, pk)

    def wout_apply(l, g, ntiles, G1B, scr):
        TMP = [scr.get([512], F32) for _ in range(2)]
        wv = wout_d[l][g * 256:(g + 1) * 256, :].rearrange("(k p) n -> p k n", p=128)
        unit, ukey = ring_load([2, D], BF16, wv)
        n = 0
        for j in range(ntiles):
            src = 0 if j < 16 else 1
            for fb in range(2):
                ps, pk = psum("a")
                for k in range(2):
                    MM(ps[:, :], YG[:, k, j * 128:(j + 1) * 128], unit[:, k, fb * 512:(fb + 1) * 512], k == 0, k == 1,
                       [ukey, ("YG", k, j)], [pk])
                t = TMP[n % 2]
                tk = ("wtmp", n % 2)
                n += 1
                TT(t, ps[:, :], G1B[:, src, fb * 512:(fb + 1) * 512], ALU.mult, [pk, "GB"], [tk])
                TT(X[:, j, fb * 512:(fb + 1) * 512], X[:, j, fb * 512:(fb + 1) * 512], t, ALU.add, [("X", j), tk], [("X", j)],
                   eng="pool")

    def qk_norm_evac(SQ, RS, gain_ap, bd, out_fn):
        def evac(cc, t0, n, ps, pk):
            ACT(SQ[:, 0:n], ps[:, 0:n], AF.Square, [pk], ["SQ"])
            ps2, pk2 = psum("c")
            MM(ps2[:, 0:n], bd, SQ[:, 0:n], True, True, ["SQ", "CB"], [pk2])
            ACT(RS[:, 0:n], ps2[:, 0:n], AF.Sqrt, [pk2, "CF"], ["RS"], bias=EPSC, scale=1.0)
            RECIP(RS[:, 0:n], RS[:, 0:n], ["RS"], ["RS"])
            out_fn(cc, t0, n, ps, pk)
        return evac

    def mixer_na(l, ctx_out, G1B, scr):
        ntok = 2304
        QT = scr.get([2, 2304], BF16)
        KT = scr.get([2, 2304], BF16)
        VA = scr.get([18, 4, 65], BF16)
        VS = scr.get([15, 4, 65], BF16)
        DB = scr.get([4, 14, 64], BF16)
        SQ = scr.get([512], BF16)
        RS = scr.get([512], F32)
        GQ = scr.get([2], F32)
        PT = [scr.get([384], BF16) for _ in range(2)]
        PTC = scr.get([2, 256], BF16)
        ON = scr.get([128], F32)
        RZ = scr.get([2], F32)
        ONC = scr.get([128], F32)
        DMA("pool", DB, nab_d[l].rearrange("p (a b c) -> p a b c", a=4, b=14), "nab", (), ["DB"])
        MEMSET(VA[:, :, :, 64:65], 1.0, ["VA"])
        MEMSET(VS[:, :, :, 64:65], 1.0, ["VS"])
        naq = pfv("naq")
        nak = pfv("nak")
        TS(GQ[:, 0:1], naq[:, l:l + 1], 0.125, None, ALU.mult, None, ["PF"], ["GQ"])

        def q_out(cc, t0, n, ps, pk):
            STT(QT[:, cc, t0:t0 + n], ps[:, 0:n], GQ[:, 0:1], RS[:, 0:n], ALU.mult, ALU.mult, [pk, "RS", "GQ"], ["QT"])

        def k_out(cc, t0, n, ps, pk):
            STT(KT[:, cc, t0:t0 + n], ps[:, 0:n], nak[:, l:l + 1], RS[:, 0:n], ALU.mult, ALU.mult, [pk, "RS", "PF"], ["KT"])

        def v_comp(unit, ukey):
            for j in range(18):
                def ev(ps, pk, j=j):
                    CPY(VA[:, j, :, 0:64], ps[:, 0:256].rearrange("p (h d) -> p h d", h=4), [pk], ["VA"], eng="act")
                proj_tm(unit, ukey, j * 128, ev)
            for i in range(15):
                def ev(ps, pk, i=i):
                    CPY(VS[:, i, :, 0:64], ps[:, 0:256].rearrange("p (h d) -> p h d", h=4), [pk], ["VS"], eng="act")
                proj_tm(unit, ukey, 64 + i * 128, ev)

        lq, lk, lv = proj_units(l, 0, 256)[0], proj_units(l, 256, 256)[0], proj_units(l, 512, 256)[0]
        units = [
            (lq, lambda u, uk: proj_fm(u, uk, ntok if ctx_out else L, qk_norm_evac(SQ, RS, None, BD64, q_out))),
            (lk, lambda u, uk: proj_fm(u, uk, ntok, qk_norm_evac(SQ, RS, None, BD64, k_out))),
            (lv, v_comp),
        ]
        run_units(units)
        NPT = 4
        PT = PT + [scr.get([384], BF16) for _ in range(NPT - 2)]
        itc = [0]
        for hp in range(2):
            its = []
            state = {}
            for r in range(32):
                for hh in range(2):
                    def AB(r=r, hh=hh, hp=hp):
                        rs = min(max(r - 4, 0), 24)
                        m0b = 7 - (r - rs)
                        h = hp * 2 + hh
                        b0 = 64 * hh
                        ps, pk = psum("a")
                        q_ap = QT[b0:b0 + 64, hp, r * 64:(r + 1) * 64]
                        for c in range(6):
                            kt0 = (rs + 2 * c) * 64 if c < 4 else 2048 + (c - 4) * 128
                            MM(ps[:, c * 64:(c + 1) * 64], KT[b0:b0 + 64, hp, kt0:kt0 + 128], q_ap, True, True, ["KT", "QT"], [pk])
                        dv = DB[:, h, m0b:m0b + 7:2, :]
                        pv = ps[:, 0:256].rearrange("p (a b) -> p a b", a=4)
                        TT(pv, pv, dv, ALU.add, [pk, "DB"], [pk])
                        i_ = itc[0] % NPT
                        itc[0] += 1
                        ACT(PT[i_], ps[:, 0:384], AF.Exp, [pk], [("PT", i_)])
                        state[(r, hh)] = i_

                    def C(r=r, hh=hh, hp=hp):
                        rs = min(max(r - 4, 0), 24)
                        h = hp * 2 + hh
                        if hh == 0:
                            state[("po", r)] = psum("b")
                        po, pok = state[("po", r)]
                        i_ = state[(r, hh)]
                        pt, ptk = PT[i_], ("PT", i_)
                        for c in range(6):
                            if c < 4:
                                kr = rs + 2 * c
                                vap = VA[:, kr // 2, h, :] if kr % 2 == 0 else VS[:, (kr - 1) // 2, h, :]
                            else:
                                vap = VA[:, 16 + (c - 4), h, :]
                            MM(po[0:64, hh * 65:hh * 65 + 65], pt[:, c * 64:(c + 1) * 64], vap, c == 0, c == 5,
                               [ptk, "VA", "VS"], [pok])
                        if hh == 1:
                            RECIP(RZ[0:64, :], po[0:64, 0:130].rearrange("p (a b) -> p a b", a=2)[:, :, 64], [pok], ["RZ"])
                            for h2 in range(2):
                                TS(ON[0:64, h2 * 64:(h2 + 1) * 64], po[0:64, h2 * 65:h2 * 65 + 64], RZ[0:64, h2:h2 + 1], None, ALU.mult, None,
                                   [pok, "RZ"], ["ON"])
                            pt2, pk2 = psum("c")
                            TR(pt2[:, 0:64], ON[0:64, :], IDF[0:64, 0:64], ["ON", "CF"], [pk2])
                            CPY(YG[:, hp, r * 64:(r + 1) * 64], pt2[:, 0:64], [pk2], [("YG", hp, r // 2)], eng="act")
                    its.append((AB, C))
            run_pipe(its, d=2)
            if ctx_out:
                for qt in range(2):
                    po, pok = psum("b")
                    for hh in range(2):
                        h = hp * 2 + hh
                        b0 = 64 * hh
                        ps, pk = psum("a")
                        for kc in range(2):
                            MM(ps[:, kc * 128:(kc + 1) * 128], KT[b0:b0 + 64, hp, 2048 + kc * 128:2048 + (kc + 1) * 128],
                               QT[b0:b0 + 64, hp, 2048 + qt * 128:2048 + (qt + 1) * 128], True, True, ["KT", "QT"], [pk])
                        ACT(PTC[:, hh, :], ps[:, 0:256], AF.Exp, [pk], [("PTC", hh)])
                        for kc in range(2):
                            MM(po[:, hh * 65:hh * 65 + 65], PTC[:, hh, kc * 128:(kc + 1) * 128], VA[:, 16 + kc, h, :], kc == 0, kc == 1,
                               [("PTC", hh), "VA"], [pok])
                    RECIP(RZ[:, :], po[:, 0:130].rearrange("p (a b) -> p a b", a=2)[:, :, 64], [pok], ["RZ"])
                    for hh in range(2):
                        TS(ONC[:, hh * 64:(hh + 1) * 64], po[:, hh * 65:hh * 65 + 64], RZ[:, hh:hh + 1], None, ALU.mult, None,
                           [pok, "RZ"], ["ONC"])
                    pt2, pk2 = psum("c")
                    TR(pt2[:, 0:128], ONC[:, :], IDF, ["ONC", "CF"], [pk2])
                    CPY(YG[:, hp, 2048 + qt * 128:2048 + (qt + 1) * 128], pt2[:, 0:128], [pk2], [("YG", hp, 16 + qt)], eng="act")
        wout_apply(l, 0, 18 if ctx_out else 16, G1B, scr)


    def mixer_diff(l, ctx_out, G1B, scr):
        lam_init = 0.8 - 0.6 * math.exp(-0.3 * l)
        ROPE = scr.get([2, L], BF16)
        DMA("sp", ROPE, rope_d.rearrange("p (a b) -> p a b", a=2), "rope", (), ["ROPE"])
        LT = scr.get([2, 32], F32)
        for i in range(2):
            TT(LT[:, i, :], LAMV[:, l, 2 * i, :], LAMV[:, l, 2 * i + 1, :], ALU.mult, ["PF"], ["LT"])
            P.op("dve", lambda e, i=i: e.reduce_sum(out=SM[:, i:i + 1], in_=LT[:, i, :], axis=mybir.AxisListType.X), ["LT"], ["SM"])
        ACT(SM[:, 0:2], SM[:, 0:2], AF.Exp, ["SM"], ["SM"])
        TT(SM[:, 2:3], SM[:, 1:2], SM[:, 0:1], ALU.subtract, ["SM"], ["SM"])
        TS(NLAM[:, 0:1], SM[:, 2:3], -lam_init, None, ALU.add, None, ["SM"], ["NLAM"])
        SLG = scr.get([64], F32)
        TS(SLG, SUBLN[:, l, :], 1.0 - lam_init, None, ALU.mult, None, ["PF"], ["SLG"])
        GQ = scr.get([2], F32)
        dfq, dfk = pfv("dfq"), pfv("dfk")
        TS(GQ[:, 0:1], dfq[:, l:l + 1], 32.0 ** -0.5, None, ALU.mult, None, ["PF"], ["GQ"])
        SQ = scr.get([512], BF16)
        QG = scr.get([512], BF16)
        RS = scr.get([512], F32)
        A_ = scr.get([512], F32)
        B_ = scr.get([512], F32)
        QQ = [scr.get([2304], BF16) for _ in range(4)]
        KT = scr.get([2304], BF16)
        VA = scr.get([18, 2, 65], BF16)
        PT = [scr.get([512], BF16) for _ in range(2)]
        OO = scr.get([2, 4, 65], F32)
        RR = scr.get([2, 4], F32)
        TQ = scr.get([64], F32)
        JK = scr.get([64], F32)
        YDT = scr.get([4, 128], F32)
        MEMSET(VA[:, :, :, 64:65], 1.0, ["VA"])
        ntq = 2304 if ctx_out else L
        mark = scr.mark()
        for hp in range(2):
            def prep(gain_ap, gkeys, outs):
                def evac(cc_unused, t0, n, ps, pk):
                    ACT(SQ[:, 0:n], ps[:, 0:n], AF.Square, [pk], ["SQ"])
                    ACT(QG[:, 0:n], ps[:, 0:n], AF.Identity, [pk] + gkeys, ["QG"], scale=gain_ap)
                    ps2, pk2 = psum("c")
                    MM(ps2[:, 0:n], BD32, SQ[:, 0:n], True, True, ["SQ", "CB"], [pk2])
                    ACT(RS[:, 0:n], ps2[:, 0:n], AF.Sqrt, [pk2, "CF"], ["RS"], bias=EPSC, scale=1.0)
                    RECIP(RS[:, 0:n], RS[:, 0:n], ["RS"], ["RS"])
                    if t0 < L:
                        ps3, pk3 = psum("c")
                        MM(ps3[:, 0:n], PERM, QG[:, 0:n], True, True, ["QG", "CB"], [pk3])
                        TT(A_[:, 0:n], QG[:, 0:n], ROPE[:, 0, t0:t0 + n], ALU.mult, ["QG", "ROPE"], ["A"])
                        TT(B_[:, 0:n], ps3[:, 0:n], ROPE[:, 1, t0:t0 + n], ALU.mult, [pk3, "ROPE"], ["B"])
                        TT(A_[:, 0:n], A_[:, 0:n], B_[:, 0:n], ALU.add, ["A", "B"], ["A"], eng="pool")
                        src_ap = A_
                        sk = "A"
                    else:
                        src_ap = QG
                        sk = "QG"
                    for (dst, dk, mk) in outs:
                        if mk is None:
                            TT(dst[:, t0:t0 + n], src_ap[:, 0:n], RS[:, 0:n], ALU.mult, [sk, "RS"], [dk])
                        else:
                            STT(dst[:, t0:t0 + n], src_ap[:, 0:n], mk, RS[:, 0:n], ALU.mult, ALU.mult, [sk, "RS", "CF"], [dk])
                return evac

            def one_chunk(unit, ukey, ntok, evac, cc):
                for (t0, n) in tok_blocks(ntok):
                    ps, pk = psum("a")
                    for k in range(8):
                        MM(ps[:, 0:n], unit[:, k, cc * 128:(cc + 1) * 128], HT[:, k, t0:t0 + n], k == 0, k == 7,
                           [ukey] + tkeys("HT", k, t0, n), [pk])
                    evac(cc, t0, n, ps, pk)

            def v_comp(unit, ukey, hp=hp):
                for j in range(18):
                    def ev(ps, pk, j=j):
                        CPY(VA[:, j, :, 0:64], ps[:, hp * 128:(hp + 1) * 128].rearrange("p (h d) -> p h d", h=2), [pk], ["VA"], eng="act")
                    proj_tm(unit, ukey, j * 128, ev)

            units = [
                (proj_units(l, 768, 256)[0], lambda u, uk, hp=hp: one_chunk(u, uk, ntq, prep(GQ[:, 0:1], ["GQ"], [(QQ[j_], ("QQ", j_), MASK4[:, j_:j_ + 1]) for j_ in range(4)]), hp)),
                (proj_units(l, 1024, 256)[0], lambda u, uk, hp=hp: one_chunk(u, uk, 2304, prep(dfk[:, l:l + 1], ["PF"], [(KT, "KT", None)]), hp)),
                (proj_units(l, 1280, 256)[0], v_comp),
            ]
            run_units(units)
            NPT = 4
            if hp == 0:
                PT = PT + [scr.get([512], BF16) for _ in range(NPT - 2)]
            itc = [0]
            state = {}
            its = []
            qblocks = [(qb * 512, 512, list(range(16, 18)) + list(range(16))) for qb in range(4)]
            if ctx_out:
                qblocks.append((2048, 256, [16, 17]))
            for (q0, qn, kcs) in qblocks:
                nqt = qn // 128
                for hh in range(2):
                    for sub in range(2):
                        for ci, kc in enumerate(kcs):
                            def AB(q0=q0, qn=qn, hh=hh, sub=sub, kc=kc, ci=ci):
                                Qs, qk_ = QQ[2 * hh + sub], ("QQ", 2 * hh + sub)
                                ps, pk = psum("a")
                                MM(ps[:, 0:qn], KT[:, kc * 128:(kc + 1) * 128], Qs[:, q0:q0 + qn], True, True, ["KT", qk_], [pk])
                                i_ = itc[0] % NPT
                                itc[0] += 1
                                ACT(PT[i_][:, 0:qn], ps[:, 0:qn], AF.Exp, [pk], [("PT", i_)])
                                state[(q0, hh, sub, ci)] = i_

                            def C(q0=q0, qn=qn, nqt=nqt, hh=hh, sub=sub, kc=kc, ci=ci, nk=len(kcs), hp=hp):
                                if ci == 0:
                                    state[("po", q0, hh, sub)] = psum("b")
                                po, pok = state[("po", q0, hh, sub)]
                                i_ = state[(q0, hh, sub, ci)]
                                pt, ptk = PT[i_], ("PT", i_)
                                for qt in range(nqt):
                                    MM(po[:, qt * 65:qt * 65 + 65], pt[:, qt * 128:(qt + 1) * 128], VA[:, kc, hh, :], ci == 0 and qt == 0,
                                       ci == nk - 1, [ptk, "VA"], [pok], skip=True)
                                if ci != nk - 1:
                                    return
                                CPY(OO[:, sub, 0:nqt, :], po[:, 0:nqt * 65].rearrange("p (a b) -> p a b", a=nqt), [pok], [("OO", sub)], eng="act")
                                if sub != 1:
                                    return
                                RECIP(RR[:, :, 0:nqt], OO[:, :, 0:nqt, 64], [("OO", 0), ("OO", 1)], ["RR"])
                                TS(RR[:, 1, 0:nqt], RR[:, 1, 0:nqt], NLAM[:, 0:1], None, ALU.mult, None, ["RR", "NLAM"], ["RR"])
                                for qt in range(nqt):
                                    TS(TQ, OO[:, 0, qt, 0:64], RR[:, 0, qt:qt + 1], None, ALU.mult, None, [("OO", 0), "RR"], ["TQ"])
                                    STT(TQ, OO[:, 1, qt, 0:64], RR[:, 1, qt:qt + 1], TQ, ALU.mult, ALU.add, [("OO", 1), "RR", "TQ"], ["TQ"])
                                    MEMSET(SM[:, 4:5], 0.0, ["SM4"], eng="dve")
                                    ACT(JK, TQ, AF.Square, ["TQ", "SM4"], ["JK", "SM4"], accum=SM[:, 4:5])
                                    ACT(SM[:, 5:6], SM[:, 4:5], AF.Sqrt, ["SM4", "CF"], ["SM5"], bias=EPSC, scale=1.0 / 64)
                                    RECIP(SM[:, 5:6], SM[:, 5:6], ["SM5"], ["SM5"])
                                    STT(YDT[:, qt, hh * 64:(hh + 1) * 64], TQ, SM[:, 5:6], SLG, ALU.mult, ALU.mult, ["TQ", "SM5", "SLG"], [("YDT", qt)])
                                if hh != 1:
                                    return
                                for qt in range(nqt):
                                    pt2, pk2 = psum("c")
                                    TR(pt2[:, 0:128], YDT[:, qt, :], IDF, [("YDT", qt), "CF"], [pk2])
                                    tt = q0 // 128 + qt
                                    CPY(YG[:, hp, tt * 128:(tt + 1) * 128], pt2[:, 0:128], [pk2], [("YG", hp, tt)], eng="act")
                            its.append((AB, C))
            run_pipe(its, d=2)
        wout_apply(l, 1, 18 if ctx_out else 16, G1B, scr)

    def mixer_pool(l, ctx_out, G1B, scr):
        ntiles = 18 if ctx_out else 16
        ntok = ntiles * 128
        TTt = scr.get([2, 2304], BF16)
        ZP = scr.get([18, 4, 128], BF16)
        PB = scr.get([4, 5, 128], BF16)
        PW = scr.get([4, 128], BF16)
        DMA("sp", PB, pband_d.rearrange("p (a b c) -> p a b c", a=4, b=5), "pband", (), ["PB"])
        DMA("pool", PW, poolw_d[l].rearrange("p (a b) -> p a b", a=4), "poolw", (), ["PW"])

        def t_evac(cc, t0, n, ps, pk):
            CPY(TTt[:, cc, t0:t0 + n], ps[:, 0:n], [pk], [("TT", cc)], eng="act")

        run_units([(proj_units(l, 1536, 256)[0], lambda u, uk: proj_fm(u, uk, ntok, t_evac))])
        for j in range(ntiles):
            ps, pk = psum("a")
            for g in range(4):
                MM(ps[:, g * 128:(g + 1) * 128], TTt[:, g // 2, j * 128:(j + 1) * 128], PW[:, g, :], True, True, [("TT", g // 2), "PW"], [pk])
            CPY(ZP[:, j, :, :], ps[:, :].rearrange("p (a b) -> p a b", a=4), [pk], [("ZP", j)], eng="dve")
        seqs = [(0, 16)] + ([(16, 2)] if ctx_out else [])
        for (j0, nt) in seqs:
            for jo in range(nt):
                for pc in range(2):
                    terms = []
                    for g in (2 * pc, 2 * pc + 1):
                        if jo > 0:
                            terms.append((j0 + jo - 1, g, 0))
                        terms.append((j0 + jo, g, 3 if jo == 0 else (4 if jo == nt - 1 else 1)))
                        if jo < nt - 1:
                            terms.append((j0 + jo + 1, g, 2))
                    ps, pk = psum("a")
                    for i, (ji, g, kind) in enumerate(terms):
                        MM(ps[:, 0:128], ZP[:, ji, g, :], PB[:, g, kind, :], i == 0, i == len(terms) - 1, [("ZP", ji), "PB"], [pk])
                    j = j0 + jo
                    ACT(YG[:, pc, j * 128:(j + 1) * 128], ps[:, 0:128], AF.Identity, [pk, "PF"], [("YG", pc, j)], scale=PSCALE[:, l, pc:pc + 1])
        wout_apply(l, 2, ntiles, G1B, scr)

    def mixer_fft(l, ctx_out, G1B, scr):
        ntiles = 18 if ctx_out else 16
        ntok = ntiles * 128
        TTt = scr.get([2, 2304], BF16)
        TCS = scr.get([18, 4, 128], BF16)
        FW = scr.get([4, 128], F32, parts=64)
        WCS = scr.get([4, 128], BF16)
        DMA("sp", FW, fftw_d[l].rearrange("p (a b) -> p a b", a=4), "fftw", (), ["FW"])
        CS64P = scr.get([2, 2, 128], F32, parts=64)
        DMA("sp", CS64P, cs64p_d.rearrange("p (a b c) -> p a b c", a=2, b=2), "cs64p", (), ["CS64P"])
        T256 = scr.get([2, 2, 256], BF16)
        DMA("sp", T256, t256_d.rearrange("p (a b c) -> p a b c", a=2, b=2), "t256", (), ["T256"])
        C256 = T256[:, 0, :, :]
        S256 = T256[:, 1, :, :]
        for pc in range(2):
            for cs in range(2):
                ps, pk = psum("c")
                for pos in range(2):
                    MM(ps[:, 0:128], CS64P[:, cs, pos, :], FW[:, 2 * pc + pos, :], pos == 0, pos == 1, ["CS64P", "FW"], [pk])
                CPY(WCS[:, pc * 2 + cs, :], ps[:, 0:128], [pk], ["WCS"], eng="act")

        def t_evac(cc, t0, n, ps, pk):
            CPY(TTt[:, cc, t0:t0 + n], ps[:, 0:n], [pk], [("TT", cc)], eng="act")

        run_units([(proj_units(l, 1792, 256)[0], lambda u, uk: proj_fm(u, uk, ntok, t_evac))])
        for j in range(ntiles):
            ps, pk = psum("a")
            for q in range(4):
                MM(ps[:, q * 128:(q + 1) * 128], TTt[:, q // 2, j * 128:(j + 1) * 128], WCS[:, q, :], True, True, [("TT", q // 2), "WCS"], [pk])
            CPY(TCS[:, j, :, :], ps[:, :].rearrange("p (a b) -> p a b", a=4), [pk], ["TCS"], eng="dve")
        units = []
        for kb in range(16):
            def ld(kb=kb):
                c_, ck_ = ring_load([16, 128], BF16, cl_d[kb].rearrange("p (a b) -> p a b", a=16))
                s_, sk_ = ring_load([16, 128], BF16, sl_d[kb].rearrange("p (a b) -> p a b", a=16))
                return (c_, s_), (ck_, sk_)

            def comp(tabs, keys, kb=kb):
                for pc in range(2):
                    ps, pk = psum("a")
                    n = 0
                    for cs in range(2):
                        for lt in range(16):
                            MM(ps[:, 0:128], TCS[:, lt, pc * 2 + cs, :], tabs[cs][:, lt, :], n == 0, n == 31, ["TCS", keys[cs]], [pk])
                            n += 1
                    CPY(YG[:, pc, kb * 128:(kb + 1) * 128], ps[:, 0:128], [pk], [("YG", pc, kb)], eng="act")
            units.append((ld, comp))
        run_units(units, depth=2)
        if ctx_out:
            for pc in range(2):
                ps, pk = psum("a")
                n = 0
                for cs in range(2):
                    tab = C256 if cs == 0 else S256
                    for lt in range(2):
                        MM(ps[:, 0:256], TCS[:, 16 + lt, pc * 2 + cs, :], tab[:, lt, :], n == 0, n == 3, ["TCS", "T256"], [pk])
                        n += 1
                CPY(YG[:, pc, 2048:2304], ps[:, 0:256], [pk], [("YG", pc, 16), ("YG", pc, 17)], eng="act")
        wout_apply(l, 3, ntiles, G1B, scr)

    def moe_phase(l, ctx_out, scr):
        ntiles = 18 if ctx_out else 16
        ntok = ntiles * 128
        G2B = scr.get([2, D], F32)
        BT = scr.get([128], F32)
        for src in range(2 if ctx_out else 1):
            bcast_vec(G2B[:, src, :], l, 5, src, BT)
        WR = scr.get([8, 32], F32)
        WRS = scr.get([2, 8, 32], F32)
        CROW = scr.get([2, 32], F32, parts=1)
        DMA("sp", WR, rw_d[l].rearrange("p (a b) -> p a b", a=8), "rw", (), ["WR"])
        B1T = scr.get([NE, 8, 2], F32)
        DMA("sp", B1T, b1t_d[l].rearrange("p (a b c) -> p a b c", a=NE, b=8), "b1t", (), ["B1T"])
        B2 = scr.get([D], F32, parts=NE)
        DMA("sp", B2, b2_d[l], "b2", (), ["B2"])
        TS(B1T[:, :, :, 1], B1T[:, :, :, 1], 1.0, None, ALU.add, None, ["B1T"], ["B1T"])
        G = scr.get([18, 32], F32)
        GA = scr.get([18, 32], F32)
        moe_mark = scr.mark()
        XNT = scr.get([8, 128], F32)
        LG = scr.get([32], F32)
        T8 = scr.get([8], F32)
        EX = scr.get([32], F32)
        MK = scr.get([32], F32)
        GT = scr.get([128], F32, parts=NE)
        def router(j, src, pss):
            for c in range(8):
                ps, pk = pss[c // 4]
                CPY(XNT[:, c, :], ps[:, (c % 4) * 128:(c % 4 + 1) * 128], [pk], ["XNT"], eng="dve")
            pl, plk = psum("c")
            for c in range(8):
                MM(pl[:, 0:32], XNT[:, c, :], WRS[:, src, c, :], c == 0, False, ["XNT", "WRS"], [plk])
            MM(pl[:, 0:32], ONESF[0:1, :], CROW[0:1, src, :], False, True, ["CF", "CROW"], [plk])
            CPY(LG, pl[:, 0:32], [plk], ["LG"], eng="dve")
            P.op("dve", lambda e: e.max(out=T8, in_=LG), ["LG"], ["T8"])
            TS(MK, LG, T8[:, 3:4], None, ALU.is_ge, None, ["LG", "T8"], ["MK"])
            TS(SM[:, 6:7], T8[:, 0:1], -1.0, None, ALU.mult, None, ["T8"], ["SM6"])
            ACT(EX, LG, AF.Exp, ["LG", "SM6"], ["EX"], bias=SM[:, 6:7], scale=1.0)
            TT(EX, EX, MK, ALU.mult, ["EX", "MK"], ["EX"])
            P.op("dve", lambda e: e.reduce_sum(out=SM[:, 7:8], in_=EX, axis=mybir.AxisListType.X), ["EX"], ["SM7"])
            RECIP(SM[:, 7:8], SM[:, 7:8], ["SM7"], ["SM7"])
            TS(G[:, j, :], EX, SM[:, 7:8], None, ALU.mult, None, ["EX", "SM7"], [("G", j)])
            TS(GA[:, j, :], G[:, j, :], 1.0 / ALPHA, None, ALU.mult, None, [("G", j)], [("GA", j)])

        def pre_router():
            for src in range(2 if ctx_out else 1):
                for c in range(8):
                    TS(WRS[:, src, c, :], WR[:, c, :], GS[:, c, src:src + 1], None, ALU.mult, None, ["WR", "GS"], ["WRS"])
                pc_, pck = psum("c")
                for c in range(8):
                    MM(pc_[0:1, 0:32], mod_ap(l, 3, c, src), WR[:, c, :], c == 0, c == 7, [("MOD", l), "WR"], [pck])
                TT(CROW[0:1, src, :], pc_[0:1, 0:32], RB[0:1, l, :], ALU.add, [pck, "PF"], ["CROW"])

        norm_phase(l, 3, G2T, list(range(ntiles)), scr, router, pre_router)

        TMPB = scr.get([512], F32)
        for j in range(ntiles):
            src = 0 if j < 16 else 1
            pt_, ptk_ = psum("c")
            TR(pt_[0:32, 0:128], G[:, j, :], IDF, [("G", j), "CF"], [ptk_])
            CPY(GT[:, :], pt_[0:32, 0:128], [ptk_], ["GT"], eng="act")
            for fb in range(2):
                ps, pk = psum("a")
                MM(ps[:, :], GT[:, :], B2[:, fb * 512:(fb + 1) * 512], True, True, ["GT", "B2"], [pk])
                TT(TMPB, ps[:, :], G2B[:, src, fb * 512:(fb + 1) * 512], ALU.mult, [pk, "GB"], ["TMPB"])
                TT(X[:, j, fb * 512:(fb + 1) * 512], X[:, j, fb * 512:(fb + 1) * 512], TMPB, ALU.add, [("X", j), "TMPB"], [("X", j)], eng="pool")

        P.barrier()
        scr.reset(moe_mark)
        NB = 2
        GC = [scr.get([512], F32) for _ in range(NB)]
        SI = [scr.get([512], F32) for _ in range(NB)]
        L1 = [scr.get([512], F32) for _ in range(NB)]
        TO = [scr.get([256], F32) for _ in range(NB)]
        blocks = tok_blocks(ntok)
        units = []
        cnt = [0, 0]
        for e in range(n_exp):
            w1v = w1_d[l, e].rearrange("(k p) n -> p k n", p=128)
            w2v = w2_d[l, e].rearrange("(k p) n -> p k n", p=128)
            for p in range(8):
                def ld(p=p, w1v=w1v):
                    return ring_load([8, 256], BF16, w1v[:, :, p * 256:(p + 1) * 256])

                def comp(unit, ukey, e=e, p=p):
                    uv = unit.rearrange("p k (f two) -> p k two f", two=2)
                    for (t0, n) in blocks:
                        pg, pgk = psum("a")
                        pl, plk = psum("a")
                        for k in range(8):
                            MM(pg[:, 0:n], uv[:, k, 0, :], HT[:, k, t0:t0 + n], k == 0, k == 7, [ukey] + tkeys("HT", k, t0, n), [pgk])
                        for k in range(8):
                            MM(pl[:, 0:n], uv[:, k, 1, :], HT[:, k, t0:t0 + n], k == 0, k == 7, [ukey] + tkeys("HT", k, t0, n), [plk])
                        i = cnt[0] % NB
                        cnt[0] += 1
                        TS(GC[i][:, 0:n], pg[:, 0:n], B1T[:, e, p, 0:1], 7.0, ALU.add, ALU.min, [pgk, "B1T"], [("GC", i)])
                        ACT(SI[i][:, 0:n], GC[i][:, 0:n], AF.Silu, [("GC", i)], [("SI", i)], scale=ALPHA)
                        TS(L1[i][:, 0:n], pl[:, 0:n], B1T[:, e, p, 1:2], -6.0, ALU.add, ALU.max, [plk, "B1T"], [("L1", i)])
                        STT(ACTB[:, p, t0:t0 + n], L1[i][:, 0:n], 8.0, SI[i][:, 0:n], ALU.min, ALU.mult, [("L1", i), ("SI", i)],
                            [("ACTB", p, t0 // 512)])
                units.append((ld, comp))
            for q in range(4):
                def ld(q=q, w2v=w2v):
                    return ring_load([8, 256], BF16, w2v[:, :, q * 256:(q + 1) * 256])

                def comp(unit, ukey, e=e, q=q):
                    for j in range(ntiles):
                        src = 0 if j < 16 else 1
                        po, pok = psum("b")
                        for k in range(8):
                            MM(po[:, 0:256], ACTB[:, k, j * 128:(j + 1) * 128], unit[:, k, :], k == 0, k == 7, [ukey, ("ACTB", k, j // 4)], [pok])
                        i = cnt[1] % NB
                        cnt[1] += 1
                        STT(TO[i], po[:, 0:256], GA[:, j, e:e + 1], G2B[:, src, q * 256:(q + 1) * 256], ALU.mult, ALU.mult,
                            [pok, ("GA", j), "GB"], [("TO", i)])
                        TT(X[:, j, q * 256:(q + 1) * 256], X[:, j, q * 256:(q + 1) * 256], TO[i], ALU.add, [("X", j), ("X", j, q), ("TO", i)], [("X", j, q)],
                           eng="pool")
                units.append((ld, comp))
        psr["b"] = (4, 4)
        run_units(units, depth=3)
        psr["b"] = (4, 2)

    for l in range(nlayers):
        ctx_out = l < nlayers - 1
        ntiles_all = 18
        scr = Bump([(OFF_B + 9216, 36864 - 9216), (OFF_S, S_SZ)])
        G1B = scr.get([2, D], F32)
        BT = scr.get([128], F32)
        base_mark = scr.mark()
        norm_phase(l, 0, G1T, list(range(18)), scr)
        for src in range(2 if ctx_out else 1):
            bcast_vec(G1B[:, src, :], l, 2, src, BT)
        P.barrier()
        for mi, fn in enumerate((mixer_na, mixer_diff, mixer_pool, mixer_fft)):
            if mi not in mixers:
                continue
            scr.reset(base_mark)
            fn(l, ctx_out, G1B, scr)
            P.barrier()
        if stop == ("mix", l):
            break
        if do_moe:
            scr = Bump([(OFF_S, S_SZ)])
            moe_phase(l, ctx_out, scr)
            P.barrier()
        if stop == ("moe", l):
            break

    for j in range(16):
        DMA("sp", out_d[j * 128:(j + 1) * 128, :], X[:, j, :], "st", [("X", j)] + [("X", j, q) for q in range(4)], [])
    if stop is not None:
        dbg = nc.dram_tensor("dbgc", [LC, D], F32, kind="ExternalOutput").ap()
        for j in range(2):
            DMA("sp", dbg[j * 128:(j + 1) * 128, :], X[:, 16 + j, :], "st", [("X", 16 + j)] + [("X", 16 + j, q) for q in range(4)], [])
    P.emit(nc, final_waits=["st"])
    return nc, len(P.ops)


def make_in_maps(inp, nlayers=2, cores=range(8)):
    f = lambda a: np.ascontiguousarray(np.asarray(a, np.float32))
    cst = host_constants()
    lay = host_layouts(inp, nlayers)
    shared = dict(cb=cst["cb"], cf=cst["cf"], t256=cst["t256"], cs64p=cst["cs64p"], rope=cst["rope"], pband=cst["pband"], cl=cst["cl"], sl=cst["sl"],
                  wada=f(inp["w_ada"]), win=f(inp["w_in"]), wout=f(inp["w_out"]),
                  nab=lay["nab"], poolw=lay["poolw"], fftw=lay["fftw"], rw=lay["rw"], b1t=lay["b1t"], b2=lay["b2"],
                  w1=f(inp["moe_w1"]), w2=f(inp["moe_w2"]))
    x = f(inp["x"])
    cx = f(inp["ctx"])
    maps = []
    for b in cores:
        m = dict(shared)
        m["x"] = x[b]
        m["cx"] = cx[b]
        m["pf"] = host_pf(inp, b, nlayers)
        maps.append(m)
    return maps


_NC_CACHE = {}


def kernel(**inputs):
    if "nc" not in _NC_CACHE:
        _NC_CACHE["nc"] = build_nc()[0]
    nc = _NC_CACHE["nc"]
    maps = make_in_maps(inputs)
    res = run_bass_kernel_spmd(nc, maps, core_ids=list(range(8)))
    return np.stack([np.asarray(r["out"], np.float32) for r in res.results], axis=0)
```

```python
import math
import numpy as np
import ml_dtypes
import concourse.bass as bass
import concourse.mybir as mybir
from concourse.bass_utils import run_bass_kernel_spmd

F32 = mybir.dt.float32
BF16 = mybir.dt.bfloat16
ALU = mybir.AluOpType
AF = mybir.ActivationFunctionType
ENGS = ("pe", "act", "dve", "pool", "sp")

D = 1024
L = 2048
LC = 256
NE = 32
ALPHA = 1.702
EPS = 1e-6
MASKV = -30000.0


class Op:
    __slots__ = ("eng", "fn", "reads", "writes", "dma", "waits", "signal", "sig_idx", "dma_val", "deps")

    def __init__(self, eng, fn, reads, writes, dma):
        self.eng = eng
        self.fn = fn
        self.reads = reads
        self.writes = writes
        self.dma = dma
        self.waits = []
        self.signal = False
        self.sig_idx = 0
        self.dma_val = 0
        self.deps = None


class Prog:
    def __init__(self):
        self.ops = []
        self.last_w = {}
        self.readers = {}
        self.dma_counts = {}
        self.group_streams = set()
        self.pending_barrier = {}

    def op(self, eng, fn, reads=(), writes=(), dma=None):
        o = Op(eng, fn, tuple(reads), tuple(writes), dma)
        idx = len(self.ops)
        deps = set()
        for k in o.reads:
            w = self.last_w.get(k)
            if w is not None:
                deps.add(w)
        for k in o.writes:
            w = self.last_w.get(k)
            if w is not None:
                deps.add(w)
            rs = self.readers.get(k)
            if rs:
                deps.update(rs)
        for k in o.reads:
            self.readers.setdefault(k, []).append(idx)
        for k in o.writes:
            self.last_w[k] = idx
            self.readers[k] = []
        if eng in self.pending_barrier:
            deps.update(self.pending_barrier.pop(eng))
        if dma is not None:
            self.dma_counts[dma] = self.dma_counts.get(dma, 0) + 16
            o.dma_val = self.dma_counts[dma]
        o.deps = deps
        self.ops.append(o)
        return idx

    def barrier(self):
        last = {}
        for i, o in enumerate(self.ops):
            last[o.eng] = i
        lastd = {}
        for i, o in enumerate(self.ops):
            if o.dma is not None:
                lastd[o.dma] = i
        s = set(last.values()) | set(lastd.values())
        for e in ENGS:
            self.pending_barrier[e] = set(s) | self.pending_barrier.get(e, set())

    def finalize(self):
        need = {}
        for ci, c in enumerate(self.ops):
            for pi in c.deps:
                p = self.ops[pi]
                if p.dma is None:
                    if p.eng == c.eng and p.eng in ("pe", "sp"):
                        continue
                    p.signal = True
                need.setdefault(ci, []).append(pi)
        cnt = {e: 0 for e in ENGS}
        for o in self.ops:
            if o.signal:
                cnt[o.eng] += 1
                o.sig_idx = cnt[o.eng]
        waited = {e: {} for e in ENGS}
        for ci, c in enumerate(self.ops):
            ws = {}
            for pi in need.get(ci, ()):
                p = self.ops[pi]
                if p.dma is not None:
                    key = ("dma", p.dma)
                    val = self.dma_counts[p.dma] if p.dma in self.group_streams else p.dma_val
                else:
                    key, val = ("eng", p.eng), p.sig_idx
                if ws.get(key, 0) < val:
                    ws[key] = val
            wd = waited[c.eng]
            for key, val in ws.items():
                if wd.get(key, 0) >= val:
                    continue
                wd[key] = val
                c.waits.append((key, val))

    def emit(self, nc, final_waits=()):
        import contextlib
        self.finalize()
        with contextlib.ExitStack() as es:
            sems = {}
            for e in ENGS:
                sems[("eng", e)] = es.enter_context(nc.semaphore("s_" + e))
            for d in self.dma_counts:
                sems[("dma", d)] = es.enter_context(nc.semaphore("d_" + d))
            block = es.enter_context(nc.Block())
            ops = self.ops
            counts = self.dma_counts

            def body(engname):
                def run(eng):
                    for o in ops:
                        if o.eng != engname:
                            continue
                        for key, val in o.waits:
                            eng.wait_ge(sems[key], val)
                        ins = o.fn(eng)
                        if o.dma is not None:
                            ins.then_inc(sems[("dma", o.dma)], 16)
                        elif o.signal:
                            ins.then_inc(sems[("eng", engname)], 1)
                    if engname == "sp":
                        for d in final_waits:
                            eng.wait_ge(sems[("dma", d)], counts[d])
                return run

            block.tensor(body("pe"))
            block.scalar(body("act"))
            block.vector(body("dve"))
            block.gpsimd(body("pool"))
            block.sync(body("sp"))


def _bf(a):
    return np.ascontiguousarray(a.astype(ml_dtypes.bfloat16))


CB_OFF = {}
CF_OFF = {}
PF_OFF = {}


def _layout(offs, items):
    o = 0
    for name, n in items:
        offs[name] = (o, n)
        o += n
    return o


NCB = _layout(CB_OFF, [("ident", 128), ("bd64", 128), ("bd32", 128), ("perm", 128)])
NCF = _layout(CF_OFF, [("identf", 128), ("onesf", 128), ("eps", 1), ("mask12", 2), ("seven", 1), ("mask4", 4)])
NPF = _layout(PF_OFF, [("cvec", 16), ("bada", 96), ("g1", 16), ("g2", 16), ("naq", 2), ("nak", 2), ("dfq", 2),
                       ("dfk", 2), ("lam", 256), ("subln", 128), ("pscale", 4), ("rb", 64)])

_CONST_CACHE = {}


def host_constants():
    if _CONST_CACHE:
        return _CONST_CACHE
    p = np.arange(128)
    cb = np.zeros((128, NCB), np.float32)
    cb[:, 0:128] = np.eye(128)
    cb[:, 128:256] = (p[:, None] // 64 == p[None, :] // 64) / 64.0
    cb[:, 256:384] = (p[:, None] // 32 == p[None, :] // 32) / 32.0
    partner = np.where((p % 16) < 8, p + 8, p - 8)
    perm = np.zeros((128, 128), np.float32)
    perm[partner, p] = 1.0
    cb[:, 384:512] = perm
    lt = np.arange(2)[None, :, None]
    lin = p[:, None, None]
    k = np.arange(256)[None, None, :]
    ang = 2 * np.pi * ((lt * 128 + lin) * k % 256) / 256.0
    t256 = np.zeros((128, 1024), np.float32)
    t256[:, 0:512] = (np.cos(ang) / 16.0).reshape(128, 512)
    t256[:, 512:1024] = (-np.sin(ang) / 16.0).reshape(128, 512)
    cf = np.zeros((128, NCF), np.float32)
    cf[:, 0:128] = np.eye(128)
    cf[:, 128:256] = 1.0
    cf[:, 256] = EPS
    cf[:, 257] = (p % 64 < 32)
    cf[:, 258] = (p % 64 >= 32)
    m = np.arange(64)[:, None]
    c = np.arange(64)[None, :]
    a64 = 2 * np.pi * (m * c % 64) / 64.0
    cs = np.zeros((64, 2, 2, 128), np.float32)
    cs[:, 0, 0, 0:64] = np.cos(a64) / 8.0
    cs[:, 0, 1, 64:128] = np.cos(a64) / 8.0
    cs[:, 1, 0, 0:64] = np.sin(a64) / 8.0
    cs[:, 1, 1, 64:128] = np.sin(a64) / 8.0
    cf[:, 259] = 7.0
    for j_ in range(4):
        cf[:, 260 + j_] = (p // 32 == j_)
    d = p % 32
    seg = d // 16
    i = d % 16
    j = i % 8
    inv = 10000.0 ** (-(2.0 * j) / 16.0)
    t = np.arange(L)
    pos = np.where(seg[:, None] == 0, (t // 64)[None, :], (t % 64)[None, :]).astype(np.float64)
    angr = pos * inv[:, None]
    rope = np.zeros((128, 2, L), np.float32)
    rope[:, 0, :] = np.cos(angr)
    rope[:, 1, :] = np.where((i < 8)[:, None], -np.sin(angr), np.sin(angr))
    pband = np.zeros((128, 4, 5, 128), np.float32)
    Lp = 512
    posp = np.arange(Lp)
    for g, win in enumerate((2, 4, 8, 16)):
        lo = np.clip(posp - win // 2, 0, Lp)
        hi = np.clip(posp - win // 2 + win, 0, Lp)
        M = np.zeros((Lp, Lp), np.float64)
        for o in range(Lp):
            M[o, lo[o]:hi[o]] = 1.0 / (hi[o] - lo[o])
        M -= np.eye(Lp)
        pband[:, g, 0, :] = M[128:256, 0:128].T
        pband[:, g, 1, :] = M[128:256, 128:256].T
        pband[:, g, 2, :] = M[128:256, 256:384].T
        pband[:, g, 3, :] = M[0:128, 0:128].T
        pband[:, g, 4, :] = M[384:512, 384:512].T
    kb = np.arange(16)[:, None, None, None]
    lin4 = np.arange(128)[None, :, None, None]
    lt4 = np.arange(16)[None, None, :, None]
    kk = np.arange(128)[None, None, None, :]
    prod = ((lt4 * 128 + lin4) * (kb * 128 + kk)) % L
    angL = 2 * np.pi * prod / float(L)
    s = 1.0 / math.sqrt(L)
    cl = (np.cos(angL) * s).reshape(16, 128, 2048)
    sl = (-np.sin(angL) * s).reshape(16, 128, 2048)
    _CONST_CACHE.update(dict(cb=_bf(cb), cf=cf, t256=_bf(t256), cs64p=np.ascontiguousarray(cs.reshape(64, 512)), rope=_bf(rope.reshape(128, 2 * L)),
                             pband=_bf(pband.reshape(128, 4 * 5 * 128)), cl=_bf(cl), sl=_bf(sl)))
    return _CONST_CACHE


def host_layouts(inp, nlayers=2):
    f = lambda a: np.asarray(a, np.float32)
    out = {}
    p = np.arange(128)
    rpb = f(inp["na_rpb"])
    ck = np.arange(64)[:, None]
    cq = np.arange(64)[None, :]
    col_start = np.clip(np.arange(64) - 8, 0, 48)
    inwin = (ck >= col_start[None, :]) & (ck < col_start[None, :] + 16)
    relc = np.clip(ck - cq, -15, 15) + 15
    nab = np.full((nlayers, 2, 64, 4, 14, 64), MASKV, np.float32)
    for l in range(nlayers):
        for h in range(4):
            for m0 in range(14):
                for jj in range(2):
                    blk = rpb[l, h, m0 + jj][relc]
                    nab[l, jj, :, h, m0, :] = np.where(inwin, blk, MASKV)
    out["nab"] = nab.reshape(nlayers, 128, 4 * 14 * 64)
    pw = f(inp["pool_w"])
    poolw = np.zeros((nlayers, 128, 4, 128), np.float32)
    fw = f(inp["fft_w"])
    fftw = np.zeros((nlayers, 64, 4, 128), np.float32)
    for l in range(nlayers):
        for g in range(4):
            o = (g % 2) * 64
            poolw[l, o:o + 64, g, o:o + 64] = pw[l, g]
            fftw[l, :, g, o:o + 64] = fw[l, g]
    out["poolw"] = poolw.reshape(nlayers, 128, 512)
    out["fftw"] = fftw.reshape(nlayers, 64, 512)
    rw = f(inp["router_w"])
    out["rw"] = np.ascontiguousarray(rw.reshape(nlayers, 8, 128, 32).transpose(0, 2, 1, 3)).reshape(nlayers, 128, 256)
    b1 = f(inp["moe_b1"])
    b1t = b1.reshape(nlayers, NE, 8, 128, 2).transpose(0, 3, 1, 2, 4)
    out["b1t"] = np.ascontiguousarray(b1t).reshape(nlayers, 128, NE * 16)
    out["b2"] = np.ascontiguousarray(f(inp["moe_b2"]))
    return out


def host_pf(inp, b, nlayers=2):
    f = lambda a: np.asarray(a, np.float32)
    p = np.arange(128)
    pf = np.zeros((128, NPF), np.float32)

    def put(name, arr):
        o, n = PF_OFF[name]
        pf[:, o:o + n] = arr.reshape(128, n)

    cv = np.zeros((128, 8, 2), np.float32)
    cv[:, :, 0] = f(inp["c"])[b].reshape(8, 128).T
    cv[:, :, 1] = f(inp["c_ctx"]).reshape(8, 128).T
    put("cvec", cv)
    put("bada", f(inp["b_ada"]).reshape(nlayers, 48, 128).transpose(2, 0, 1))
    put("g1", f(inp["g_norm1"]).reshape(nlayers, 8, 128).transpose(2, 0, 1))
    put("g2", f(inp["g_norm2"]).reshape(nlayers, 8, 128).transpose(2, 0, 1))
    put("naq", f(inp["na_q_gain"])[:, p % 64].T)
    put("nak", f(inp["na_k_gain"])[:, p % 64].T)
    put("dfq", f(inp["diff_q_gain"])[:, p % 32].T)
    put("dfk", f(inp["diff_k_gain"])[:, p % 32].T)
    lam = np.stack([f(inp["diff_lambda_q1"]), f(inp["diff_lambda_k1"]), f(inp["diff_lambda_q2"]),
                    f(inp["diff_lambda_k2"])], axis=1)
    put("lam", np.broadcast_to(lam[None], (128, nlayers, 4, 32)).copy())
    put("subln", np.broadcast_to(f(inp["diff_subln"])[None], (128, nlayers, 64)).copy())
    put("pscale", f(inp["pool_scale"]).reshape(nlayers, 2, 128).transpose(2, 0, 1))
    put("rb", np.broadcast_to(f(inp["router_b"])[None], (128, nlayers, 32)).copy())
    return pf


def build_nc(nlayers=2, do_moe=True, n_exp=NE, stop=None, mixers=(0, 1, 2, 3), ne_decl=NE):
    nc = bass.Bass("TRN2", target_bir_lowering=False)
    P = Prog()
    P.group_streams = {"const", "xin"}

    def din(name, shape, dt=F32):
        return nc.dram_tensor(name, list(shape), dt, kind="ExternalInput").ap()

    x_d = din("x", [L, D])
    cx_d = din("cx", [LC, D])
    pf_d = din("pf", [128, NPF])
    cb_d = din("cb", [128, NCB], BF16)
    cf_d = din("cf", [128, NCF])
    wada_d = din("wada", [nlayers, D, 6 * D])
    win_d = din("win", [nlayers, D, 2048])
    wout_d = din("wout", [nlayers, D, D])
    nab_d = din("nab", [nlayers, 128, 4 * 14 * 64])
    rope_d = din("rope", [128, 2 * L], BF16)
    pband_d = din("pband", [128, 2560], BF16)
    poolw_d = din("poolw", [nlayers, 128, 512])
    fftw_d = din("fftw", [nlayers, 64, 512])
    cl_d = din("cl", [16, 128, 2048], BF16)
    t256_d = din("t256", [128, 1024], BF16)
    cs64p_d = din("cs64p", [64, 512])
    sl_d = din("sl", [16, 128, 2048], BF16)
    rw_d = din("rw", [nlayers, 128, 256])
    b1t_d = din("b1t", [nlayers, 128, NE * 16])
    b2_d = din("b2", [nlayers, NE, D])
    w1_d = din("w1", [nlayers, ne_decl, D, 2 * D])
    w2_d = din("w2", [nlayers, ne_decl, D, D])
    out_d = nc.dram_tensor("out", [L, D], F32, kind="ExternalOutput").ap()

    TOTAL = 212000
    ALL = nc.alloc_sbuf_tensor("allsb", [128, TOTAL // 2], BF16)
    OFF_X = 0
    OFF_HT = 73728
    OFF_B = OFF_HT + 36864
    OFF_RING = OFF_B + 36864
    OFF_MISC = OFF_RING + 16384
    MISC_SZ = 7168
    OFF_S = OFF_MISC + MISC_SZ
    S_SZ = TOTAL - OFF_S

    def carve(off, shape, dt, parts=128):
        n = 1
        for s_ in shape:
            n *= s_
        assert off % 4 == 0
        if dt == F32:
            ap = ALL[0:parts, off // 2: off // 2 + 2 * n].bitcast(F32)
        else:
            ap = ALL[0:parts, off // 2: off // 2 + n]
        if len(shape) == 2:
            ap = ap.rearrange("p (a b) -> p a b", a=shape[0])
        elif len(shape) == 3:
            ap = ap.rearrange("p (a b c) -> p a b c", a=shape[0], b=shape[1])
        elif len(shape) == 4:
            ap = ap.rearrange("p (a b c d) -> p a b c d", a=shape[0], b=shape[1], c=shape[2])
        return ap

    def nbytes(shape, dt):
        n = 4 if dt == F32 else 2
        for s_ in shape:
            n *= s_
        return (n + 31) // 32 * 32

    class Bump:
        def __init__(self, regions):
            self.regions = regions
            self.cur = [r[0] for r in regions]

        def get(self, shape, dt, parts=128):
            nb = nbytes(shape, dt)
            for i, (o, sz) in enumerate(self.regions):
                if self.cur[i] + nb <= o + sz:
                    a = carve(self.cur[i], shape, dt, parts)
                    self.cur[i] += nb
                    return a
            raise RuntimeError("scratch overflow %s" % (shape,))

        def mark(self):
            return list(self.cur)

        def reset(self, m):
            self.cur = list(m)

    X = carve(OFF_X, [18, D], F32)
    HT = carve(OFF_HT, [8, 2304], BF16)
    YG = carve(OFF_B, [2, 2304], BF16)
    ACTB = carve(OFF_B, [8, 2304], BF16)
    misc = Bump([(OFF_MISC, MISC_SZ)])
    CB = misc.get([NCB], BF16)
    CF = misc.get([NCF], F32)
    PF = misc.get([NPF], F32)
    MOD = misc.get([nlayers, 48, 2], F32)
    CS = misc.get([8, 2], F32)
    GS = misc.get([8, 2], F32)
    SS = misc.get([18], F32)
    RSTD = misc.get([18], F32)
    NLAM = misc.get([2], F32)
    SM = misc.get([16], F32)

    def cbv(name):
        o, n = CB_OFF[name]
        return CB[:, o:o + n]

    def cfv(name, parts=128):
        o, n = CF_OFF[name]
        return CF[0:parts, o:o + n]

    def pfv(name):
        o, n = PF_OFF[name]
        return PF[:, o:o + n]

    IDB, BD64, BD32, PERM = cbv("ident"), cbv("bd64"), cbv("bd32"), cbv("perm")
    IDF, ONESF, EPSC = cfv("identf"), cfv("onesf"), cfv("eps")
    MASK12 = cfv("mask12")
    MASK4 = cfv("mask4")
    CVEC = pfv("cvec").rearrange("p (a b) -> p a b", a=8)
    BADA = pfv("bada").rearrange("p (a b) -> p a b", a=nlayers)
    G1T = pfv("g1").rearrange("p (a b) -> p a b", a=nlayers)
    G2T = pfv("g2").rearrange("p (a b) -> p a b", a=nlayers)
    LAMV = pfv("lam").rearrange("p (a b c) -> p a b c", a=nlayers, b=4)
    SUBLN = pfv("subln").rearrange("p (a b) -> p a b", a=nlayers)
    PSCALE = pfv("pscale").rearrange("p (a b) -> p a b", a=nlayers)
    RB = pfv("rb").rearrange("p (a b) -> p a b", a=nlayers)

    PS = [nc.alloc_psum_tensor("ps%d" % i, [128, 512], F32) for i in range(8)]
    psc = {"a": 0, "b": 0, "c": 0}
    psr = {"a": (0, 4), "b": (4, 2), "c": (6, 2)}

    def psum(role):
        base, n = psr[role]
        i = base + psc[role] % n
        psc[role] += 1
        return PS[i], ("ps", i)

    def MM(out, lhsT, rhs, start, stop, r, w, skip=False):
        if skip:
            P.op("pe", lambda e: e.matmul(out, lhsT=lhsT, rhs=rhs, start=start, stop=stop, skip_group_check=True), r, w)
        else:
            P.op("pe", lambda e: e.matmul(out, lhsT=lhsT, rhs=rhs, start=start, stop=stop), r, w)

    def TR(out, in_, ident, r, w):
        P.op("pe", lambda e: e.transpose(out=out, in_=in_, identity=ident), r, w)

    def ACT(out, in_, func, r, w, bias=None, scale=None, accum=None):
        kw = {}
        if bias is not None:
            kw["bias"] = bias
        if scale is not None:
            kw["scale"] = scale
        if accum is not None:
            kw["accum_out"] = accum
        P.op("act", lambda e: e.activation(out=out, in_=in_, func=func, **kw), r, w)

    def TS(out, in0, s1, s2, op0, op1, r, w, eng="dve"):
        if op1 is None:
            P.op(eng, lambda e: e.tensor_scalar(out=out, in0=in0, scalar1=s1, scalar2=None, op0=op0), r, w)
        else:
            P.op(eng, lambda e: e.tensor_scalar(out=out, in0=in0, scalar1=s1, scalar2=s2, op0=op0, op1=op1), r, w)

    def TT(out, in0, in1, op, r, w, eng="dve"):
        P.op(eng, lambda e: e.tensor_tensor(out=out, in0=in0, in1=in1, op=op), r, w)

    def STT(out, in0, scalar, in1, op0, op1, r, w, eng="dve"):
        P.op(eng, lambda e: e.scalar_tensor_tensor(out=out, in0=in0, scalar=scalar, in1=in1, op0=op0, op1=op1), r, w)

    def CPY(out, in_, r, w, eng="dve"):
        if eng == "act":
            P.op("act", lambda e: e.copy(out=out, in_=in_), r, w)
        else:
            P.op(eng, lambda e: e.tensor_copy(out=out, in_=in_), r, w)

    def RECIP(out, in_, r, w):
        P.op("dve", lambda e: e.reciprocal(out=out, in_=in_), r, w)

    def MEMSET(out, val, w, eng="pool"):
        P.op(eng, lambda e: e.memset(out, val), (), w)

    def DMA(eng, out, in_, stream, r, w):
        P.op(eng, lambda e: e.dma_start(out=out, in_=in_), r, w, dma=stream)

    ring_n = [0]

    def ring_load(shape, dt, dram_ap, parts=128):
        s = ring_n[0] % 4
        ring_n[0] += 1
        v = carve(OFF_RING + 4096 * s, shape, dt, parts)
        DMA("pool", v, dram_ap, "ring%d" % s, (), [("ring", s)])
        return v, ("ring", s)

    def run_units(units, depth=3):
        loaded = []
        for i in range(len(units)):
            while len(loaded) < min(len(units), i + depth):
                loaded.append(units[len(loaded)][0]())
            units[i][1](*loaded[i])

    def run_pipe(its, d=2):
        n = len(its)
        for i in range(n + d):
            if i < n:
                its[i][0]()
            if i >= d:
                its[i - d][1]()

    def tkeys(name, c, t0, n):
        return [(name, c, t) for t in range(t0 // 128, (t0 + n + 127) // 128)]

    DMA("sp", CB, cb_d, "const", (), ["CB"])
    DMA("sp", CF, cf_d, "const", (), ["CF"])
    DMA("sp", PF, pf_d, "const", (), ["PF"])
    for j in range(16):
        DMA("sp", X[:, j, :], x_d[j * 128:(j + 1) * 128, :], "xin", (), [("X", j)])
    for j in range(2):
        DMA("sp", X[:, 16 + j, :], cx_d[j * 128:(j + 1) * 128, :], "xin", (), [("X", 16 + j)])
    CONSTS = ["CB", "CF", "PF"]
    ACT(CS, CVEC, AF.Silu, CONSTS, ["CS"])
    sA = Bump([(OFF_HT, 36864)])
    WA = [sA.get([8, 256], F32) for _ in range(2)]
    for l in range(nlayers):
        wv = wada_d[l].rearrange("(k p) n -> p k n", p=128)
        for u in range(24):
            b_ = u % 2
            DMA("sp", WA[b_], wv[:, :, u * 256:(u + 1) * 256], "wa%d" % b_, (), [("WA", b_)])
            for jj in range(2):
                j = u * 2 + jj
                ps, pk = psum("c")
                for k in range(8):
                    MM(ps[:, 0:2], WA[b_][:, k, jj * 128:(jj + 1) * 128], CS[:, k, :], k == 0, k == 7,
                       [("WA", b_), "CS"], [pk])
                TS(MOD[:, l, j, :], ps[:, 0:2], BADA[:, l, j:j + 1], None, ALU.add, None, [pk, "PF"], [("MOD", l)])
    P.barrier()

    def mod_ap(l, which, c, src):
        return MOD[:, l, which * 8 + c, src:src + 1]

    def bcast_vec(dst, l, which, src, tmp):
        for c in range(8):
            TS(tmp, IDF, mod_ap(l, which, c, src), None, ALU.mult, None, ["CF", ("MOD", l)], ["bctmp"])
            ps, pk = psum("c")
            MM(ps[:, 0:128], ONESF, tmp, True, True, ["CF", "bctmp"], [pk])
            CPY(dst[:, c * 128:(c + 1) * 128], ps[:, 0:128], [pk], ["GB"], eng="act")

    def norm_phase(l, which0, gT, tiles, scr, router=None, pre_router=None):
        XNs = [scr.get([D], F32) for _ in range(2)]
        JUNK = scr.get([D], BF16)
        for src in range(2):
            TS(GS[:, :, src], MOD[:, l, (which0 + 1) * 8:(which0 + 2) * 8, src], 1.0, None, ALU.add, None,
               [("MOD", l)], ["GS"])
            TT(GS[:, :, src], GS[:, :, src], gT[:, l, :], ALU.mult, ["GS", "PF"], ["GS"])
        if pre_router is not None:
            pre_router()
        MEMSET(SS, 0.0, [("SS", j) for j in range(18)], eng="dve")
        for j in tiles:
            src = 0 if j < 16 else 1
            XN = XNs[j % 2]
            xnk = ("XN", j % 2)
            xk = [("X", j)] + [("X", j, q) for q in range(4)]
            ACT(JUNK, X[:, j, :], AF.Square, xk, [("SS", j)], accum=SS[:, j:j + 1])
            ACT(RSTD[:, j:j + 1], SS[:, j:j + 1], AF.Ln, [("SS", j), "CF"], [("RSTD", j)], bias=EPSC, scale=1.0 / D)
            ACT(RSTD[:, j:j + 1], RSTD[:, j:j + 1], AF.Exp, [("RSTD", j)], [("RSTD", j)], scale=-0.5)
            TS(XN, X[:, j, :], RSTD[:, j:j + 1], None, ALU.mult, None, xk + [("RSTD", j)], [xnk])
            pss = [psum("a"), psum("a")]
            for c in range(8):
                ps, pk = pss[c // 4]
                TR(ps[:, (c % 4) * 128:(c % 4 + 1) * 128], XN[:, c * 128:(c + 1) * 128], IDF, [xnk, "CF"], [pk])
            if router is not None:
                router(j, src, pss)
            for c in range(8):
                ps, pk = pss[c // 4]
                ACT(HT[:, c, j * 128:(j + 1) * 128], ps[:, (c % 4) * 128:(c % 4 + 1) * 128], AF.Identity,
                    [pk, "GS", ("MOD", l)], [("HT", c, j)], bias=mod_ap(l, which0, c, src), scale=GS[:, c, src:src + 1])

    def proj_units(l, col0, ncols):
        wv = win_d[l].rearrange("(k p) n -> p k n", p=128)
        return [(lambda c0=c0: ring_load([8, 256], BF16, wv[:, :, c0:c0 + 256])) for c0 in range(col0, col0 + ncols, 256)]

    def tok_blocks(ntok):
        return [(t0, min(512, ntok - t0)) for t0 in range(0, ntok, 512)]

    def proj_fm(unit, ukey, ntok, evac):
        for cc in range(2):
            for (t0, n) in tok_blocks(ntok):
                ps, pk = psum("a")
                for k in range(8):
                    MM(ps[:, 0:n], unit[:, k, cc * 128:(cc + 1) * 128], HT[:, k, t0:t0 + n], k == 0, k == 7,
                       [ukey] + tkeys("HT", k, t0, n), [pk])
                evac(cc, t0, n, ps, pk)

    def proj_tm(unit, ukey, t0, evac):
        ps, pk = psum("a")
        for k in range(8):
            MM(ps[:, 0:256], HT[:, k, t0:t0 + 128], unit[:, k, :], k == 0, k == 7, [ukey] + tkeys("HT", k, t0, 128), [pk])
        evac(ps, pk)

    def wout_apply(l, g, ntiles, G1B, scr):
        TMP = [scr.get([512], F32) for _ in range(2)]
        wv = wout_d[l][g * 256:(g + 1) * 256, :].rearrange("(k p) n -> p k n", p=128)
        unit, ukey = ring_load([2, D], BF16, wv)
        n = 0
        for j in range(ntiles):
            src = 0 if j < 16 else 1
            for fb in range(2):
                ps, pk = psum("a")
                for k in range(2):
                    MM(ps[:, :], YG[:, k, j * 128:(j + 1) * 128], unit[:, k, fb * 512:(fb + 1) * 512], k == 0, k == 1,
                       [ukey, ("YG", k, j)], [pk])
                t = TMP[n % 2]
                tk = ("wtmp", n % 2)
                n += 1
                TT(t, ps[:, :], G1B[:, src, fb * 512:(fb + 1) * 512], ALU.mult, [pk, "GB"], [tk])
                TT(X[:, j, fb * 512:(fb + 1) * 512], X[:, j, fb * 512:(fb + 1) * 512], t, ALU.add, [("X", j), tk], [("X", j)],
                   eng="pool")

    def qk_norm_evac(SQ, RS, gain_ap, bd, out_fn):
        def evac(cc, t0, n, ps, pk):
            ACT(SQ[:, 0:n], ps[:, 0:n], AF.Square, [pk], ["SQ"])
            ps2, pk2 = psum("c")
            MM(ps2[:, 0:n], bd, SQ[:, 0:n], True, True, ["SQ", "CB"], [pk2])
            ACT(RS[:, 0:n], ps2[:, 0:n], AF.Sqrt, [pk2, "CF"], ["RS"], bias=EPSC, scale=1.0)
            RECIP(RS[:, 0:n], RS[:, 0:n], ["RS"], ["RS"])
            out_fn(cc, t0, n, ps, pk)
        return evac

    def mixer_na(l, ctx_out, G1B, scr):
        ntok = 2304
        QT = scr.get([2, 2304], BF16)
        KT = scr.get([2, 2304], BF16)
        VA = scr.get([18, 4, 65], BF16)
        VS = scr.get([15, 4, 65], BF16)
        DB = scr.get([4, 14, 64], BF16)
        SQ = scr.get([512], BF16)
        RS = scr.get([512], F32)
        GQ = scr.get([2], F32)
        PT = [scr.get([384], BF16) for _ in range(2)]
        PTC = scr.get([2, 256], BF16)
        ON = scr.get([128], F32)
        RZ = scr.get([2], F32)
        ONC = scr.get([128], F32)
        DMA("pool", DB, nab_d[l].rearrange("p (a b c) -> p a b c", a=4, b=14), "nab", (), ["DB"])
        MEMSET(VA[:, :, :, 64:65], 1.0, ["VA"])
        MEMSET(VS[:, :, :, 64:65], 1.0, ["VS"])
        naq = pfv("naq")
        nak = pfv("nak")
        TS(GQ[:, 0:1], naq[:, l:l + 1], 0.125, None, ALU.mult, None, ["PF"], ["GQ"])

        def q_out(cc, t0, n, ps, pk):
            STT(QT[:, cc, t0:t0 + n], ps[:, 0:n], GQ[:, 0:1], RS[:, 0:n], ALU.mult, ALU.mult, [pk, "RS", "GQ"], ["QT"])

        def k_out(cc, t0, n, ps, pk):
            STT(KT[:, cc, t0:t0 + n], ps[:, 0:n], nak[:, l:l + 1], RS[:, 0:n], ALU.mult, ALU.mult, [pk, "RS", "PF"], ["KT"])

        def v_comp(unit, ukey):
            for j in range(18):
                def ev(ps, pk, j=j):
                    CPY(VA[:, j, :, 0:64], ps[:, 0:256].rearrange("p (h d) -> p h d", h=4), [pk], ["VA"], eng="act")
                proj_tm(unit, ukey, j * 128, ev)
            for i in range(15):
                def ev(ps, pk, i=i):
                    CPY(VS[:, i, :, 0:64], ps[:, 0:256].rearrange("p (h d) -> p h d", h=4), [pk], ["VS"], eng="act")
                proj_tm(unit, ukey, 64 + i * 128, ev)

        lq, lk, lv = proj_units(l, 0, 256)[0], proj_units(l, 256, 256)[0], proj_units(l, 512, 256)[0]
        units = [
            (lq, lambda u, uk: proj_fm(u, uk, ntok if ctx_out else L, qk_norm_evac(SQ, RS, None, BD64, q_out))),
            (lk, lambda u, uk: proj_fm(u, uk, ntok, qk_norm_evac(SQ, RS, None, BD64, k_out))),
            (lv, v_comp),
        ]
        run_units(units)
        NPT = 4
        PT = PT + [scr.get([384], BF16) for _ in range(NPT - 2)]
        itc = [0]
        for hp in range(2):
            its = []
            state = {}
            for r in range(32):
                for hh in range(2):
                    def AB(r=r, hh=hh, hp=hp):
                        rs = min(max(r - 4, 0), 24)
                        m0b = 7 - (r - rs)
                        h = hp * 2 + hh
                        b0 = 64 * hh
                        ps, pk = psum("a")
                        q_ap = QT[b0:b0 + 64, hp, r * 64:(r + 1) * 64]
                        for c in range(6):
                            kt0 = (rs + 2 * c) * 64 if c < 4 else 2048 + (c - 4) * 128
                            MM(ps[:, c * 64:(c + 1) * 64], KT[b0:b0 + 64, hp, kt0:kt0 + 128], q_ap, True, True, ["KT", "QT"], [pk])
                        dv = DB[:, h, m0b:m0b + 7:2, :]
                        pv = ps[:, 0:256].rearrange("p (a b) -> p a b", a=4)
                        TT(pv, pv, dv, ALU.add, [pk, "DB"], [pk])
                        i_ = itc[0] % NPT
                        itc[0] += 1
                        ACT(PT[i_], ps[:, 0:384], AF.Exp, [pk], [("PT", i_)])
                        state[(r, hh)] = i_

                    def C(r=r, hh=hh, hp=hp):
                        rs = min(max(r - 4, 0), 24)
                        h = hp * 2 + hh
                        if hh == 0:
                            state[("po", r)] = psum("b")
                        po, pok = state[("po", r)]
                        i_ = state[(r, hh)]
                        pt, ptk = PT[i_], ("PT", i_)
                        for c in range(6):
                            if c < 4:
                                kr = rs + 2 * c
                                vap = VA[:, kr // 2, h, :] if kr % 2 == 0 else VS[:, (kr - 1) // 2, h, :]
                            else:
                                vap = VA[:, 16 + (c - 4), h, :]
                            MM(po[0:64, hh * 65:hh * 65 + 65], pt[:, c * 64:(c + 1) * 64], vap, c == 0, c == 5,
                               [ptk, "VA", "VS"], [pok])
                        if hh == 1:
                            RECIP(RZ[0:64, :], po[0:64, 0:130].rearrange("p (a b) -> p a b", a=2)[:, :, 64], [pok], ["RZ"])
                            for h2 in range(2):
                                TS(ON[0:64, h2 * 64:(h2 + 1) * 64], po[0:64, h2 * 65:h2 * 65 + 64], RZ[0:64, h2:h2 + 1], None, ALU.mult, None,
                                   [pok, "RZ"], ["ON"])
                            pt2, pk2 = psum("c")
                            TR(pt2[:, 0:64], ON[0:64, :], IDF[0:64, 0:64], ["ON", "CF"], [pk2])
                            CPY(YG[:, hp, r * 64:(r + 1) * 64], pt2[:, 0:64], [pk2], [("YG", hp, r // 2)], eng="act")
                    its.append((AB, C))
            run_pipe(its, d=2)
            if ctx_out:
                for qt in range(2):
                    po, pok = psum("b")
                    for hh in range(2):
                        h = hp * 2 + hh
                        b0 = 64 * hh
                        ps, pk = psum("a")
                        for kc in range(2):
                            MM(ps[:, kc * 128:(kc + 1) * 128], KT[b0:b0 + 64, hp, 2048 + kc * 128:2048 + (kc + 1) * 128],
                               QT[b0:b0 + 64, hp, 2048 + qt * 128:2048 + (qt + 1) * 128], True, True, ["KT", "QT"], [pk])
                        ACT(PTC[:, hh, :], ps[:, 0:256], AF.Exp, [pk], [("PTC", hh)])
                        for kc in range(2):
                            MM(po[:, hh * 65:hh * 65 + 65], PTC[:, hh, kc * 128:(kc + 1) * 128], VA[:, 16 + kc, h, :], kc == 0, kc == 1,
                               [("PTC", hh), "VA"], [pok])
                    RECIP(RZ[:, :], po[:, 0:130].rearrange("p (a b) -> p a b", a=2)[:, :, 64], [pok], ["RZ"])
                    for hh in range(2):
                        TS(ONC[:, hh * 64:(hh + 1) * 64], po[:, hh * 65:hh * 65 + 64], RZ[:, hh:hh + 1], None, ALU.mult, None,
                           [pok, "RZ"], ["ONC"])
                    pt2, pk2 = psum("c")
                    TR(pt2[:, 0:128], ONC[:, :], IDF, ["ONC", "CF"], [pk2])
                    CPY(YG[:, hp, 2048 + qt * 128:2048 + (qt + 1) * 128], pt2[:, 0:128], [pk2], [("YG", hp, 16 + qt)], eng="act")
        wout_apply(l, 0, 18 if ctx_out else 16, G1B, scr)


    def mixer_diff(l, ctx_out, G1B, scr):
        lam_init = 0.8 - 0.6 * math.exp(-0.3 * l)
        ROPE = scr.get([2, L], BF16)
        DMA("sp", ROPE, rope_d.rearrange("p (a b) -> p a b", a=2), "rope", (), ["ROPE"])
        LT = scr.get([2, 32], F32)
        for i in range(2):
            TT(LT[:, i, :], LAMV[:, l, 2 * i, :], LAMV[:, l, 2 * i + 1, :], ALU.mult, ["PF"], ["LT"])
            P.op("dve", lambda e, i=i: e.reduce_sum(out=SM[:, i:i + 1], in_=LT[:, i, :], axis=mybir.AxisListType.X), ["LT"], ["SM"])
        ACT(SM[:, 0:2], SM[:, 0:2], AF.Exp, ["SM"], ["SM"])
        TT(SM[:, 2:3], SM[:, 1:2], SM[:, 0:1], ALU.subtract, ["SM"], ["SM"])
        TS(NLAM[:, 0:1], SM[:, 2:3], -lam_init, None, ALU.add, None, ["SM"], ["NLAM"])
        SLG = scr.get([64], F32)
        TS(SLG, SUBLN[:, l, :], 1.0 - lam_init, None, ALU.mult, None, ["PF"], ["SLG"])
        GQ = scr.get([2], F32)
        dfq, dfk = pfv("dfq"), pfv("dfk")
        TS(GQ[:, 0:1], dfq[:, l:l + 1], 32.0 ** -0.5, None, ALU.mult, None, ["PF"], ["GQ"])
        SQ = scr.get([512], BF16)
        QG = scr.get([512], BF16)
        RS = scr.get([512], F32)
        A_ = scr.get([512], F32)
        B_ = scr.get([512], F32)
        QQ = [scr.get([2304], BF16) for _ in range(4)]
        KT = scr.get([2304], BF16)
        VA = scr.get([18, 2, 65], BF16)
        PT = [scr.get([512], BF16) for _ in range(2)]
        OO = scr.get([2, 4, 65], F32)
        RR = scr.get([2, 4], F32)
        TQ = scr.get([64], F32)
        JK = scr.get([64], F32)
        YDT = scr.get([4, 128], F32)
        MEMSET(VA[:, :, :, 64:65], 1.0, ["VA"])
        ntq = 2304 if ctx_out else L
        mark = scr.mark()
        for hp in range(2):
            def prep(gain_ap, gkeys, outs):
                def evac(cc_unused, t0, n, ps, pk):
                    ACT(SQ[:, 0:n], ps[:, 0:n], AF.Square, [pk], ["SQ"])
                    ACT(QG[:, 0:n], ps[:, 0:n], AF.Identity, [pk] + gkeys, ["QG"], scale=gain_ap)
                    ps2, pk2 = psum("c")
                    MM(ps2[:, 0:n], BD32, SQ[:, 0:n], True, True, ["SQ", "CB"], [pk2])
                    ACT(RS[:, 0:n], ps2[:, 0:n], AF.Sqrt, [pk2, "CF"], ["RS"], bias=EPSC, scale=1.0)
                    RECIP(RS[:, 0:n], RS[:, 0:n], ["RS"], ["RS"])
                    if t0 < L:
                        ps3, pk3 = psum("c")
                        MM(ps3[:, 0:n], PERM, QG[:, 0:n], True, True, ["QG", "CB"], [pk3])
                        TT(A_[:, 0:n], QG[:, 0:n], ROPE[:, 0, t0:t0 + n], ALU.mult, ["QG", "ROPE"], ["A"])
                        TT(B_[:, 0:n], ps3[:, 0:n], ROPE[:, 1, t0:t0 + n], ALU.mult, [pk3, "ROPE"], ["B"])
                        TT(A_[:, 0:n], A_[:, 0:n], B_[:, 0:n], ALU.add, ["A", "B"], ["A"], eng="pool")
                        src_ap = A_
                        sk = "A"
                    else:
                        src_ap = QG
                        sk = "QG"
                    for (dst, dk, mk) in outs:
                        if mk is None:
                            TT(dst[:, t0:t0 + n], src_ap[:, 0:n], RS[:, 0:n], ALU.mult, [sk, "RS"], [dk])
                        else:
                            STT(dst[:, t0:t0 + n], src_ap[:, 0:n], mk, RS[:, 0:n], ALU.mult, ALU.mult, [sk, "RS", "CF"], [dk])
                return evac

            def one_chunk(unit, ukey, ntok, evac, cc):
                for (t0, n) in tok_blocks(ntok):
                    ps, pk = psum("a")
                    for k in range(8):
                        MM(ps[:, 0:n], unit[:, k, cc * 128:(cc + 1) * 128], HT[:, k, t0:t0 + n], k == 0, k == 7,
                           [ukey] + tkeys("HT", k, t0, n), [pk])
                    evac(cc, t0, n, ps, pk)

            def v_comp(unit, ukey, hp=hp):
                for j in range(18):
                    def ev(ps, pk, j=j):
                        CPY(VA[:, j, :, 0:64], ps[:, hp * 128:(hp + 1) * 128].rearrange("p (h d) -> p h d", h=2), [pk], ["VA"], eng="act")
                    proj_tm(unit, ukey, j * 128, ev)

            units = [
                (proj_units(l, 768, 256)[0], lambda u, uk, hp=hp: one_chunk(u, uk, ntq, prep(GQ[:, 0:1], ["GQ"], [(QQ[j_], ("QQ", j_), MASK4[:, j_:j_ + 1]) for j_ in range(4)]), hp)),
                (proj_units(l, 1024, 256)[0], lambda u, uk, hp=hp: one_chunk(u, uk, 2304, prep(dfk[:, l:l + 1], ["PF"], [(KT, "KT", None)]), hp)),
                (proj_units(l, 1280, 256)[0], v_comp),
            ]
            run_units(units)
            NPT = 4
            if hp == 0:
                PT = PT + [scr.get([512], BF16) for _ in range(NPT - 2)]
            itc = [0]
            state = {}
            its = []
            qblocks = [(qb * 512, 512, list(range(16, 18)) + list(range(16))) for qb in range(4)]
            if ctx_out:
                qblocks.append((2048, 256, [16, 17]))
            for (q0, qn, kcs) in qblocks:
                nqt = qn // 128
                for hh in range(2):
                    for sub in range(2):
                        for ci, kc in enumerate(kcs):
                            def AB(q0=q0, qn=qn, hh=hh, sub=sub, kc=kc, ci=ci):
                                Qs, qk_ = QQ[2 * hh + sub], ("QQ", 2 * hh + sub)
                                ps, pk = psum("a")
                                MM(ps[:, 0:qn], KT[:, kc * 128:(kc + 1) * 128], Qs[:, q0:q0 + qn], True, True, ["KT", qk_], [pk])
                                i_ = itc[0] % NPT
                                itc[0] += 1
                                ACT(PT[i_][:, 0:qn], ps[:, 0:qn], AF.Exp, [pk], [("PT", i_)])
                                state[(q0, hh, sub, ci)] = i_

                            def C(q0=q0, qn=qn, nqt=nqt, hh=hh, sub=sub, kc=kc, ci=ci, nk=len(kcs), hp=hp):
                                if ci == 0:
                                    state[("po", q0, hh, sub)] = psum("b")
                                po, pok = state[("po", q0, hh, sub)]
                                i_ = state[(q0, hh, sub, ci)]
                                pt, ptk = PT[i_], ("PT", i_)
                                for qt in range(nqt):
                                    MM(po[:, qt * 65:qt * 65 + 65], pt[:, qt * 128:(qt + 1) * 128], VA[:, kc, hh, :], ci == 0 and qt == 0,
                                       ci == nk - 1, [ptk, "VA"], [pok], skip=True)
                                if ci != nk - 1:
                                    return
                                CPY(OO[:, sub, 0:nqt, :], po[:, 0:nqt * 65].rearrange("p (a b) -> p a b", a=nqt), [pok], [("OO", sub)], eng="act")
                                if sub != 1:
                                    return
                                RECIP(RR[:, :, 0:nqt], OO[:, :, 0:nqt, 64], [("OO", 0), ("OO", 1)], ["RR"])
                                TS(RR[:, 1, 0:nqt], RR[:, 1, 0:nqt], NLAM[:, 0:1], None, ALU.mult, None, ["RR", "NLAM"], ["RR"])
                                for qt in range(nqt):
                                    TS(TQ, OO[:, 0, qt, 0:64], RR[:, 0, qt:qt + 1], None, ALU.mult, None, [("OO", 0), "RR"], ["TQ"])
                                    STT(TQ, OO[:, 1, qt, 0:64], RR[:, 1, qt:qt + 1], TQ, ALU.mult, ALU.add, [("OO", 1), "RR", "TQ"], ["TQ"])
                                    TT(JK, TQ, TQ, ALU.mult, ["TQ"], ["JK"])
                                    P.op("dve", lambda e: e.reduce_sum(out=SM[:, 4:5], in_=JK, axis=mybir.AxisListType.X), ["JK"], ["SM4"])
                                    ACT(SM[:, 5:6], SM[:, 4:5], AF.Ln, ["SM4", "CF"], ["SM5"], bias=EPSC, scale=1.0 / 64)
                                    ACT(SM[:, 5:6], SM[:, 5:6], AF.Exp, ["SM5"], ["SM5"], scale=-0.5)
                                    STT(YDT[:, qt, hh * 64:(hh + 1) * 64], TQ, SM[:, 5:6], SLG, ALU.mult, ALU.mult, ["TQ", "SM5", "SLG"], [("YDT", qt)])
                                if hh != 1:
                                    return
                                for qt in range(nqt):
                                    pt2, pk2 = psum("c")
                                    TR(pt2[:, 0:128], YDT[:, qt, :], IDF, [("YDT", qt), "CF"], [pk2])
                                    tt = q0 // 128 + qt
                                    CPY(YG[:, hp, tt * 128:(tt + 1) * 128], pt2[:, 0:128], [pk2], [("YG", hp, tt)], eng="act")
                            its.append((AB, C))
            run_pipe(its, d=2)
        wout_apply(l, 1, 18 if ctx_out else 16, G1B, scr)

    def mixer_pool(l, ctx_out, G1B, scr):
        ntiles = 18 if ctx_out else 16
        ntok = ntiles * 128
        TTt = scr.get([2, 2304], BF16)
        ZP = scr.get([18, 4, 128], BF16)
        PB = scr.get([4, 5, 128], BF16)
        PW = scr.get([4, 128], BF16)
        DMA("sp", PB, pband_d.rearrange("p (a b c) -> p a b c", a=4, b=5), "pband", (), ["PB"])
        DMA("pool", PW, poolw_d[l].rearrange("p (a b) -> p a b", a=4), "poolw", (), ["PW"])

        def t_evac(cc, t0, n, ps, pk):
            CPY(TTt[:, cc, t0:t0 + n], ps[:, 0:n], [pk], [("TT", cc)], eng="act")

        run_units([(proj_units(l, 1536, 256)[0], lambda u, uk: proj_fm(u, uk, ntok, t_evac))])
        for j in range(ntiles):
            ps, pk = psum("a")
            for g in range(4):
                MM(ps[:, g * 128:(g + 1) * 128], TTt[:, g // 2, j * 128:(j + 1) * 128], PW[:, g, :], True, True, [("TT", g // 2), "PW"], [pk])
            CPY(ZP[:, j, :, :], ps[:, :].rearrange("p (a b) -> p a b", a=4), [pk], [("ZP", j)], eng="dve")
        seqs = [(0, 16)] + ([(16, 2)] if ctx_out else [])
        for (j0, nt) in seqs:
            for jo in range(nt):
                for pc in range(2):
                    terms = []
                    for g in (2 * pc, 2 * pc + 1):
                        if jo > 0:
                            terms.append((j0 + jo - 1, g, 0))
                        terms.append((j0 + jo, g, 3 if jo == 0 else (4 if jo == nt - 1 else 1)))
                        if jo < nt - 1:
                            terms.append((j0 + jo + 1, g, 2))
                    ps, pk = psum("a")
                    for i, (ji, g, kind) in enumerate(terms):
                        MM(ps[:, 0:128], ZP[:, ji, g, :], PB[:, g, kind, :], i == 0, i == len(terms) - 1, [("ZP", ji), "PB"], [pk])
                    j = j0 + jo
                    ACT(YG[:, pc, j * 128:(j + 1) * 128], ps[:, 0:128], AF.Identity, [pk, "PF"], [("YG", pc, j)], scale=PSCALE[:, l, pc:pc + 1])
        wout_apply(l, 2, ntiles, G1B, scr)

    def mixer_fft(l, ctx_out, G1B, scr):
        ntiles = 18 if ctx_out else 16
        ntok = ntiles * 128
        TTt = scr.get([2, 2304], BF16)
        TCS = scr.get([18, 4, 128], BF16)
        FW = scr.get([4, 128], F32, parts=64)
        WCS = scr.get([4, 128], BF16)
        DMA("sp", FW, fftw_d[l].rearrange("p (a b) -> p a b", a=4), "fftw", (), ["FW"])
        CS64P = scr.get([2, 2, 128], F32, parts=64)
        DMA("sp", CS64P, cs64p_d.rearrange("p (a b c) -> p a b c", a=2, b=2), "cs64p", (), ["CS64P"])
        T256 = scr.get([2, 2, 256], BF16)
        DMA("sp", T256, t256_d.rearrange("p (a b c) -> p a b c", a=2, b=2), "t256", (), ["T256"])
        C256 = T256[:, 0, :, :]
        S256 = T256[:, 1, :, :]
        for pc in range(2):
            for cs in range(2):
                ps, pk = psum("c")
                for pos in range(2):
                    MM(ps[:, 0:128], CS64P[:, cs, pos, :], FW[:, 2 * pc + pos, :], pos == 0, pos == 1, ["CS64P", "FW"], [pk])
                CPY(WCS[:, pc * 2 + cs, :], ps[:, 0:128], [pk], ["WCS"], eng="act")

        def t_evac(cc, t0, n, ps, pk):
            CPY(TTt[:, cc, t0:t0 + n], ps[:, 0:n], [pk], [("TT", cc)], eng="act")

        run_units([(proj_units(l, 1792, 256)[0], lambda u, uk: proj_fm(u, uk, ntok, t_evac))])
        for j in range(ntiles):
            ps, pk = psum("a")
            for q in range(4):
                MM(ps[:, q * 128:(q + 1) * 128], TTt[:, q // 2, j * 128:(j + 1) * 128], WCS[:, q, :], True, True, [("TT", q // 2), "WCS"], [pk])
            CPY(TCS[:, j, :, :], ps[:, :].rearrange("p (a b) -> p a b", a=4), [pk], ["TCS"], eng="dve")
        units = []
        for kb in range(16):
            def ld(kb=kb):
                c_, ck_ = ring_load([16, 128], BF16, cl_d[kb].rearrange("p (a b) -> p a b", a=16))
                s_, sk_ = ring_load([16, 128], BF16, sl_d[kb].rearrange("p (a b) -> p a b", a=16))
                return (c_, s_), (ck_, sk_)

            def comp(tabs, keys, kb=kb):
                for pc in range(2):
                    ps, pk = psum("a")
                    n = 0
                    for cs in range(2):
                        for lt in range(16):
                            MM(ps[:, 0:128], TCS[:, lt, pc * 2 + cs, :], tabs[cs][:, lt, :], n == 0, n == 31, ["TCS", keys[cs]], [pk])
                            n += 1
                    CPY(YG[:, pc, kb * 128:(kb + 1) * 128], ps[:, 0:128], [pk], [("YG", pc, kb)], eng="act")
            units.append((ld, comp))
        run_units(units, depth=2)
        if ctx_out:
            for pc in range(2):
                ps, pk = psum("a")
                n = 0
                for cs in range(2):
                    tab = C256 if cs == 0 else S256
                    for lt in range(2):
                        MM(ps[:, 0:256], TCS[:, 16 + lt, pc * 2 + cs, :], tab[:, lt, :], n == 0, n == 3, ["TCS", "T256"], [pk])
                        n += 1
                CPY(YG[:, pc, 2048:2304], ps[:, 0:256], [pk], [("YG", pc, 16), ("YG", pc, 17)], eng="act")
        wout_apply(l, 3, ntiles, G1B, scr)

    def moe_phase(l, ctx_out, scr):
        ntiles = 18 if ctx_out else 16
        ntok = ntiles * 128
        G2B = scr.get([2, D], F32)
        BT = scr.get([128], F32)
        for src in range(2 if ctx_out else 1):
            bcast_vec(G2B[:, src, :], l, 5, src, BT)
        WR = scr.get([8, 32], F32)
        WRS = scr.get([2, 8, 32], F32)
        CROW = scr.get([2, 32], F32, parts=1)
        DMA("sp", WR, rw_d[l].rearrange("p (a b) -> p a b", a=8), "rw", (), ["WR"])
        B1T = scr.get([NE, 8, 2], F32)
        DMA("sp", B1T, b1t_d[l].rearrange("p (a b c) -> p a b c", a=NE, b=8), "b1t", (), ["B1T"])
        B2 = scr.get([D], F32, parts=NE)
        DMA("sp", B2, b2_d[l], "b2", (), ["B2"])
        TS(B1T[:, :, :, 1], B1T[:, :, :, 1], 1.0, None, ALU.add, None, ["B1T"], ["B1T"])
        G = scr.get([18, 32], F32)
        GA = scr.get([18, 32], F32)
        moe_mark = scr.mark()
        XNT = scr.get([8, 128], F32)
        LG = scr.get([32], F32)
        T8 = scr.get([8], F32)
        EX = scr.get([32], F32)
        MK = scr.get([32], F32)
        GT = scr.get([128], F32, parts=NE)
        def router(j, src, pss):
            for hb in range(2):
                ps, pk = pss[hb]
                CPY(XNT[:, hb * 4:(hb + 1) * 4, :], ps[:, :].rearrange("p (a b) -> p a b", a=4), [pk], [("XNT", hb)], eng="dve")
            pl, plk = psum("c")
            for c in range(8):
                MM(pl[:, 0:32], XNT[:, c, :], WRS[:, src, c, :], c == 0, False, [("XNT", c // 4), "WRS"], [plk])
            MM(pl[:, 0:32], ONESF[0:1, :], CROW[0:1, src, :], False, True, ["CF", "CROW"], [plk])
            CPY(LG, pl[:, 0:32], [plk], ["LG"], eng="dve")
            P.op("dve", lambda e: e.max(out=T8, in_=LG), ["LG"], ["T8"])
            TS(MK, LG, T8[:, 3:4], None, ALU.is_ge, None, ["LG", "T8"], ["MK"])
            TS(SM[:, 6:7], T8[:, 0:1], -1.0, None, ALU.mult, None, ["T8"], ["SM6"])
            ACT(EX, LG, AF.Exp, ["LG", "SM6"], ["EX"], bias=SM[:, 6:7], scale=1.0)
            TT(EX, EX, MK, ALU.mult, ["EX", "MK"], ["EX"])
            P.op("dve", lambda e: e.reduce_sum(out=SM[:, 7:8], in_=EX, axis=mybir.AxisListType.X), ["EX"], ["SM7"])
            RECIP(SM[:, 7:8], SM[:, 7:8], ["SM7"], ["SM7"])
            TS(G[:, j, :], EX, SM[:, 7:8], None, ALU.mult, None, ["EX", "SM7"], [("G", j)])
            TS(GA[:, j, :], G[:, j, :], 1.0 / ALPHA, None, ALU.mult, None, [("G", j)], [("GA", j)])

        def pre_router():
            for src in range(2 if ctx_out else 1):
                for c in range(8):
                    TS(WRS[:, src, c, :], WR[:, c, :], GS[:, c, src:src + 1], None, ALU.mult, None, ["WR", "GS"], ["WRS"])
                pc_, pck = psum("c")
                for c in range(8):
                    MM(pc_[0:1, 0:32], mod_ap(l, 3, c, src), WR[:, c, :], c == 0, c == 7, [("MOD", l), "WR"], [pck])
                TT(CROW[0:1, src, :], pc_[0:1, 0:32], RB[0:1, l, :], ALU.add, [pck, "PF"], ["CROW"])

        norm_phase(l, 3, G2T, list(range(ntiles)), scr, router, pre_router)

        TMPB = scr.get([512], F32)
        for j in range(ntiles):
            src = 0 if j < 16 else 1
            pt_, ptk_ = psum("c")
            TR(pt_[0:32, 0:128], G[:, j, :], IDF, [("G", j), "CF"], [ptk_])
            CPY(GT[:, :], pt_[0:32, 0:128], [ptk_], ["GT"], eng="act")
            for fb in range(2):
                ps, pk = psum("a")
                MM(ps[:, :], GT[:, :], B2[:, fb * 512:(fb + 1) * 512], True, True, ["GT", "B2"], [pk])
                TT(TMPB, ps[:, :], G2B[:, src, fb * 512:(fb + 1) * 512], ALU.mult, [pk, "GB"], ["TMPB"])
                TT(X[:, j, fb * 512:(fb + 1) * 512], X[:, j, fb * 512:(fb + 1) * 512], TMPB, ALU.add, [("X", j), "TMPB"], [("X", j)], eng="pool")

        P.barrier()
        scr.reset(moe_mark)
        NB = 2
        GC = [scr.get([512], F32) for _ in range(NB)]
        SI = [scr.get([512], F32) for _ in range(NB)]
        L1 = [scr.get([512], F32) for _ in range(NB)]
        TO = [scr.get([256], F32) for _ in range(NB)]
        blocks = tok_blocks(ntok)
        units = []
        cnt = [0, 0]
        for e in range(n_exp):
            w1v = w1_d[l, e].rearrange("(k p) n -> p k n", p=128)
            w2v = w2_d[l, e].rearrange("(k p) n -> p k n", p=128)
            for p in range(8):
                def ld(p=p, w1v=w1v):
                    return ring_load([8, 256], BF16, w1v[:, :, p * 256:(p + 1) * 256])

                def comp(unit, ukey, e=e, p=p):
                    uv = unit.rearrange("p k (f two) -> p k two f", two=2)
                    for (t0, n) in blocks:
                        pg, pgk = psum("a")
                        pl, plk = psum("a")
                        for k in range(8):
                            MM(pg[:, 0:n], uv[:, k, 0, :], HT[:, k, t0:t0 + n], k == 0, k == 7, [ukey] + tkeys("HT", k, t0, n), [pgk])
                        for k in range(8):
                            MM(pl[:, 0:n], uv[:, k, 1, :], HT[:, k, t0:t0 + n], k == 0, k == 7, [ukey] + tkeys("HT", k, t0, n), [plk])
                        i = cnt[0] % NB
                        cnt[0] += 1
                        TS(GC[i][:, 0:n], pg[:, 0:n], B1T[:, e, p, 0:1], 7.0, ALU.add, ALU.min, [pgk, "B1T"], [("GC", i)])
                        ACT(SI[i][:, 0:n], GC[i][:, 0:n], AF.Silu, [("GC", i)], [("SI", i)], scale=ALPHA)
                        TS(L1[i][:, 0:n], pl[:, 0:n], B1T[:, e, p, 1:2], -6.0, ALU.add, ALU.max, [plk, "B1T"], [("L1", i)])
                        STT(ACTB[:, p, t0:t0 + n], L1[i][:, 0:n], 8.0, SI[i][:, 0:n], ALU.min, ALU.mult, [("L1", i), ("SI", i)],
                            [("ACTB", p, t0 // 512)])
                units.append((ld, comp))
            for q in range(4):
                def ld(q=q, w2v=w2v):
                    return ring_load([8, 256], BF16, w2v[:, :, q * 256:(q + 1) * 256])

                def comp(unit, ukey, e=e, q=q):
                    for j in range(ntiles):
                        src = 0 if j < 16 else 1
                        po, pok = psum("b")
                        for k in range(8):
                            MM(po[:, 0:256], ACTB[:, k, j * 128:(j + 1) * 128], unit[:, k, :], k == 0, k == 7, [ukey, ("ACTB", k, j // 4)], [pok])
                        i = cnt[1] % NB
                        cnt[1] += 1
                        STT(TO[i], po[:, 0:256], GA[:, j, e:e + 1], G2B[:, src, q * 256:(q + 1) * 256], ALU.mult, ALU.mult,
                            [pok, ("GA", j), "GB"], [("TO", i)])
                        TT(X[:, j, q * 256:(q + 1) * 256], X[:, j, q * 256:(q + 1) * 256], TO[i], ALU.add, [("X", j), ("X", j, q), ("TO", i)], [("X", j, q)],
                           eng="pool")
                units.append((ld, comp))
        psr["b"] = (4, 4)
        run_units(units, depth=3)
        psr["b"] = (4, 2)

    for l in range(nlayers):
        ctx_out = l < nlayers - 1
        ntiles_all = 18
        scr = Bump([(OFF_B + 9216, 36864 - 9216), (OFF_S, S_SZ)])
        G1B = scr.get([2, D], F32)
        BT = scr.get([128], F32)
        base_mark = scr.mark()
        norm_phase(l, 0, G1T, list(range(18)), scr)
        for src in range(2 if ctx_out else 1):
            bcast_vec(G1B[:, src, :], l, 2, src, BT)
        P.barrier()
        for mi, fn in enumerate((mixer_na, mixer_diff, mixer_pool, mixer_fft)):
            if mi not in mixers:
                continue
            scr.reset(base_mark)
            fn(l, ctx_out, G1B, scr)
            P.barrier()
        if stop == ("mix", l):
            break
        if do_moe:
            scr = Bump([(OFF_S, S_SZ)])
            moe_phase(l, ctx_out, scr)
            P.barrier()
        if stop == ("moe", l):
            break

    for j in range(16):
        DMA("sp", out_d[j * 128:(j + 1) * 128, :], X[:, j, :], "st", [("X", j)] + [("X", j, q) for q in range(4)], [])
    if stop is not None:
        dbg = nc.dram_tensor("dbgc", [LC, D], F32, kind="ExternalOutput").ap()
        for j in range(2):
            DMA("sp", dbg[j * 128:(j + 1) * 128, :], X[:, 16 + j, :], "st", [("X", 16 + j)] + [("X", 16 + j, q) for q in range(4)], [])
    P.emit(nc, final_waits=["st"])
    return nc, len(P.ops)


def make_in_maps(inp, nlayers=2, cores=range(8)):
    f = lambda a: np.ascontiguousarray(np.asarray(a, np.float32))
    cst = host_constants()
    lay = host_layouts(inp, nlayers)
    shared = dict(cb=cst["cb"], cf=cst["cf"], t256=cst["t256"], cs64p=cst["cs64p"], rope=cst["rope"], pband=cst["pband"], cl=cst["cl"], sl=cst["sl"],
                  wada=f(inp["w_ada"]), win=f(inp["w_in"]), wout=f(inp["w_out"]),
                  nab=lay["nab"], poolw=lay["poolw"], fftw=lay["fftw"], rw=lay["rw"], b1t=lay["b1t"], b2=lay["b2"],
                  w1=f(inp["moe_w1"]), w2=f(inp["moe_w2"]))
    x = f(inp["x"])
    cx = f(inp["ctx"])
    maps = []
    for b in cores:
        m = dict(shared)
        m["x"] = x[b]
        m["cx"] = cx[b]
        m["pf"] = host_pf(inp, b, nlayers)
        maps.append(m)
    return maps


_NC_CACHE = {}


def kernel(**inputs):
    if "nc" not in _NC_CACHE:
        _NC_CACHE["nc"] = build_nc()[0]
    nc = _NC_CACHE["nc"]
    maps = make_in_maps(inputs)
    res = run_bass_kernel_spmd(nc, maps, core_ids=list(range(8)))
    return np.stack([np.asarray(r["out"], np.float32) for r in res.results], axis=0)
```

```python
import math
import numpy as np
import ml_dtypes
import concourse.bass as bass
import concourse.mybir as mybir
from concourse.bass_utils import run_bass_kernel_spmd

F32 = mybir.dt.float32
BF16 = mybir.dt.bfloat16
ALU = mybir.AluOpType
AF = mybir.ActivationFunctionType
ENGS = ("pe", "act", "dve", "pool", "sp")

D = 1024
L = 2048
LC = 256
NE = 32
ALPHA = 1.702
EPS = 1e-6
MASKV = -30000.0


class Op:
    __slots__ = ("eng", "fn", "reads", "writes", "dma", "waits", "signal", "sig_idx", "dma_val", "deps")

    def __init__(self, eng, fn, reads, writes, dma):
        self.eng = eng
        self.fn = fn
        self.reads = reads
        self.writes = writes
        self.dma = dma
        self.waits = []
        self.signal = False
        self.sig_idx = 0
        self.dma_val = 0
        self.deps = None


class Prog:
    def __init__(self):
        self.ops = []
        self.last_w = {}
        self.readers = {}
        self.dma_counts = {}
        self.group_streams = set()
        self.pending_barrier = {}

    def op(self, eng, fn, reads=(), writes=(), dma=None):
        o = Op(eng, fn, tuple(reads), tuple(writes), dma)
        idx = len(self.ops)
        deps = set()
        for k in o.reads:
            w = self.last_w.get(k)
            if w is not None:
                deps.add(w)
        for k in o.writes:
            w = self.last_w.get(k)
            if w is not None:
                deps.add(w)
            rs = self.readers.get(k)
            if rs:
                deps.update(rs)
        for k in o.reads:
            self.readers.setdefault(k, []).append(idx)
        for k in o.writes:
            self.last_w[k] = idx
            self.readers[k] = []
        if eng in self.pending_barrier:
            deps.update(self.pending_barrier.pop(eng))
        if dma is not None:
            self.dma_counts[dma] = self.dma_counts.get(dma, 0) + 16
            o.dma_val = self.dma_counts[dma]
        o.deps = deps
        self.ops.append(o)
        return idx

    def barrier(self):
        last = {}
        for i, o in enumerate(self.ops):
            last[o.eng] = i
        lastd = {}
        for i, o in enumerate(self.ops):
            if o.dma is not None:
                lastd[o.dma] = i
        s = set(last.values()) | set(lastd.values())
        for e in ENGS:
            self.pending_barrier[e] = set(s) | self.pending_barrier.get(e, set())

    def finalize(self):
        need = {}
        for ci, c in enumerate(self.ops):
            for pi in c.deps:
                p = self.ops[pi]
                if p.dma is None:
                    if p.eng == c.eng and p.eng in ("pe", "sp"):
                        continue
                    p.signal = True
                need.setdefault(ci, []).append(pi)
        cnt = {e: 0 for e in ENGS}
        for o in self.ops:
            if o.signal:
                cnt[o.eng] += 1
                o.sig_idx = cnt[o.eng]
        waited = {e: {} for e in ENGS}
        for ci, c in enumerate(self.ops):
            ws = {}
            for pi in need.get(ci, ()):
                p = self.ops[pi]
                if p.dma is not None:
                    key = ("dma", p.dma)
                    val = self.dma_counts[p.dma] if p.dma in self.group_streams else p.dma_val
                else:
                    key, val = ("eng", p.eng), p.sig_idx
                if ws.get(key, 0) < val:
                    ws[key] = val
            wd = waited[c.eng]
            for key, val in ws.items():
                if wd.get(key, 0) >= val:
                    continue
                wd[key] = val
                c.waits.append((key, val))

    def emit(self, nc, final_waits=()):
        import contextlib
        self.finalize()
        with contextlib.ExitStack() as es:
            sems = {}
            for e in ENGS:
                sems[("eng", e)] = es.enter_context(nc.semaphore("s_" + e))
            for d in self.dma_counts:
                sems[("dma", d)] = es.enter_context(nc.semaphore("d_" + d))
            block = es.enter_context(nc.Block())
            ops = self.ops
            counts = self.dma_counts

            def body(engname):
                def run(eng):
                    for o in ops:
                        if o.eng != engname:
                            continue
                        for key, val in o.waits:
                            eng.wait_ge(sems[key], val)
                        ins = o.fn(eng)
                        if o.dma is not None:
                            ins.then_inc(sems[("dma", o.dma)], 16)
                        elif o.signal:
                            ins.then_inc(sems[("eng", engname)], 1)
                    if engname == "sp":
                        for d in final_waits:
                            eng.wait_ge(sems[("dma", d)], counts[d])
                return run

            block.tensor(body("pe"))
            block.scalar(body("act"))
            block.vector(body("dve"))
            block.gpsimd(body("pool"))
            block.sync(body("sp"))


def _bf(a):
    return np.ascontiguousarray(a.astype(ml_dtypes.bfloat16))


CB_OFF = {}
CF_OFF = {}
PF_OFF = {}


def _layout(offs, items):
    o = 0
    for name, n in items:
        offs[name] = (o, n)
        o += n
    return o


NCB = _layout(CB_OFF, [("ident", 128), ("bd64", 128), ("bd32", 128), ("perm", 128)])
NCF = _layout(CF_OFF, [("identf", 128), ("onesf", 128), ("eps", 1), ("mask12", 2), ("seven", 1), ("mask4", 4)])
NPF = _layout(PF_OFF, [("cvec", 16), ("bada", 96), ("g1", 16), ("g2", 16), ("naq", 2), ("nak", 2), ("dfq", 2),
                       ("dfk", 2), ("lam", 256), ("subln", 128), ("pscale", 4), ("rb", 64)])

_CONST_CACHE = {}


def host_constants():
    if _CONST_CACHE:
        return _CONST_CACHE
    p = np.arange(128)
    cb = np.zeros((128, NCB), np.float32)
    cb[:, 0:128] = np.eye(128)
    cb[:, 128:256] = (p[:, None] // 64 == p[None, :] // 64) / 64.0
    cb[:, 256:384] = (p[:, None] // 32 == p[None, :] // 32) / 32.0
    partner = np.where((p % 16) < 8, p + 8, p - 8)
    perm = np.zeros((128, 128), np.float32)
    perm[partner, p] = 1.0
    cb[:, 384:512] = perm
    lt = np.arange(2)[None, :, None]
    lin = p[:, None, None]
    k = np.arange(256)[None, None, :]
    ang = 2 * np.pi * ((lt * 128 + lin) * k % 256) / 256.0
    t256 = np.zeros((128, 1024), np.float32)
    t256[:, 0:512] = (np.cos(ang) / 16.0).reshape(128, 512)
    t256[:, 512:1024] = (-np.sin(ang) / 16.0).reshape(128, 512)
    cf = np.zeros((128, NCF), np.float32)
    cf[:, 0:128] = np.eye(128)
    cf[:, 128:256] = 1.0
    cf[:, 256] = EPS
    cf[:, 257] = (p % 64 < 32)
    cf[:, 258] = (p % 64 >= 32)
    m = np.arange(64)[:, None]
    c = np.arange(64)[None, :]
    a64 = 2 * np.pi * (m * c % 64) / 64.0
    cs = np.zeros((64, 2, 2, 128), np.float32)
    cs[:, 0, 0, 0:64] = np.cos(a64) / 8.0
    cs[:, 0, 1, 64:128] = np.cos(a64) / 8.0
    cs[:, 1, 0, 0:64] = np.sin(a64) / 8.0
    cs[:, 1, 1, 64:128] = np.sin(a64) / 8.0
    cf[:, 259] = 7.0
    for j_ in range(4):
        cf[:, 260 + j_] = (p // 32 == j_)
    d = p % 32
    seg = d // 16
    i = d % 16
    j = i % 8
    inv = 10000.0 ** (-(2.0 * j) / 16.0)
    t = np.arange(L)
    pos = np.where(seg[:, None] == 0, (t // 64)[None, :], (t % 64)[None, :]).astype(np.float64)
    angr = pos * inv[:, None]
    rope = np.zeros((128, 2, L), np.float32)
    rope[:, 0, :] = np.cos(angr)
    rope[:, 1, :] = np.where((i < 8)[:, None], -np.sin(angr), np.sin(angr))
    pband = np.zeros((128, 4, 5, 128), np.float32)
    Lp = 512
    posp = np.arange(Lp)
    for g, win in enumerate((2, 4, 8, 16)):
        lo = np.clip(posp - win // 2, 0, Lp)
        hi = np.clip(posp - win // 2 + win, 0, Lp)
        M = np.zeros((Lp, Lp), np.float64)
        for o in range(Lp):
            M[o, lo[o]:hi[o]] = 1.0 / (hi[o] - lo[o])
        M -= np.eye(Lp)
        pband[:, g, 0, :] = M[128:256, 0:128].T
        pband[:, g, 1, :] = M[128:256, 128:256].T
        pband[:, g, 2, :] = M[128:256, 256:384].T
        pband[:, g, 3, :] = M[0:128, 0:128].T
        pband[:, g, 4, :] = M[384:512, 384:512].T
    kb = np.arange(16)[:, None, None, None]
    lin4 = np.arange(128)[None, :, None, None]
    lt4 = np.arange(16)[None, None, :, None]
    kk = np.arange(128)[None, None, None, :]
    prod = ((lt4 * 128 + lin4) * (kb * 128 + kk)) % L
    angL = 2 * np.pi * prod / float(L)
    s = 1.0 / math.sqrt(L)
    cl = (np.cos(angL) * s).reshape(16, 128, 2048)
    sl = (-np.sin(angL) * s).reshape(16, 128, 2048)
    _CONST_CACHE.update(dict(cb=_bf(cb), cf=cf, t256=_bf(t256), cs64p=np.ascontiguousarray(cs.reshape(64, 512)), rope=_bf(rope.reshape(128, 2 * L)),
                             pband=_bf(pband.reshape(128, 4 * 5 * 128)), cl=_bf(cl), sl=_bf(sl)))
    return _CONST_CACHE


def host_layouts(inp, nlayers=2):
    f = lambda a: np.asarray(a, np.float32)
    out = {}
    p = np.arange(128)
    rpb = f(inp["na_rpb"])
    ck = np.arange(64)[:, None]
    cq = np.arange(64)[None, :]
    col_start = np.clip(np.arange(64) - 8, 0, 48)
    inwin = (ck >= col_start[None, :]) & (ck < col_start[None, :] + 16)
    relc = np.clip(ck - cq, -15, 15) + 15
    nab = np.full((nlayers, 2, 64, 4, 14, 64), MASKV, np.float32)
    for l in range(nlayers):
        for h in range(4):
            for m0 in range(14):
                for jj in range(2):
                    blk = rpb[l, h, m0 + jj][relc]
                    nab[l, jj, :, h, m0, :] = np.where(inwin, blk, MASKV)
    out["nab"] = nab.reshape(nlayers, 128, 4 * 14 * 64)
    pw = f(inp["pool_w"])
    poolw = np.zeros((nlayers, 128, 4, 128), np.float32)
    fw = f(inp["fft_w"])
    fftw = np.zeros((nlayers, 64, 4, 128), np.float32)
    for l in range(nlayers):
        for g in range(4):
            o = (g % 2) * 64
            poolw[l, o:o + 64, g, o:o + 64] = pw[l, g]
            fftw[l, :, g, o:o + 64] = fw[l, g]
    out["poolw"] = poolw.reshape(nlayers, 128, 512)
    out["fftw"] = fftw.reshape(nlayers, 64, 512)
    rw = f(inp["router_w"])
    out["rw"] = np.ascontiguousarray(rw.reshape(nlayers, 8, 128, 32).transpose(0, 2, 1, 3)).reshape(nlayers, 128, 256)
    b1 = f(inp["moe_b1"])
    b1t = b1.reshape(nlayers, NE, 8, 128, 2).transpose(0, 3, 1, 2, 4)
    out["b1t"] = np.ascontiguousarray(b1t).reshape(nlayers, 128, NE * 16)
    out["b2"] = np.ascontiguousarray(f(inp["moe_b2"]))
    return out


def host_pf(inp, b, nlayers=2):
    f = lambda a: np.asarray(a, np.float32)
    p = np.arange(128)
    pf = np.zeros((128, NPF), np.float32)

    def put(name, arr):
        o, n = PF_OFF[name]
        pf[:, o:o + n] = arr.reshape(128, n)

    cv = np.zeros((128, 8, 2), np.float32)
    cv[:, :, 0] = f(inp["c"])[b].reshape(8, 128).T
    cv[:, :, 1] = f(inp["c_ctx"]).reshape(8, 128).T
    put("cvec", cv)
    put("bada", f(inp["b_ada"]).reshape(nlayers, 48, 128).transpose(2, 0, 1))
    put("g1", f(inp["g_norm1"]).reshape(nlayers, 8, 128).transpose(2, 0, 1))
    put("g2", f(inp["g_norm2"]).reshape(nlayers, 8, 128).transpose(2, 0, 1))
    put("naq", f(inp["na_q_gain"])[:, p % 64].T)
    put("nak", f(inp["na_k_gain"])[:, p % 64].T)
    put("dfq", f(inp["diff_q_gain"])[:, p % 32].T)
    put("dfk", f(inp["diff_k_gain"])[:, p % 32].T)
    lam = np.stack([f(inp["diff_lambda_q1"]), f(inp["diff_lambda_k1"]), f(inp["diff_lambda_q2"]),
                    f(inp["diff_lambda_k2"])], axis=1)
    put("lam", np.broadcast_to(lam[None], (128, nlayers, 4, 32)).copy())
    put("subln", np.broadcast_to(f(inp["diff_subln"])[None], (128, nlayers, 64)).copy())
    put("pscale", f(inp["pool_scale"]).reshape(nlayers, 2, 128).transpose(2, 0, 1))
    put("rb", np.broadcast_to(f(inp["router_b"])[None], (128, nlayers, 32)).copy())
    return pf


def build_nc(nlayers=2, do_moe=True, n_exp=NE, stop=None, mixers=(0, 1, 2, 3), ne_decl=NE):
    nc = bass.Bass("TRN2", target_bir_lowering=False)
    P = Prog()
    P.group_streams = {"const", "xin"}

    def din(name, shape, dt=F32):
        return nc.dram_tensor(name, list(shape), dt, kind="ExternalInput").ap()

    x_d = din("x", [L, D])
    cx_d = din("cx", [LC, D])
    pf_d = din("pf", [128, NPF])
    cb_d = din("cb", [128, NCB], BF16)
    cf_d = din("cf", [128, NCF])
    wada_d = din("wada", [nlayers, D, 6 * D])
    win_d = din("win", [nlayers, D, 2048])
    wout_d = din("wout", [nlayers, D, D])
    nab_d = din("nab", [nlayers, 128, 4 * 14 * 64])
    rope_d = din("rope", [128, 2 * L], BF16)
    pband_d = din("pband", [128, 2560], BF16)
    poolw_d = din("poolw", [nlayers, 128, 512])
    fftw_d = din("fftw", [nlayers, 64, 512])
    cl_d = din("cl", [16, 128, 2048], BF16)
    t256_d = din("t256", [128, 1024], BF16)
    cs64p_d = din("cs64p", [64, 512])
    sl_d = din("sl", [16, 128, 2048], BF16)
    rw_d = din("rw", [nlayers, 128, 256])
    b1t_d = din("b1t", [nlayers, 128, NE * 16])
    b2_d = din("b2", [nlayers, NE, D])
    w1_d = din("w1", [nlayers, ne_decl, D, 2 * D])
    w2_d = din("w2", [nlayers, ne_decl, D, D])
    out_d = nc.dram_tensor("out", [L, D], F32, kind="ExternalOutput").ap()

    TOTAL = 212000
    ALL = nc.alloc_sbuf_tensor("allsb", [128, TOTAL // 2], BF16)
    OFF_X = 0
    OFF_HT = 73728
    OFF_B = OFF_HT + 36864
    OFF_RING = OFF_B + 36864
    OFF_MISC = OFF_RING + 16384
    MISC_SZ = 7168
    OFF_S = OFF_MISC + MISC_SZ
    S_SZ = TOTAL - OFF_S

    def carve(off, shape, dt, parts=128):
        n = 1
        for s_ in shape:
            n *= s_
        assert off % 4 == 0
        if dt == F32:
            ap = ALL[0:parts, off // 2: off // 2 + 2 * n].bitcast(F32)
        else:
            ap = ALL[0:parts, off // 2: off // 2 + n]
        if len(shape) == 2:
            ap = ap.rearrange("p (a b) -> p a b", a=shape[0])
        elif len(shape) == 3:
            ap = ap.rearrange("p (a b c) -> p a b c", a=shape[0], b=shape[1])
        elif len(shape) == 4:
            ap = ap.rearrange("p (a b c d) -> p a b c d", a=shape[0], b=shape[1], c=shape[2])
        return ap

    def nbytes(shape, dt):
        n = 4 if dt == F32 else 2
        for s_ in shape:
            n *= s_
        return (n + 31) // 32 * 32

    class Bump:
        def __init__(self, regions):
            self.regions = regions
            self.cur = [r[0] for r in regions]

        def get(self, shape, dt, parts=128):
            nb = nbytes(shape, dt)
            for i, (o, sz) in enumerate(self.regions):
                if self.cur[i] + nb <= o + sz:
                    a = carve(self.cur[i], shape, dt, parts)
                    self.cur[i] += nb
                    return a
            raise RuntimeError("scratch overflow %s" % (shape,))

        def mark(self):
            return list(self.cur)

        def reset(self, m):
            self.cur = list(m)

    X = carve(OFF_X, [18, D], F32)
    HT = carve(OFF_HT, [8, 2304], BF16)
    YG = carve(OFF_B, [2, 2304], BF16)
    ACTB = carve(OFF_B, [8, 2304], BF16)
    misc = Bump([(OFF_MISC, MISC_SZ)])
    CB = misc.get([NCB], BF16)
    CF = misc.get([NCF], F32)
    PF = misc.get([NPF], F32)
    MOD = misc.get([nlayers, 48, 2], F32)
    CS = misc.get([8, 2], F32)
    GS = misc.get([8, 2], F32)
    SS = misc.get([18], F32)
    RSTD = misc.get([18], F32)
    NLAM = misc.get([2], F32)
    SM = misc.get([16], F32)

    def cbv(name):
        o, n = CB_OFF[name]
        return CB[:, o:o + n]

    def cfv(name, parts=128):
        o, n = CF_OFF[name]
        return CF[0:parts, o:o + n]

    def pfv(name):
        o, n = PF_OFF[name]
        return PF[:, o:o + n]

    IDB, BD64, BD32, PERM = cbv("ident"), cbv("bd64"), cbv("bd32"), cbv("perm")
    IDF, ONESF, EPSC = cfv("identf"), cfv("onesf"), cfv("eps")
    MASK12 = cfv("mask12")
    MASK4 = cfv("mask4")
    CVEC = pfv("cvec").rearrange("p (a b) -> p a b", a=8)
    BADA = pfv("bada").rearrange("p (a b) -> p a b", a=nlayers)
    G1T = pfv("g1").rearrange("p (a b) -> p a b", a=nlayers)
    G2T = pfv("g2").rearrange("p (a b) -> p a b", a=nlayers)
    LAMV = pfv("lam").rearrange("p (a b c) -> p a b c", a=nlayers, b=4)
    SUBLN = pfv("subln").rearrange("p (a b) -> p a b", a=nlayers)
    PSCALE = pfv("pscale").rearrange("p (a b) -> p a b", a=nlayers)
    RB = pfv("rb").rearrange("p (a b) -> p a b", a=nlayers)

    PS = [nc.alloc_psum_tensor("ps%d" % i, [128, 512], F32) for i in range(8)]
    psc = {"a": 0, "b": 0, "c": 0}
    psr = {"a": (0, 4), "b": (4, 2), "c": (6, 2)}

    def psum(role):
        base, n = psr[role]
        i = base + psc[role] % n
        psc[role] += 1
        return PS[i], ("ps", i)

    def MM(out, lhsT, rhs, start, stop, r, w, skip=False):
        if skip:
            P.op("pe", lambda e: e.matmul(out, lhsT=lhsT, rhs=rhs, start=start, stop=stop, skip_group_check=True), r, w)
        else:
            P.op("pe", lambda e: e.matmul(out, lhsT=lhsT, rhs=rhs, start=start, stop=stop), r, w)

    def TR(out, in_, ident, r, w):
        P.op("pe", lambda e: e.transpose(out=out, in_=in_, identity=ident), r, w)

    def ACT(out, in_, func, r, w, bias=None, scale=None, accum=None):
        kw = {}
        if bias is not None:
            kw["bias"] = bias
        if scale is not None:
            kw["scale"] = scale
        if accum is not None:
            kw["accum_out"] = accum
        P.op("act", lambda e: e.activation(out=out, in_=in_, func=func, **kw), r, w)

    def TS(out, in0, s1, s2, op0, op1, r, w, eng="dve"):
        if op1 is None:
            P.op(eng, lambda e: e.tensor_scalar(out=out, in0=in0, scalar1=s1, scalar2=None, op0=op0), r, w)
        else:
            P.op(eng, lambda e: e.tensor_scalar(out=out, in0=in0, scalar1=s1, scalar2=s2, op0=op0, op1=op1), r, w)

    def TT(out, in0, in1, op, r, w, eng="dve"):
        P.op(eng, lambda e: e.tensor_tensor(out=out, in0=in0, in1=in1, op=op), r, w)

    def STT(out, in0, scalar, in1, op0, op1, r, w, eng="dve"):
        P.op(eng, lambda e: e.scalar_tensor_tensor(out=out, in0=in0, scalar=scalar, in1=in1, op0=op0, op1=op1), r, w)

    def CPY(out, in_, r, w, eng="dve"):
        if eng == "act":
            P.op("act", lambda e: e.copy(out=out, in_=in_), r, w)
        else:
            P.op(eng, lambda e: e.tensor_copy(out=out, in_=in_), r, w)

    def RECIP(out, in_, r, w):
        P.op("dve", lambda e: e.reciprocal(out=out, in_=in_), r, w)

    def MEMSET(out, val, w, eng="pool"):
        P.op(eng, lambda e: e.memset(out, val), (), w)

    def DMA(eng, out, in_, stream, r, w):
        P.op(eng, lambda e: e.dma_start(out=out, in_=in_), r, w, dma=stream)

    ring_n = [0]

    def ring_load(shape, dt, dram_ap, parts=128):
        s = ring_n[0] % 4
        ring_n[0] += 1
        v = carve(OFF_RING + 4096 * s, shape, dt, parts)
        DMA("pool", v, dram_ap, "ring%d" % s, (), [("ring", s)])
        return v, ("ring", s)

    def run_units(units, depth=3):
        loaded = []
        for i in range(len(units)):
            while len(loaded) < min(len(units), i + depth):
                loaded.append(units[len(loaded)][0]())
            units[i][1](*loaded[i])

    def run_pipe(its, d=2):
        n = len(its)
        for i in range(n + d):
            if i < n:
                its[i][0]()
            if i >= d:
                its[i - d][1]()

    def tkeys(name, c, t0, n):
        return [(name, c, t) for t in range(t0 // 128, (t0 + n + 127) // 128)]

    DMA("sp", CB, cb_d, "const", (), ["CB"])
    DMA("sp", CF, cf_d, "const", (), ["CF"])
    DMA("sp", PF, pf_d, "const", (), ["PF"])
    for j in range(16):
        DMA("sp", X[:, j, :], x_d[j * 128:(j + 1) * 128, :], "xin", (), [("X", j)])
    for j in range(2):
        DMA("sp", X[:, 16 + j, :], cx_d[j * 128:(j + 1) * 128, :], "xin", (), [("X", 16 + j)])
    CONSTS = ["CB", "CF", "PF"]
    ACT(CS, CVEC, AF.Silu, CONSTS, ["CS"])
    sA = Bump([(OFF_HT, 36864)])
    WA = [sA.get([8, 256], BF16) for _ in range(4)]
    CSb = sA.get([8, 2], BF16)
    CPY(CSb, CS, ["CS"], ["CSb"], eng="dve")
    un_ = 0
    for l in range(nlayers):
        wv = wada_d[l].rearrange("(k p) n -> p k n", p=128)
        for u in range(24):
            b_ = un_ % 4
            un_ += 1
            DMA("pool", WA[b_], wv[:, :, u * 256:(u + 1) * 256], "wa%d" % b_, (), [("WA", b_)])
            for jj in range(2):
                j = u * 2 + jj
                ps, pk = psum("c")
                for k in range(8):
                    MM(ps[:, 0:2], WA[b_][:, k, jj * 128:(jj + 1) * 128], CSb[:, k, :], k == 0, k == 7,
                       [("WA", b_), "CSb"], [pk])
                TS(MOD[:, l, j, :], ps[:, 0:2], BADA[:, l, j:j + 1], None, ALU.add, None, [pk, "PF"], [("MOD", l)])
    P.barrier()

    def mod_ap(l, which, c, src):
        return MOD[:, l, which * 8 + c, src:src + 1]

    def bcast_vec(dst, l, which, src, tmp):
        for c in range(8):
            TS(tmp, IDF, mod_ap(l, which, c, src), None, ALU.mult, None, ["CF", ("MOD", l)], ["bctmp"])
            ps, pk = psum("c")
            MM(ps[:, 0:128], ONESF, tmp, True, True, ["CF", "bctmp"], [pk])
            CPY(dst[:, c * 128:(c + 1) * 128], ps[:, 0:128], [pk], ["GB"], eng="act")

    def norm_phase(l, which0, gT, tiles, scr, router=None, pre_router=None):
        XNs = [scr.get([D], F32) for _ in range(2)]
        JUNK = scr.get([D], BF16)
        for src in range(2):
            TS(GS[:, :, src], MOD[:, l, (which0 + 1) * 8:(which0 + 2) * 8, src], 1.0, None, ALU.add, None,
               [("MOD", l)], ["GS"])
            TT(GS[:, :, src], GS[:, :, src], gT[:, l, :], ALU.mult, ["GS", "PF"], ["GS"])
        if pre_router is not None:
            pre_router()
        MEMSET(SS, 0.0, [("SS", j) for j in range(18)], eng="dve")
        for j in tiles:
            src = 0 if j < 16 else 1
            XN = XNs[j % 2]
            xnk = ("XN", j % 2)
            xk = [("X", j)] + [("X", j, q) for q in range(4)]
            ACT(JUNK, X[:, j, :], AF.Square, xk, [("SS", j)], accum=SS[:, j:j + 1])
            ACT(RSTD[:, j:j + 1], SS[:, j:j + 1], AF.Ln, [("SS", j), "CF"], [("RSTD", j)], bias=EPSC, scale=1.0 / D)
            ACT(RSTD[:, j:j + 1], RSTD[:, j:j + 1], AF.Exp, [("RSTD", j)], [("RSTD", j)], scale=-0.5)
            TS(XN, X[:, j, :], RSTD[:, j:j + 1], None, ALU.mult, None, xk + [("RSTD", j)], [xnk])
            pss = [psum("a"), psum("a")]
            for c in range(8):
                ps, pk = pss[c // 4]
                TR(ps[:, (c % 4) * 128:(c % 4 + 1) * 128], XN[:, c * 128:(c + 1) * 128], IDF, [xnk, "CF"], [pk])
            if router is not None:
                router(j, src, pss)
            for c in range(8):
                ps, pk = pss[c // 4]
                ACT(HT[:, c, j * 128:(j + 1) * 128], ps[:, (c % 4) * 128:(c % 4 + 1) * 128], AF.Identity,
                    [pk, "GS", ("MOD", l)], [("HT", c, j)], bias=mod_ap(l, which0, c, src), scale=GS[:, c, src:src + 1])

    def proj_units(l, col0, ncols):
        wv = win_d[l].rearrange("(k p) n -> p k n", p=128)
        return [(lambda c0=c0: ring_load([8, 256], BF16, wv[:, :, c0:c0 + 256])) for c0 in range(col0, col0 + ncols, 256)]

    def tok_blocks(ntok):
        return [(t0, min(512, ntok - t0)) for t0 in range(0, ntok, 512)]

    def proj_fm(unit, ukey, ntok, evac):
        for cc in range(2):
            for (t0, n) in tok_blocks(ntok):
                ps, pk = psum("a")
                for k in range(8):
                    MM(ps[:, 0:n], unit[:, k, cc * 128:(cc + 1) * 128], HT[:, k, t0:t0 + n], k == 0, k == 7,
                       [ukey] + tkeys("HT", k, t0, n), [pk])
                evac(cc, t0, n, ps, pk)

    def proj_tm(unit, ukey, t0, evac):
        ps, pk = psum("a")
        for k in range(8):
            MM(ps[:, 0:256], HT[:, k, t0:t0 + 128], unit[:, k, :], k == 0, k == 7, [ukey] + tkeys("HT", k, t0, 128), [pk])
        evac(ps, pk)

    def wout_apply(l, g, ntiles, G1B, scr):
        TMP = [scr.get([512], F32) for _ in range(2)]
        wv = wout_d[l][g * 256:(g + 1) * 256, :].rearrange("(k p) n -> p k n", p=128)
        unit, ukey = ring_load([2, D], BF16, wv)
        n = 0
        for j in range(ntiles):
            src = 0 if j < 16 else 1
            for fb in range(2):
                ps, pk = psum("a")
                for k in range(2):
                    MM(ps[:, :], YG[:, k, j * 128:(j + 1) * 128], unit[:, k, fb * 512:(fb + 1) * 512], k == 0, k == 1,
                       [ukey, ("YG", k, j)], [pk])
                t = TMP[n % 2]
                tk = ("wtmp", n % 2)
                n += 1
                TT(t, ps[:, :], G1B[:, src, fb * 512:(fb + 1) * 512], ALU.mult, [pk, "GB"], [tk])
                TT(X[:, j, fb * 512:(fb + 1) * 512], X[:, j, fb * 512:(fb + 1) * 512], t, ALU.add, [("X", j), tk], [("X", j)],
                   eng="pool")

    def qk_norm_evac(SQ, RS, gain_ap, bd, out_fn):
        def evac(cc, t0, n, ps, pk):
            ACT(SQ[:, 0:n], ps[:, 0:n], AF.Square, [pk], ["SQ"])
            ps2, pk2 = psum("c")
            MM(ps2[:, 0:n], bd, SQ[:, 0:n], True, True, ["SQ", "CB"], [pk2])
            ACT(RS[:, 0:n], ps2[:, 0:n], AF.Sqrt, [pk2, "CF"], ["RS"], bias=EPSC, scale=1.0)
            RECIP(RS[:, 0:n], RS[:, 0:n], ["RS"], ["RS"])
            out_fn(cc, t0, n, ps, pk)
        return evac

    def mixer_na(l, ctx_out, G1B, scr):
        ntok = 2304
        QT = scr.get([2, 2304], BF16)
        KT = scr.get([2, 2304], BF16)
        VA = scr.get([18, 4, 65], BF16)
        VS = scr.get([15, 4, 65], BF16)
        DB = scr.get([4, 14, 64], BF16)
        SQ = scr.get([512], BF16)
        RS = scr.get([512], F32)
        GQ = scr.get([2], F32)
        PT = [scr.get([384], BF16) for _ in range(2)]
        PTC = scr.get([2, 256], BF16)
        ON = scr.get([128], F32)
        RZ = scr.get([2], F32)
        ONC = scr.get([128], F32)
        DMA("pool", DB, nab_d[l].rearrange("p (a b c) -> p a b c", a=4, b=14), "nab", (), ["DB"])
        MEMSET(VA[:, :, :, 64:65], 1.0, ["VA"])
        MEMSET(VS[:, :, :, 64:65], 1.0, ["VS"])
        naq = pfv("naq")
        nak = pfv("nak")
        TS(GQ[:, 0:1], naq[:, l:l + 1], 0.125, None, ALU.mult, None, ["PF"], ["GQ"])

        def q_out(cc, t0, n, ps, pk):
            STT(QT[:, cc, t0:t0 + n], ps[:, 0:n], GQ[:, 0:1], RS[:, 0:n], ALU.mult, ALU.mult, [pk, "RS", "GQ"], ["QT"])

        def k_out(cc, t0, n, ps, pk):
            STT(KT[:, cc, t0:t0 + n], ps[:, 0:n], nak[:, l:l + 1], RS[:, 0:n], ALU.mult, ALU.mult, [pk, "RS", "PF"], ["KT"])

        def v_comp(unit, ukey):
            for j in range(18):
                def ev(ps, pk, j=j):
                    CPY(VA[:, j, :, 0:64], ps[:, 0:256].rearrange("p (h d) -> p h d", h=4), [pk], ["VA"], eng="act")
                proj_tm(unit, ukey, j * 128, ev)
            for i in range(15):
                def ev(ps, pk, i=i):
                    CPY(VS[:, i, :, 0:64], ps[:, 0:256].rearrange("p (h d) -> p h d", h=4), [pk], ["VS"], eng="act")
                proj_tm(unit, ukey, 64 + i * 128, ev)

        lq, lk, lv = proj_units(l, 0, 256)[0], proj_units(l, 256, 256)[0], proj_units(l, 512, 256)[0]
        units = [
            (lq, lambda u, uk: proj_fm(u, uk, ntok if ctx_out else L, qk_norm_evac(SQ, RS, None, BD64, q_out))),
            (lk, lambda u, uk: proj_fm(u, uk, ntok, qk_norm_evac(SQ, RS, None, BD64, k_out))),
            (lv, v_comp),
        ]
        run_units(units)
        NPT = 4
        PT = PT + [scr.get([384], BF16) for _ in range(NPT - 2)]
        itc = [0]
        for hp in range(2):
            its = []
            state = {}
            for r in range(32):
                for hh in range(2):
                    def AB(r=r, hh=hh, hp=hp):
                        rs = min(max(r - 4, 0), 24)
                        m0b = 7 - (r - rs)
                        h = hp * 2 + hh
                        b0 = 64 * hh
                        ps, pk = psum("a")
                        q_ap = QT[b0:b0 + 64, hp, r * 64:(r + 1) * 64]
                        for c in range(6):
                            kt0 = (rs + 2 * c) * 64 if c < 4 else 2048 + (c - 4) * 128
                            MM(ps[:, c * 64:(c + 1) * 64], KT[b0:b0 + 64, hp, kt0:kt0 + 128], q_ap, True, True, ["KT", "QT"], [pk])
                        dv = DB[:, h, m0b:m0b + 7:2, :]
                        pv = ps[:, 0:256].rearrange("p (a b) -> p a b", a=4)
                        TT(pv, pv, dv, ALU.add, [pk, "DB"], [pk])
                        i_ = itc[0] % NPT
                        itc[0] += 1
                        ACT(PT[i_], ps[:, 0:384], AF.Exp, [pk], [("PT", i_)])
                        state[(r, hh)] = i_

                    def C(r=r, hh=hh, hp=hp):
                        rs = min(max(r - 4, 0), 24)
                        h = hp * 2 + hh
                        if hh == 0:
                            state[("po", r)] = psum("b")
                        po, pok = state[("po", r)]
                        i_ = state[(r, hh)]
                        pt, ptk = PT[i_], ("PT", i_)
                        for c in range(6):
                            if c < 4:
                                kr = rs + 2 * c
                                vap = VA[:, kr // 2, h, :] if kr % 2 == 0 else VS[:, (kr - 1) // 2, h, :]
                            else:
                                vap = VA[:, 16 + (c - 4), h, :]
                            MM(po[0:64, hh * 65:hh * 65 + 65], pt[:, c * 64:(c + 1) * 64], vap, c == 0, c == 5,
                               [ptk, "VA", "VS"], [pok])
                        if hh == 1:
                            RECIP(RZ[0:64, :], po[0:64, 0:130].rearrange("p (a b) -> p a b", a=2)[:, :, 64], [pok], ["RZ"])
                            for h2 in range(2):
                                TS(ON[0:64, h2 * 64:(h2 + 1) * 64], po[0:64, h2 * 65:h2 * 65 + 64], RZ[0:64, h2:h2 + 1], None, ALU.mult, None,
                                   [pok, "RZ"], ["ON"])
                            pt2, pk2 = psum("c")
                            TR(pt2[:, 0:64], ON[0:64, :], IDF[0:64, 0:64], ["ON", "CF"], [pk2])
                            CPY(YG[:, hp, r * 64:(r + 1) * 64], pt2[:, 0:64], [pk2], [("YG", hp, r // 2)], eng="act")
                    its.append((AB, C))
            run_pipe(its, d=2)
            if ctx_out:
                for qt in range(2):
                    po, pok = psum("b")
                    for hh in range(2):
                        h = hp * 2 + hh
                        b0 = 64 * hh
                        ps, pk = psum("a")
                        for kc in range(2):
                            MM(ps[:, kc * 128:(kc + 1) * 128], KT[b0:b0 + 64, hp, 2048 + kc * 128:2048 + (kc + 1) * 128],
                               QT[b0:b0 + 64, hp, 2048 + qt * 128:2048 + (qt + 1) * 128], True, True, ["KT", "QT"], [pk])
                        ACT(PTC[:, hh, :], ps[:, 0:256], AF.Exp, [pk], [("PTC", hh)])
                        for kc in range(2):
                            MM(po[:, hh * 65:hh * 65 + 65], PTC[:, hh, kc * 128:(kc + 1) * 128], VA[:, 16 + kc, h, :], kc == 0, kc == 1,
                               [("PTC", hh), "VA"], [pok])
                    RECIP(RZ[:, :], po[:, 0:130].rearrange("p (a b) -> p a b", a=2)[:, :, 64], [pok], ["RZ"])
                    for hh in range(2):
                        TS(ONC[:, hh * 64:(hh + 1) * 64], po[:, hh * 65:hh * 65 + 64], RZ[:, hh:hh + 1], None, ALU.mult, None,
                           [pok, "RZ"], ["ONC"])
                    pt2, pk2 = psum("c")
                    TR(pt2[:, 0:128], ONC[:, :], IDF, ["ONC", "CF"], [pk2])
                    CPY(YG[:, hp, 2048 + qt * 128:2048 + (qt + 1) * 128], pt2[:, 0:128], [pk2], [("YG", hp, 16 + qt)], eng="act")
        wout_apply(l, 0, 18 if ctx_out else 16, G1B, scr)


    def mixer_diff(l, ctx_out, G1B, scr):
        lam_init = 0.8 - 0.6 * math.exp(-0.3 * l)
        ROPE = scr.get([2, L], BF16)
        DMA("sp", ROPE, rope_d.rearrange("p (a b) -> p a b", a=2), "rope", (), ["ROPE"])
        LT = scr.get([2, 32], F32)
        for i in range(2):
            TT(LT[:, i, :], LAMV[:, l, 2 * i, :], LAMV[:, l, 2 * i + 1, :], ALU.mult, ["PF"], ["LT"])
            P.op("dve", lambda e, i=i: e.reduce_sum(out=SM[:, i:i + 1], in_=LT[:, i, :], axis=mybir.AxisListType.X), ["LT"], ["SM"])
        ACT(SM[:, 0:2], SM[:, 0:2], AF.Exp, ["SM"], ["SM"])
        TT(SM[:, 2:3], SM[:, 1:2], SM[:, 0:1], ALU.subtract, ["SM"], ["SM"])
        TS(NLAM[:, 0:1], SM[:, 2:3], -lam_init, None, ALU.add, None, ["SM"], ["NLAM"])
        SLG = scr.get([64], F32)
        TS(SLG, SUBLN[:, l, :], 1.0 - lam_init, None, ALU.mult, None, ["PF"], ["SLG"])
        GQ = scr.get([2], F32)
        dfq, dfk = pfv("dfq"), pfv("dfk")
        TS(GQ[:, 0:1], dfq[:, l:l + 1], 32.0 ** -0.5, None, ALU.mult, None, ["PF"], ["GQ"])
        SQ = scr.get([512], BF16)
        QG = scr.get([512], BF16)
        RS = scr.get([512], F32)
        A_ = scr.get([512], F32)
        B_ = scr.get([512], F32)
        QQ = [scr.get([2304], BF16) for _ in range(4)]
        KT = scr.get([2304], BF16)
        VA = scr.get([18, 2, 65], BF16)
        PT = [scr.get([512], BF16) for _ in range(2)]
        OO = scr.get([2, 4, 65], F32)
        RR = scr.get([2, 4], F32)
        TQ = scr.get([64], F32)
        JK = scr.get([64], F32)
        YDT = scr.get([4, 128], F32)
        MEMSET(VA[:, :, :, 64:65], 1.0, ["VA"])
        ntq = 2304 if ctx_out else L
        mark = scr.mark()
        for hp in range(2):
            def prep(gain_ap, gkeys, outs):
                def evac(cc_unused, t0, n, ps, pk):
                    ACT(SQ[:, 0:n], ps[:, 0:n], AF.Square, [pk], ["SQ"])
                    ACT(QG[:, 0:n], ps[:, 0:n], AF.Identity, [pk] + gkeys, ["QG"], scale=gain_ap)
                    ps2, pk2 = psum("c")
                    MM(ps2[:, 0:n], BD32, SQ[:, 0:n], True, True, ["SQ", "CB"], [pk2])
                    ACT(RS[:, 0:n], ps2[:, 0:n], AF.Sqrt, [pk2, "CF"], ["RS"], bias=EPSC, scale=1.0)
                    RECIP(RS[:, 0:n], RS[:, 0:n], ["RS"], ["RS"])
                    if t0 < L:
                        ps3, pk3 = psum("c")
                        MM(ps3[:, 0:n], PERM, QG[:, 0:n], True, True, ["QG", "CB"], [pk3])
                        TT(A_[:, 0:n], QG[:, 0:n], ROPE[:, 0, t0:t0 + n], ALU.mult, ["QG", "ROPE"], ["A"])
                        TT(B_[:, 0:n], ps3[:, 0:n], ROPE[:, 1, t0:t0 + n], ALU.mult, [pk3, "ROPE"], ["B"])
                        TT(A_[:, 0:n], A_[:, 0:n], B_[:, 0:n], ALU.add, ["A", "B"], ["A"], eng="pool")
                        src_ap = A_
                        sk = "A"
                    else:
                        src_ap = QG
                        sk = "QG"
                    for (dst, dk, mk) in outs:
                        if mk is None:
                            TT(dst[:, t0:t0 + n], src_ap[:, 0:n], RS[:, 0:n], ALU.mult, [sk, "RS"], [dk])
                        else:
                            STT(dst[:, t0:t0 + n], src_ap[:, 0:n], mk, RS[:, 0:n], ALU.mult, ALU.mult, [sk, "RS", "CF"], [dk])
                return evac

            def one_chunk(unit, ukey, ntok, evac, cc):
                for (t0, n) in tok_blocks(ntok):
                    ps, pk = psum("a")
                    for k in range(8):
                        MM(ps[:, 0:n], unit[:, k, cc * 128:(cc + 1) * 128], HT[:, k, t0:t0 + n], k == 0, k == 7,
                           [ukey] + tkeys("HT", k, t0, n), [pk])
                    evac(cc, t0, n, ps, pk)

            def v_comp(unit, ukey, hp=hp):
                for j in range(18):
                    def ev(ps, pk, j=j):
                        CPY(VA[:, j, :, 0:64], ps[:, hp * 128:(hp + 1) * 128].rearrange("p (h d) -> p h d", h=2), [pk], ["VA"], eng="act")
                    proj_tm(unit, ukey, j * 128, ev)

            units = [
                (proj_units(l, 768, 256)[0], lambda u, uk, hp=hp: one_chunk(u, uk, ntq, prep(GQ[:, 0:1], ["GQ"], [(QQ[j_], ("QQ", j_), MASK4[:, j_:j_ + 1]) for j_ in range(4)]), hp)),
                (proj_units(l, 1024, 256)[0], lambda u, uk, hp=hp: one_chunk(u, uk, 2304, prep(dfk[:, l:l + 1], ["PF"], [(KT, "KT", None)]), hp)),
                (proj_units(l, 1280, 256)[0], v_comp),
            ]
            run_units(units)
            NPT = 4
            if hp == 0:
                PT = PT + [scr.get([512], BF16) for _ in range(NPT - 2)]
            itc = [0]
            state = {}
            its = []
            qblocks = [(qb * 512, 512, list(range(16, 18)) + list(range(16))) for qb in range(4)]
            if ctx_out:
                qblocks.append((2048, 256, [16, 17]))
            for (q0, qn, kcs) in qblocks:
                nqt = qn // 128
                for hh in range(2):
                    for sub in range(2):
                        for ci, kc in enumerate(kcs):
                            def AB(q0=q0, qn=qn, hh=hh, sub=sub, kc=kc, ci=ci):
                                Qs, qk_ = QQ[2 * hh + sub], ("QQ", 2 * hh + sub)
                                ps, pk = psum("a")
                                MM(ps[:, 0:qn], KT[:, kc * 128:(kc + 1) * 128], Qs[:, q0:q0 + qn], True, True, ["KT", qk_], [pk])
                                i_ = itc[0] % NPT
                                itc[0] += 1
                                ACT(PT[i_][:, 0:qn], ps[:, 0:qn], AF.Exp, [pk], [("PT", i_)])
                                state[(q0, hh, sub, ci)] = i_

                            def C(q0=q0, qn=qn, nqt=nqt, hh=hh, sub=sub, kc=kc, ci=ci, nk=len(kcs), hp=hp):
                                if ci == 0:
                                    state[("po", q0, hh, sub)] = psum("b")
                                po, pok = state[("po", q0, hh, sub)]
                                i_ = state[(q0, hh, sub, ci)]
                                pt, ptk = PT[i_], ("PT", i_)
                                for qt in range(nqt):
                                    MM(po[:, qt * 65:qt * 65 + 65], pt[:, qt * 128:(qt + 1) * 128], VA[:, kc, hh, :], ci == 0 and qt == 0,
                                       ci == nk - 1, [ptk, "VA"], [pok], skip=True)
                                if ci != nk - 1:
                                    return
                                CPY(OO[:, sub, 0:nqt, :], po[:, 0:nqt * 65].rearrange("p (a b) -> p a b", a=nqt), [pok], [("OO", sub)], eng="act")
                                if sub != 1:
                                    return
                                RECIP(RR[:, :, 0:nqt], OO[:, :, 0:nqt, 64], [("OO", 0), ("OO", 1)], ["RR"])
                                TS(RR[:, 1, 0:nqt], RR[:, 1, 0:nqt], NLAM[:, 0:1], None, ALU.mult, None, ["RR", "NLAM"], ["RR"])
                                for qt in range(nqt):
                                    TS(TQ, OO[:, 0, qt, 0:64], RR[:, 0, qt:qt + 1], None, ALU.mult, None, [("OO", 0), "RR"], ["TQ"])
                                    STT(TQ, OO[:, 1, qt, 0:64], RR[:, 1, qt:qt + 1], TQ, ALU.mult, ALU.add, [("OO", 1), "RR", "TQ"], ["TQ"])
                                    TT(JK, TQ, TQ, ALU.mult, ["TQ"], ["JK"])
                                    P.op("dve", lambda e: e.reduce_sum(out=SM[:, 4:5], in_=JK, axis=mybir.AxisListType.X), ["JK"], ["SM4"])
                                    ACT(SM[:, 5:6], SM[:, 4:5], AF.Ln, ["SM4", "CF"], ["SM5"], bias=EPSC, scale=1.0 / 64)
                                    ACT(SM[:, 5:6], SM[:, 5:6], AF.Exp, ["SM5"], ["SM5"], scale=-0.5)
                                    STT(YDT[:, qt, hh * 64:(hh + 1) * 64], TQ, SM[:, 5:6], SLG, ALU.mult, ALU.mult, ["TQ", "SM5", "SLG"], [("YDT", qt)])
                                if hh != 1:
                                    return
                                for qt in range(nqt):
                                    pt2, pk2 = psum("c")
                                    TR(pt2[:, 0:128], YDT[:, qt, :], IDF, [("YDT", qt), "CF"], [pk2])
                                    tt = q0 // 128 + qt
                                    CPY(YG[:, hp, tt * 128:(tt + 1) * 128], pt2[:, 0:128], [pk2], [("YG", hp, tt)], eng="act")
                            its.append((AB, C))
            run_pipe(its, d=2)
        wout_apply(l, 1, 18 if ctx_out else 16, G1B, scr)

    def mixer_pool(l, ctx_out, G1B, scr):
        ntiles = 18 if ctx_out else 16
        ntok = ntiles * 128
        TTt = scr.get([2, 2304], BF16)
        ZP = scr.get([18, 4, 128], BF16)
        PB = scr.get([4, 5, 128], BF16)
        PW = scr.get([4, 128], BF16)
        DMA("sp", PB, pband_d.rearrange("p (a b c) -> p a b c", a=4, b=5), "pband", (), ["PB"])
        DMA("pool", PW, poolw_d[l].rearrange("p (a b) -> p a b", a=4), "poolw", (), ["PW"])

        def t_evac(cc, t0, n, ps, pk):
            CPY(TTt[:, cc, t0:t0 + n], ps[:, 0:n], [pk], [("TT", cc)], eng="act")

        run_units([(proj_units(l, 1536, 256)[0], lambda u, uk: proj_fm(u, uk, ntok, t_evac))])
        for j in range(ntiles):
            ps, pk = psum("a")
            for g in range(4):
                MM(ps[:, g * 128:(g + 1) * 128], TTt[:, g // 2, j * 128:(j + 1) * 128], PW[:, g, :], True, True, [("TT", g // 2), "PW"], [pk])
            CPY(ZP[:, j, :, :], ps[:, :].rearrange("p (a b) -> p a b", a=4), [pk], [("ZP", j)], eng="dve")
        seqs = [(0, 16)] + ([(16, 2)] if ctx_out else [])
        for (j0, nt) in seqs:
            for jo in range(nt):
                for pc in range(2):
                    terms = []
                    for g in (2 * pc, 2 * pc + 1):
                        if jo > 0:
                            terms.append((j0 + jo - 1, g, 0))
                        terms.append((j0 + jo, g, 3 if jo == 0 else (4 if jo == nt - 1 else 1)))
                        if jo < nt - 1:
                            terms.append((j0 + jo + 1, g, 2))
                    ps, pk = psum("a")
                    for i, (ji, g, kind) in enumerate(terms):
                        MM(ps[:, 0:128], ZP[:, ji, g, :], PB[:, g, kind, :], i == 0, i == len(terms) - 1, [("ZP", ji), "PB"], [pk])
                    j = j0 + jo
                    ACT(YG[:, pc, j * 128:(j + 1) * 128], ps[:, 0:128], AF.Identity, [pk, "PF"], [("YG", pc, j)], scale=PSCALE[:, l, pc:pc + 1])
        wout_apply(l, 2, ntiles, G1B, scr)

    def mixer_fft(l, ctx_out, G1B, scr):
        ntiles = 18 if ctx_out else 16
        ntok = ntiles * 128
        TTt = scr.get([2, 2304], BF16)
        TCS = scr.get([18, 4, 128], BF16)
        FW = scr.get([4, 128], F32, parts=64)
        WCS = scr.get([4, 128], BF16)
        DMA("sp", FW, fftw_d[l].rearrange("p (a b) -> p a b", a=4), "fftw", (), ["FW"])
        CS64P = scr.get([2, 2, 128], F32, parts=64)
        DMA("sp", CS64P, cs64p_d.rearrange("p (a b c) -> p a b c", a=2, b=2), "cs64p", (), ["CS64P"])
        T256 = scr.get([2, 2, 256], BF16)
        DMA("sp", T256, t256_d.rearrange("p (a b c) -> p a b c", a=2, b=2), "t256", (), ["T256"])
        C256 = T256[:, 0, :, :]
        S256 = T256[:, 1, :, :]
        for pc in range(2):
            for cs in range(2):
                ps, pk = psum("c")
                for pos in range(2):
                    MM(ps[:, 0:128], CS64P[:, cs, pos, :], FW[:, 2 * pc + pos, :], pos == 0, pos == 1, ["CS64P", "FW"], [pk])
                CPY(WCS[:, pc * 2 + cs, :], ps[:, 0:128], [pk], ["WCS"], eng="act")

        def t_evac(cc, t0, n, ps, pk):
            CPY(TTt[:, cc, t0:t0 + n], ps[:, 0:n], [pk], [("TT", cc)], eng="act")

        run_units([(proj_units(l, 1792, 256)[0], lambda u, uk: proj_fm(u, uk, ntok, t_evac))])
        for j in range(ntiles):
            ps, pk = psum("a")
            for q in range(4):
                MM(ps[:, q * 128:(q + 1) * 128], TTt[:, q // 2, j * 128:(j + 1) * 128], WCS[:, q, :], True, True, [("TT", q // 2), "WCS"], [pk])
            CPY(TCS[:, j, :, :], ps[:, :].rearrange("p (a b) -> p a b", a=4), [pk], ["TCS"], eng="dve")
        units = []
        for kb in range(16):
            def ld(kb=kb):
                c_, ck_ = ring_load([16, 128], BF16, cl_d[kb].rearrange("p (a b) -> p a b", a=16))
                s_, sk_ = ring_load([16, 128], BF16, sl_d[kb].rearrange("p (a b) -> p a b", a=16))
                return (c_, s_), (ck_, sk_)

            def comp(tabs, keys, kb=kb):
                for pc in range(2):
                    ps, pk = psum("a")
                    n = 0
                    for cs in range(2):
                        for lt in range(16):
                            MM(ps[:, 0:128], TCS[:, lt, pc * 2 + cs, :], tabs[cs][:, lt, :], n == 0, n == 31, ["TCS", keys[cs]], [pk])
                            n += 1
                    CPY(YG[:, pc, kb * 128:(kb + 1) * 128], ps[:, 0:128], [pk], [("YG", pc, kb)], eng="act")
            units.append((ld, comp))
        run_units(units, depth=2)
        if ctx_out:
            for pc in range(2):
                ps, pk = psum("a")
                n = 0
                for cs in range(2):
                    tab = C256 if cs == 0 else S256
                    for lt in range(2):
                        MM(ps[:, 0:256], TCS[:, 16 + lt, pc * 2 + cs, :], tab[:, lt, :], n == 0, n == 3, ["TCS", "T256"], [pk])
                        n += 1
                CPY(YG[:, pc, 2048:2304], ps[:, 0:256], [pk], [("YG", pc, 16), ("YG", pc, 17)], eng="act")
        wout_apply(l, 3, ntiles, G1B, scr)

    def moe_phase(l, ctx_out, scr):
        ntiles = 18 if ctx_out else 16
        ntok = ntiles * 128
        G2B = scr.get([2, D], F32)
        BT = scr.get([128], F32)
        for src in range(2 if ctx_out else 1):
            bcast_vec(G2B[:, src, :], l, 5, src, BT)
        WR = scr.get([8, 32], F32)
        WRS = scr.get([2, 8, 32], F32)
        CROW = scr.get([2, 32], F32, parts=1)
        DMA("sp", WR, rw_d[l].rearrange("p (a b) -> p a b", a=8), "rw", (), ["WR"])
        B1T = scr.get([NE, 8, 2], F32)
        DMA("sp", B1T, b1t_d[l].rearrange("p (a b c) -> p a b c", a=NE, b=8), "b1t", (), ["B1T"])
        B2 = scr.get([D], F32, parts=NE)
        DMA("sp", B2, b2_d[l], "b2", (), ["B2"])
        TS(B1T[:, :, :, 1], B1T[:, :, :, 1], 1.0, None, ALU.add, None, ["B1T"], ["B1T"])
        G = scr.get([18, 32], F32)
        GA = scr.get([18, 32], F32)
        moe_mark = scr.mark()
        XNT = scr.get([8, 128], F32)
        LG = scr.get([32], F32)
        T8 = scr.get([8], F32)
        EX = scr.get([32], F32)
        MK = scr.get([32], F32)
        GT = scr.get([128], F32, parts=NE)
        def router(j, src, pss):
            for hb in range(2):
                ps, pk = pss[hb]
                CPY(XNT[:, hb * 4:(hb + 1) * 4, :], ps[:, :].rearrange("p (a b) -> p a b", a=4), [pk], [("XNT", hb)], eng="dve")
            pl, plk = psum("c")
            for c in range(8):
                MM(pl[:, 0:32], XNT[:, c, :], WRS[:, src, c, :], c == 0, False, [("XNT", c // 4), "WRS"], [plk])
            MM(pl[:, 0:32], ONESF[0:1, :], CROW[0:1, src, :], False, True, ["CF", "CROW"], [plk])
            CPY(LG, pl[:, 0:32], [plk], ["LG"], eng="dve")
            P.op("dve", lambda e: e.max(out=T8, in_=LG), ["LG"], ["T8"])
            TS(MK, LG, T8[:, 3:4], None, ALU.is_ge, None, ["LG", "T8"], ["MK"])
            TS(SM[:, 6:7], T8[:, 0:1], -1.0, None, ALU.mult, None, ["T8"], ["SM6"])
            ACT(EX, LG, AF.Exp, ["LG", "SM6"], ["EX"], bias=SM[:, 6:7], scale=1.0)
            TT(EX, EX, MK, ALU.mult, ["EX", "MK"], ["EX"])
            P.op("dve", lambda e: e.reduce_sum(out=SM[:, 7:8], in_=EX, axis=mybir.AxisListType.X), ["EX"], ["SM7"])
            RECIP(SM[:, 7:8], SM[:, 7:8], ["SM7"], ["SM7"])
            TS(G[:, j, :], EX, SM[:, 7:8], None, ALU.mult, None, ["EX", "SM7"], [("G", j)])
            TS(GA[:, j, :], G[:, j, :], 1.0 / ALPHA, None, ALU.mult, None, [("G", j)], [("GA", j)])

        def pre_router():
            for src in range(2 if ctx_out else 1):
                for c in range(8):
                    TS(WRS[:, src, c, :], WR[:, c, :], GS[:, c, src:src + 1], None, ALU.mult, None, ["WR", "GS"], ["WRS"])
                pc_, pck = psum("c")
                for c in range(8):
                    MM(pc_[0:1, 0:32], mod_ap(l, 3, c, src), WR[:, c, :], c == 0, c == 7, [("MOD", l), "WR"], [pck])
                TT(CROW[0:1, src, :], pc_[0:1, 0:32], RB[0:1, l, :], ALU.add, [pck, "PF"], ["CROW"])

        norm_phase(l, 3, G2T, list(range(ntiles)), scr, router, pre_router)

        TMPB = scr.get([512], F32)
        for j in range(ntiles):
            src = 0 if j < 16 else 1
            pt_, ptk_ = psum("c")
            TR(pt_[0:32, 0:128], G[:, j, :], IDF, [("G", j), "CF"], [ptk_])
            CPY(GT[:, :], pt_[0:32, 0:128], [ptk_], ["GT"], eng="act")
            for fb in range(2):
                ps, pk = psum("a")
                MM(ps[:, :], GT[:, :], B2[:, fb * 512:(fb + 1) * 512], True, True, ["GT", "B2"], [pk])
                TT(TMPB, ps[:, :], G2B[:, src, fb * 512:(fb + 1) * 512], ALU.mult, [pk, "GB"], ["TMPB"])
                TT(X[:, j, fb * 512:(fb + 1) * 512], X[:, j, fb * 512:(fb + 1) * 512], TMPB, ALU.add, [("X", j), "TMPB"], [("X", j)], eng="pool")

        P.barrier()
        scr.reset(moe_mark)
        NB = 2
        GC = [scr.get([512], F32) for _ in range(NB)]
        SI = [scr.get([512], F32) for _ in range(NB)]
        L1 = [scr.get([512], F32) for _ in range(NB)]
        TO = [scr.get([256], F32) for _ in range(NB)]
        blocks = tok_blocks(ntok)
        units = []
        cnt = [0, 0]
        for e in range(n_exp):
            w1v = w1_d[l, e].rearrange("(k p) n -> p k n", p=128)
            w2v = w2_d[l, e].rearrange("(k p) n -> p k n", p=128)
            for p in range(8):
                def ld(p=p, w1v=w1v):
                    return ring_load([8, 256], BF16, w1v[:, :, p * 256:(p + 1) * 256])

                def comp(unit, ukey, e=e, p=p):
                    uv = unit.rearrange("p k (f two) -> p k two f", two=2)
                    for (t0, n) in blocks:
                        pg, pgk = psum("a")
                        pl, plk = psum("a")
                        for k in range(8):
                            MM(pg[:, 0:n], uv[:, k, 0, :], HT[:, k, t0:t0 + n], k == 0, k == 7, [ukey] + tkeys("HT", k, t0, n), [pgk])
                        for k in range(8):
                            MM(pl[:, 0:n], uv[:, k, 1, :], HT[:, k, t0:t0 + n], k == 0, k == 7, [ukey] + tkeys("HT", k, t0, n), [plk])
                        i = cnt[0] % NB
                        cnt[0] += 1
                        TS(GC[i][:, 0:n], pg[:, 0:n], B1T[:, e, p, 0:1], 7.0, ALU.add, ALU.min, [pgk, "B1T"], [("GC", i)])
                        ACT(SI[i][:, 0:n], GC[i][:, 0:n], AF.Silu, [("GC", i)], [("SI", i)], scale=ALPHA)
                        TS(L1[i][:, 0:n], pl[:, 0:n], B1T[:, e, p, 1:2], -6.0, ALU.add, ALU.max, [plk, "B1T"], [("L1", i)])
                        STT(ACTB[:, p, t0:t0 + n], L1[i][:, 0:n], 8.0, SI[i][:, 0:n], ALU.min, ALU.mult, [("L1", i), ("SI", i)],
                            [("ACTB", p, t0 // 512)])
                units.append((ld, comp))
            for q in range(4):
                def ld(q=q, w2v=w2v):
                    return ring_load([8, 256], BF16, w2v[:, :, q * 256:(q + 1) * 256])

                def comp(unit, ukey, e=e, q=q):
                    for j in range(ntiles):
                        src = 0 if j < 16 else 1
                        po, pok = psum("b")
                        for k in range(8):
                            MM(po[:, 0:256], ACTB[:, k, j * 128:(j + 1) * 128], unit[:, k, :], k == 0, k == 7, [ukey, ("ACTB", k, j // 4)], [pok])
                        i = cnt[1] % NB
                        cnt[1] += 1
                        STT(TO[i], po[:, 0:256], GA[:, j, e:e + 1], G2B[:, src, q * 256:(q + 1) * 256], ALU.mult, ALU.mult,
                            [pok, ("GA", j), "GB"], [("TO", i)])
                        TT(X[:, j, q * 256:(q + 1) * 256], X[:, j, q * 256:(q + 1) * 256], TO[i], ALU.add, [("X", j), ("X", j, q), ("TO", i)], [("X", j, q)],
                           eng="pool")
                units.append((ld, comp))
        psr["b"] = (4, 4)
        run_units(units, depth=3)
        psr["b"] = (4, 2)

    for l in range(nlayers):
        ctx_out = l < nlayers - 1
        ntiles_all = 18
        scr = Bump([(OFF_B + 9216, 36864 - 9216), (OFF_S, S_SZ)])
        G1B = scr.get([2, D], F32)
        BT = scr.get([128], F32)
        base_mark = scr.mark()
        norm_phase(l, 0, G1T, list(range(18)), scr)
        for src in range(2 if ctx_out else 1):
            bcast_vec(G1B[:, src, :], l, 2, src, BT)
        P.barrier()
        for mi, fn in enumerate((mixer_na, mixer_diff, mixer_pool, mixer_fft)):
            if mi not in mixers:
                continue
            scr.reset(base_mark)
            fn(l, ctx_out, G1B, scr)
            P.barrier()
        if stop == ("mix", l):
            break
        if do_moe:
            scr = Bump([(OFF_S, S_SZ)])
            moe_phase(l, ctx_out, scr)
            P.barrier()
        if stop == ("moe", l):
            break

    for j in range(16):
        DMA("sp", out_d[j * 128:(j + 1) * 128, :], X[:, j, :], "st", [("X", j)] + [("X", j, q) for q in range(4)], [])
    if stop is not None:
        dbg = nc.dram_tensor("dbgc", [LC, D], F32, kind="ExternalOutput").ap()
        for j in range(2):
            DMA("sp", dbg[j * 128:(j + 1) * 128, :], X[:, 16 + j, :], "st", [("X", 16 + j)] + [("X", 16 + j, q) for q in range(4)], [])
    P.emit(nc, final_waits=["st"])
    return nc, len(P.ops)


def make_in_maps(inp, nlayers=2, cores=range(8)):
    f = lambda a: np.ascontiguousarray(np.asarray(a, np.float32))
    cst = host_constants()
    lay = host_layouts(inp, nlayers)
    shared = dict(cb=cst["cb"], cf=cst["cf"], t256=cst["t256"], cs64p=cst["cs64p"], rope=cst["rope"], pband=cst["pband"], cl=cst["cl"], sl=cst["sl"],
                  wada=f(inp["w_ada"]), win=f(inp["w_in"]), wout=f(inp["w_out"]),
                  nab=lay["nab"], poolw=lay["poolw"], fftw=lay["fftw"], rw=lay["rw"], b1t=lay["b1t"], b2=lay["b2"],
                  w1=f(inp["moe_w1"]), w2=f(inp["moe_w2"]))
    x = f(inp["x"])
    cx = f(inp["ctx"])
    maps = []
    for b in cores:
        m = dict(shared)
        m["x"] = x[b]
        m["cx"] = cx[b]
        m["pf"] = host_pf(inp, b, nlayers)
        maps.append(m)
    return maps


_NC_CACHE = {}


def kernel(**inputs):
    if "nc" not in _NC_CACHE:
        _NC_CACHE["nc"] = build_nc()[0]
    nc = _NC_CACHE["nc"]
    maps = make_in_maps(inputs)
    res = run_bass_kernel_spmd(nc, maps, core_ids=list(range(8)))
    return np.stack([np.asarray(r["out"], np.float32) for r in res.results], axis=0)
```
